# Optimizing a Trainium2 kernel written in Bass

```python
import math
import jax, jax.numpy as jnp
from jax import lax
import numpy as np

D_MODEL = 1024
BATCH = 4
SEQ = 8192
DEPTH = 2

HEAD_DIM = 64
D_MIX = D_MODEL
D_CONV = D_MIX // 4
D_SSM = D_MIX // 4
D_SB = D_MIX // 4
D_NSA = D_MIX - D_CONV - D_SSM - D_SB

CONV_WIDTH = 3

SSM_GROUP = 16
SSM_N_GROUPS = D_SSM // SSM_GROUP
SSM_STATE = 64
SSM_DT_MIN = 1e-3
SSM_DT_MAX = 1e-1
SSM_MAX_RE = -1e-4

SB_HEADS = D_SB // HEAD_DIM

NSA_HEADS = D_NSA // HEAD_DIM
NSA_KV_HEADS = 2
NSA_HPG = NSA_HEADS // NSA_KV_HEADS
CMP_LEN = 32
CMP_STRIDE = 16
CMP_HIDDEN = 4 * HEAD_DIM
SLC_BLOCK = 64
SLC_TOPK = 16
WINDOW = 512
FORCE_BONUS = 1e4
NEG_INF = -1e30

Q_BLOCK = 128
ROPE_THETA = 10000.0
RMS_EPS = 1e-6

D_FF = 7 * D_MODEL // 2
N_EXPERTS = 8
TOP_K = 2
N_DENSE = (DEPTH + 1) // 2
N_MOE = DEPTH // 2

N_CONV_COLS = 3 * D_CONV
N_SSM_COLS = D_SSM
N_SB_COLS = 3 * D_SB
N_NSA_KV_COLS = 6 * NSA_KV_HEADS * HEAD_DIM
N_NSA_COLS = D_NSA + N_NSA_KV_COLS + 3 * NSA_HEADS
N_IN = N_CONV_COLS + N_SSM_COLS + N_SB_COLS + N_NSA_COLS

kernel_name = 'hybrid_parallel_mixer_block'


def rms_norm(x, g):
    xf = x.astype(jnp.float32)
    y = xf * lax.rsqrt(jnp.mean(xf * xf, axis=-1, keepdims=True) + RMS_EPS)
    return (y * g.astype(jnp.float32)).astype(x.dtype)


def rope(x, pos):
    half = HEAD_DIM // 2
    inv_freq = jnp.power(jnp.float32(ROPE_THETA), -jnp.arange(half, dtype=jnp.float32) / half)
    ang = pos.astype(jnp.float32)[..., None] * inv_freq
    cos = jnp.cos(ang)[:, :, None, :]
    sin = jnp.sin(ang)[:, :, None, :]
    xf = x.astype(jnp.float32)
    x1, x2 = xf[..., :half], xf[..., half:]
    return jnp.concatenate([x1 * cos - x2 * sin, x2 * cos + x1 * sin], axis=-1).astype(x.dtype)


def masked_softmax(scores, mask):
    p = jax.nn.softmax(jnp.where(mask, scores, NEG_INF), axis=-1)
    return jnp.where(mask, p, 0.0)


def to_blocks(a):
    return a.reshape(a.shape[0], a.shape[1] // Q_BLOCK, Q_BLOCK, *a.shape[2:]).swapaxes(0, 1)


def from_blocks(a):
    a = a.swapaxes(0, 1)
    return a.reshape(a.shape[0], a.shape[1] * a.shape[2], -1)


def short_conv_mixer(z, conv_w):
    gate_b, gate_c, u = jnp.split(z, 3, axis=-1)
    v = gate_c * u
    y = lax.conv_general_dilated(
        v, conv_w.astype(v.dtype)[:, None, :], window_strides=(1,),
        padding=[(CONV_WIDTH - 1, 0)], dimension_numbers=('NWC', 'WIO', 'NWC'),
        feature_group_count=D_CONV)
    return gate_b * y


def s5_mixer(u, lam_re, lam_im, b_re, b_im, c_re, c_im, d_skip, log_dt, w_glu):
    f32 = jnp.float32
    Bsz, T, _ = u.shape
    uf = u.astype(f32).reshape(Bsz, T, SSM_N_GROUPS, SSM_GROUP)
    lam = lax.complex(jnp.minimum(lam_re.astype(f32), SSM_MAX_RE), lam_im.astype(f32))
    dt = jnp.exp(log_dt.astype(f32))[:, None]
    lam_bar = jnp.exp(lam * dt)
    b_bar = ((lam_bar - 1.0) / lam)[..., None] * lax.complex(b_re.astype(f32), b_im.astype(f32))
    bu = jnp.einsum('gpc,btgc->btgp', b_bar, uf)
    a = jnp.broadcast_to(lam_bar, bu.shape)

    def combine(left, right):
        a_l, b_l = left
        a_r, b_r = right
        return a_r * a_l, a_r * b_l + b_r

    _, hs = lax.associative_scan(combine, (a, bu), axis=1)
    cmat = lax.complex(c_re.astype(f32), c_im.astype(f32))
    y = jnp.einsum('gcp,btgp->btgc', cmat, hs).real + d_skip.astype(f32).reshape(SSM_N_GROUPS, SSM_GROUP) * uf
    g = jax.nn.gelu(y.reshape(Bsz, T, D_SSM))
    out = g * jax.nn.sigmoid(g @ w_glu.astype(f32))
    return out.astype(u.dtype)


def stick_breaking_attention(q, k, v):
    f32 = jnp.float32
    Bsz, T, H, dh = q.shape
    scale = dh ** -0.5
    kf = k.astype(f32)
    vf = v.astype(f32)
    key_pos = jnp.arange(T)

    def block(args):
        q_blk, start = args
        z = jnp.einsum('bqhd,bshd->bhqs', q_blk.astype(f32), kf) * scale
        t = start + jnp.arange(Q_BLOCK)
        past = key_pos[None, :] < t[:, None]
        log_beta = jax.nn.log_sigmoid(z)
        log_keep = jnp.where(past, log_beta - z, 0.0)
        log_w = log_beta + lax.cumsum(log_keep, axis=3, reverse=True) - log_keep
        w = jnp.where(past, jnp.exp(log_w), 0.0)
        return jnp.einsum('bhqs,bshd->bqhd', w, vf)

    starts = jnp.arange(T // Q_BLOCK) * Q_BLOCK
    out = lax.map(block, (to_blocks(q), starts))
    return from_blocks(out).astype(q.dtype)


def compress_tokens(x, pos_emb, w1, w2):
    Bsz, T, G, dh = x.shape
    r = CMP_LEN // CMP_STRIDE
    nc = T // CMP_STRIDE - r + 1
    chunks = x.reshape(Bsz, T // CMP_STRIDE, CMP_STRIDE, G, dh)
    win = jnp.concatenate([chunks[:, j:j + nc] for j in range(r)], axis=2)
    win = win + pos_emb[:, None, :]
    win = win.transpose(0, 1, 3, 2, 4).reshape(Bsz, nc, G, CMP_LEN * dh)
    return jax.nn.silu(win @ w1) @ w2


def nsa_mixer(q, kv, gate_logits, positions, q_norm_g, k_norm_g, pos_k, pos_v, k_w1, k_w2, v_w1, v_w2):
    f32 = jnp.float32
    Bsz, T, _ = q.shape
    G, HPG, dh = NSA_KV_HEADS, NSA_HPG, HEAD_DIM
    scale = dh ** -0.5
    qh = rope(rms_norm(q.reshape(Bsz, T, NSA_HEADS, dh), q_norm_g), positions)
    qh = qh.astype(f32).reshape(Bsz, T, G, HPG, dh)
    k_cmp, v_cmp, k_slc, v_slc, k_win, v_win = jnp.moveaxis(kv.reshape(Bsz, T, 6, G, dh), 2, 0)

    nc = T // CMP_STRIDE - CMP_LEN // CMP_STRIDE + 1
    cmp_end = jnp.arange(nc) * CMP_STRIDE + CMP_LEN - 1
    kc = rope(rms_norm(compress_tokens(k_cmp, pos_k, k_w1, k_w2), k_norm_g), positions[:, cmp_end]).astype(f32)
    vc = compress_tokens(v_cmp, pos_v, v_w1, v_w2).astype(f32)

    ns = T // SLC_BLOCK
    top_n = min(SLC_TOPK, ns)
    ks = rope(rms_norm(k_slc, k_norm_g), positions).astype(f32)
    ks = ks.reshape(Bsz, ns, SLC_BLOCK, G, dh).transpose(0, 3, 1, 2, 4)
    vs = v_slc.astype(f32).reshape(Bsz, ns, SLC_BLOCK, G, dh).transpose(0, 3, 1, 2, 4)
    cmp_start = jnp.arange(nc) * CMP_STRIDE
    slc_start = jnp.arange(ns) * SLC_BLOCK
    overlap = ((cmp_start[:, None] < slc_start[None, :] + SLC_BLOCK)
               & (cmp_start[:, None] + CMP_LEN > slc_start[None, :])).astype(f32)
    blk_ids = jnp.arange(ns)
    b_ix = jnp.arange(Bsz)[:, None, None, None]
    g_ix = jnp.arange(G)[None, None, :, None]

    pad = ((0, 0), (WINDOW, 0), (0, 0), (0, 0))
    kw = jnp.pad(rope(rms_norm(k_win, k_norm_g), positions).astype(f32), pad)
    vw = jnp.pad(v_win.astype(f32), pad)

    gates = jax.nn.sigmoid(gate_logits.astype(f32)).reshape(Bsz, T, G, HPG, 3)

    def block(args):
        q_blk, g_blk, start = args
        t = start + jnp.arange(Q_BLOCK)
        m_c = (cmp_end[None, :] <= t[:, None])[None, :, None, None, :]
        p_c = masked_softmax(jnp.einsum('bqghd,bngd->bqghn', q_blk, kc) * scale, m_c)
        o_c = jnp.einsum('bqghn,bngd->bqghd', p_c, vc)
        imp = jnp.einsum('bqghn,ns->bqgs', p_c, overlap)
        cur = (t // SLC_BLOCK)[:, None]
        forced = (blk_ids[None, :] == 0) | (blk_ids[None, :] == cur) | (blk_ids[None, :] == cur - 1)
        causal = blk_ids[None, :] <= cur
        imp = jnp.where(causal[None, :, None, :], imp + FORCE_BONUS * forced[None, :, None, :], NEG_INF)
        _, idx = lax.top_k(imp, top_n)
        k_sel = ks[b_ix, g_ix, idx]
        v_sel = vs[b_ix, g_ix, idx].reshape(Bsz, Q_BLOCK, G, top_n * SLC_BLOCK, dh)
        key_pos = (idx[..., None] * SLC_BLOCK + jnp.arange(SLC_BLOCK)).reshape(Bsz, Q_BLOCK, G, 1, top_n * SLC_BLOCK)
        m_s = key_pos <= t[None, :, None, None, None]
        s_s = jnp.einsum('bqghd,bqgksd->bqghks', q_blk, k_sel).reshape(Bsz, Q_BLOCK, G, HPG, top_n * SLC_BLOCK) * scale
        o_s = jnp.einsum('bqghm,bqgmd->bqghd', masked_softmax(s_s, m_s), v_sel)
        k_w = lax.dynamic_slice_in_dim(kw, start, WINDOW + Q_BLOCK, axis=1)
        v_w = lax.dynamic_slice_in_dim(vw, start, WINDOW + Q_BLOCK, axis=1)
        wpos = start - WINDOW + jnp.arange(WINDOW + Q_BLOCK)
        m_w = ((wpos[None, :] <= t[:, None]) & (wpos[None, :] > t[:, None] - WINDOW)
               & (wpos[None, :] >= 0))[None, :, None, None, :]
        p_w = masked_softmax(jnp.einsum('bqghd,bsgd->bqghs', q_blk, k_w) * scale, m_w)
        o_w = jnp.einsum('bqghs,bsgd->bqghd', p_w, v_w)
        return g_blk[..., 0:1] * o_c + g_blk[..., 1:2] * o_s + g_blk[..., 2:3] * o_w

    starts = jnp.arange(T // Q_BLOCK) * Q_BLOCK
    out = lax.map(block, (to_blocks(qh), to_blocks(gates), starts))
    return from_blocks(out).astype(q.dtype)


def hybrid_token_mixing(a, positions, w_in, w_out, conv_w, lam_re, lam_im, b_re, b_im, c_re, c_im, d_skip,
                        log_dt, w_glu, q_norm_g, k_norm_g, pos_k, pos_v, k_w1, k_w2, v_w1, v_w2):
    Bsz, T, _ = a.shape
    z = a @ w_in
    o1 = N_CONV_COLS
    o2 = o1 + N_SSM_COLS
    o3 = o2 + N_SB_COLS
    o4 = o3 + D_NSA
    o5 = o4 + N_NSA_KV_COLS
    y_conv = short_conv_mixer(z[..., :o1], conv_w)
    y_ssm = s5_mixer(z[..., o1:o2], lam_re, lam_im, b_re, b_im, c_re, c_im, d_skip, log_dt, w_glu)
    sb = z[..., o2:o3].reshape(Bsz, T, 3, SB_HEADS, HEAD_DIM)
    y_sb = stick_breaking_attention(sb[:, :, 0], sb[:, :, 1], sb[:, :, 2])
    y_nsa = nsa_mixer(z[..., o3:o4], z[..., o4:o5], z[..., o5:], positions, q_norm_g, k_norm_g,
                      pos_k, pos_v, k_w1, k_w2, v_w1, v_w2)
    y = jnp.concatenate([y_conv, y_ssm, y_sb, y_nsa], axis=-1).astype(a.dtype)
    return y @ w_out


def swiglu(h, w_gate, w_up, w_down):
    return (jax.nn.silu(h @ w_gate) * (h @ w_up)) @ w_down


def moe_swiglu(h, router_w, w_gate, w_up, w_down):
    logits = (h @ router_w).astype(jnp.float32)
    top_val, top_idx = lax.top_k(logits, TOP_K)
    top_w = jax.nn.softmax(top_val, axis=-1)
    gate = jnp.sum(jax.nn.one_hot(top_idx, N_EXPERTS, dtype=jnp.float32) * top_w[..., None], axis=-2)
    gate = gate.astype(h.dtype)
    out = jnp.zeros_like(h)
    for e in range(N_EXPERTS):
        out = out + gate[..., e:e + 1] * swiglu(h, w_gate[e], w_up[e], w_down[e])
    return out


def setup_inputs(seed: int = 0) -> dict:
    key = jax.random.key(seed)
    keys = iter(jax.random.split(key, 64))

    def nrm(shape, scale):
        return jax.random.normal(next(keys), shape, jnp.float32) * scale

    D, F, E = D_MODEL, D_FF, N_EXPERTS
    G, P, HC = SSM_N_GROUPS, SSM_STATE, SSM_GROUP
    x = nrm((BATCH, SEQ, D), 1.0)
    c = nrm((BATCH, D), 1.0)
    positions = (jnp.arange(SEQ, dtype=jnp.int32)[None, :]
                 + jax.random.randint(next(keys), (BATCH, 1), 0, 1024, dtype=jnp.int32))
    return {
        'x': x,
        'c': c,
        'positions': positions,
        'ada_w': nrm((DEPTH, D, 6 * D), 0.5 * D ** -0.5),
        'ada_b': nrm((DEPTH, 6 * D), 0.01),
        'norm_mix_g': 1.0 + nrm((DEPTH, D), 0.02),
        'norm_ffn_g': 1.0 + nrm((DEPTH, D), 0.02),
        'w_in': nrm((DEPTH, D, N_IN), D ** -0.5),
        'w_out': nrm((DEPTH, D_MIX, D), D_MIX ** -0.5),
        'conv_w': nrm((DEPTH, CONV_WIDTH, D_CONV), CONV_WIDTH ** -0.5),
        'ssm_lam_re': -0.5 + nrm((DEPTH, G, P), 0.01),
        'ssm_lam_im': math.pi * jnp.arange(P, dtype=jnp.float32) + nrm((DEPTH, G, P), 0.01),
        'ssm_b_re': nrm((DEPTH, G, P, HC), (2 * HC) ** -0.5),
        'ssm_b_im': nrm((DEPTH, G, P, HC), (2 * HC) ** -0.5),
        'ssm_c_re': nrm((DEPTH, G, HC, P), P ** -0.5),
        'ssm_c_im': nrm((DEPTH, G, HC, P), P ** -0.5),
        'ssm_d': nrm((DEPTH, D_SSM), 1.0),
        'ssm_log_dt': jax.random.uniform(next(keys), (DEPTH, G), dtype=jnp.float32,
                                         minval=math.log(SSM_DT_MIN), maxval=math.log(SSM_DT_MAX)),
        'ssm_w_glu': nrm((DEPTH, D_SSM, D_SSM), D_SSM ** -0.5),
        'nsa_q_norm_g': 1.0 + nrm((DEPTH, HEAD_DIM), 0.02),
        'nsa_k_norm_g': 1.0 + nrm((DEPTH, HEAD_DIM), 0.02),
        'cmp_pos_k': nrm((DEPTH, CMP_LEN, HEAD_DIM), 0.1),
        'cmp_pos_v': nrm((DEPTH, CMP_LEN, HEAD_DIM), 0.1),
        'cmp_k_w1': nrm((DEPTH, CMP_LEN * HEAD_DIM, CMP_HIDDEN), (CMP_LEN * HEAD_DIM) ** -0.5),
        'cmp_k_w2': nrm((DEPTH, CMP_HIDDEN, HEAD_DIM), CMP_HIDDEN ** -0.5),
        'cmp_v_w1': nrm((DEPTH, CMP_LEN * HEAD_DIM, CMP_HIDDEN), (CMP_LEN * HEAD_DIM) ** -0.5),
        'cmp_v_w2': nrm((DEPTH, CMP_HIDDEN, HEAD_DIM), CMP_HIDDEN ** -0.5),
        'ffn_w_gate': nrm((N_DENSE, D, F), D ** -0.5),
        'ffn_w_up': nrm((N_DENSE, D, F), D ** -0.5),
        'ffn_w_down': nrm((N_DENSE, F, D), F ** -0.5),
        'moe_router': nrm((N_MOE, D, E), D ** -0.5),
        'moe_w_gate': nrm((N_MOE, E, D, F), D ** -0.5),
        'moe_w_up': nrm((N_MOE, E, D, F), D ** -0.5),
        'moe_w_down': nrm((N_MOE, E, F, D), F ** -0.5),
    }


def reference(x, c, positions, ada_w, ada_b, norm_mix_g, norm_ffn_g, w_in, w_out, conv_w,
              ssm_lam_re, ssm_lam_im, ssm_b_re, ssm_b_im, ssm_c_re, ssm_c_im, ssm_d, ssm_log_dt, ssm_w_glu,
              nsa_q_norm_g, nsa_k_norm_g, cmp_pos_k, cmp_pos_v, cmp_k_w1, cmp_k_w2, cmp_v_w1, cmp_v_w2,
              ffn_w_gate, ffn_w_up, ffn_w_down, moe_router, moe_w_gate, moe_w_up, moe_w_down):
    h = x
    cond = jax.nn.silu(c)
    for layer in range(DEPTH):
        mod = (cond @ ada_w[layer] + ada_b[layer])[:, None, :]
        shift_mix, scale_mix, gate_mix, shift_ffn, scale_ffn, gate_ffn = jnp.split(mod, 6, axis=-1)
        a = rms_norm(h, norm_mix_g[layer]) * (1.0 + scale_mix) + shift_mix
        mix = hybrid_token_mixing(
            a, positions, w_in[layer], w_out[layer], conv_w[layer],
            ssm_lam_re[layer], ssm_lam_im[layer], ssm_b_re[layer], ssm_b_im[layer],
            ssm_c_re[layer], ssm_c_im[layer], ssm_d[layer], ssm_log_dt[layer], ssm_w_glu[layer],
            nsa_q_norm_g[layer], nsa_k_norm_g[layer], cmp_pos_k[layer], cmp_pos_v[layer],
            cmp_k_w1[layer], cmp_k_w2[layer], cmp_v_w1[layer], cmp_v_w2[layer])
        h = h + gate_mix * mix
        a = rms_norm(h, norm_ffn_g[layer]) * (1.0 + scale_ffn) + shift_ffn
        i = layer // 2
        if layer % 2 == 0:
            f = swiglu(a, ffn_w_gate[i], ffn_w_up[i], ffn_w_down[i])
        else:
            f = moe_swiglu(a, moe_router[i], moe_w_gate[i], moe_w_up[i], moe_w_down[i])
        h = h + gate_ffn * f
    return h
```

```python
import math
from contextlib import ExitStack

import numpy as np
import concourse.bass as bass
import concourse.mybir as mybir
from concourse.bass_utils import run_bass_kernel_spmd

F32 = mybir.dt.float32
BF16 = mybir.dt.bfloat16
I32 = mybir.dt.int32
AF = mybir.ActivationFunctionType
ALU = mybir.AluOpType
AX = mybir.AxisListType

D = 1024
T = 8192
NB = 4
DFF = 3584
NEXP = 8
N_IN = 2828
GELU_K = 1.5957691216057308


class Buf:
    __slots__ = ("t", "name", "w", "r", "dsem", "dcnt", "excl")

    def __init__(self, t, name):
        self.t = t
        self.name = name
        self.w = None
        self.r = {}
        self.dsem = None
        self.dcnt = 0
        self.excl = False

    def __getitem__(self, idx):
        return self.t[idx]


class View:
    def __init__(self, parent, ap):
        object.__setattr__(self, "parent", parent)
        object.__setattr__(self, "t", ap)

    def __getitem__(self, idx):
        return self.t[idx]

    def __getattr__(self, k):
        return getattr(object.__getattribute__(self, "parent"), k)

    def __setattr__(self, k, v):
        setattr(object.__getattribute__(self, "parent"), k, v)


SEM_LIMIT = 30000


class KB:
    def __init__(self, strict=True, num_devices=None):
        if num_devices is None:
            self.nc = bass.Bass("TRN2", target_bir_lowering=False)
        else:
            self.nc = bass.Bass("TRN2", target_bir_lowering=False, num_devices=num_devices)
        nc = self.nc
        self.eng = {"pe": nc.tensor, "act": nc.scalar, "dve": nc.vector, "pool": nc.gpsimd, "sp": nc.sync}
        self.esem = {k: nc.alloc_semaphore(name="es_" + k) for k in self.eng}
        self.ecnt = {k: 0 for k in self.eng}
        self.eold = {}
        self.waited = {k: {} for k in self.eng}
        self.strict = strict
        self.nbuf = 0
        self.ninst = 0
        self.nsem = len(self.eng)
        self.stack = ExitStack()
        self.phases = []
        self.sem_pool = []

    def dram(self, name, shape, dt, kind="Internal"):
        t = self.nc.dram_tensor(name, list(shape), dt, kind=kind)
        return Buf(t.ap(), name)

    def _alloc(self, ctx, name, stack):
        ph = self.phases[-1] if (self.phases and stack is None) else None
        st = stack or (ph.st if ph is not None else self.stack)
        b = Buf(st.enter_context(ctx), name)
        if ph is not None:
            ph.bufs.append(b)
        return b

    def sb(self, shape, dt, name=None, stack=None):
        self.nbuf += 1
        name = (name or "sb") + f"_{self.nbuf}"
        return self._alloc(self.nc.sbuf_tensor(name, list(shape), dt), name, stack)

    def ps(self, name=None, stack=None):
        self.nbuf += 1
        name = (name or "ps") + f"_{self.nbuf}"
        b = self._alloc(self.nc.psum_tensor(name, [128, 512], F32), name, stack)
        b.excl = True
        return b

    def _new_sem(self, name):
        self.nsem += 1
        return self.nc.alloc_semaphore(name=f"{name}_{self.nsem}")

    def _wait(self, e, ev):
        if ev is None:
            return
        sem, val = ev
        if sem is self.esem[e] and (e == "pe" or not self.strict):
            return
        k = id(sem)
        if self.waited[e].get(k, 0) >= val:
            return
        self.eng[e].wait_ge(sem, val)
        self.waited[e][k] = val

    def _deps(self, e, reads, writes):
        for b in reads:
            self._wait(e, b.w)
            if b.excl:
                for ev in b.r.values():
                    self._wait(e, ev)
        for b in writes:
            self._wait(e, b.w)
            for ev in b.r.values():
                self._wait(e, ev)

    @staticmethod
    def _record(ev, reads, writes):
        for b in reads:
            b.r[id(ev[0])] = ev
        for b in writes:
            b.w = ev
            b.r = {}

    def op(self, e, fn, reads=(), writes=()):
        self._deps(e, reads, writes)
        ins = fn(self.eng[e])
        self.ecnt[e] += 1
        ins.then_inc(self.esem[e], 1)
        self._record((self.esem[e], self.ecnt[e]), reads, writes)
        self.ninst += 1
        if self.ecnt[e] >= SEM_LIMIT:
            self.eold[e] = (self.esem[e], self.ecnt[e])
            self.esem[e] = self._new_sem("es_" + e)
            self.ecnt[e] = 0
        return ins

    def dma(self, q, out_buf, out_ap, in_buf, in_ap, anchor=None, **kw):
        if anchor is None:
            anchor = out_buf
        if anchor.dsem is None or anchor.dcnt >= SEM_LIMIT:
            got = False
            while self.sem_pool:
                sem, cnt = self.sem_pool.pop()
                if cnt < SEM_LIMIT - 4096:
                    anchor.dsem, anchor.dcnt, got = sem, cnt, True
                    break
            if not got:
                anchor.dsem, anchor.dcnt = self._new_sem("ds"), 0
        self._deps(q, [in_buf], [out_buf])
        ins = self.eng[q].dma_start(out=out_ap, in_=in_ap, **kw)
        anchor.dcnt += 16
        ins.then_inc(anchor.dsem, 16)
        self._record((anchor.dsem, anchor.dcnt), [in_buf], [out_buf])
        self.ninst += 1
        return ins

    def gather(self, out_buf, out_ap, in_buf, in_ap, idx_buf, idx_ap):
        anchor = out_buf
        if anchor.dsem is None or anchor.dcnt >= SEM_LIMIT:
            got = False
            while self.sem_pool:
                sem, cnt = self.sem_pool.pop()
                if cnt < SEM_LIMIT - 4096:
                    anchor.dsem, anchor.dcnt, got = sem, cnt, True
                    break
            if not got:
                anchor.dsem, anchor.dcnt = self._new_sem("ds"), 0
        self._deps("pool", [in_buf, idx_buf], [out_buf])
        ins = self.nc.gpsimd.indirect_dma_start(out=out_ap, out_offset=None, in_=in_ap,
                                                in_offset=bass.IndirectOffsetOnAxis(ap=idx_ap, axis=0))
        anchor.dcnt += 16
        ins.then_inc(anchor.dsem, 16)
        self._record((anchor.dsem, anchor.dcnt), [in_buf, idx_buf], [out_buf])
        self.ninst += 1
        return ins

    def _last_ev(self, k):
        if self.ecnt[k] > 0:
            return (self.esem[k], self.ecnt[k])
        return self.eold.get(k)

    def barrier(self, bufs=()):
        for e in self.eng:
            for k in self.eng:
                if k != e:
                    self._wait(e, self._last_ev(k))
            for b in bufs:
                if b.dsem is not None and b.dcnt > 0:
                    self._wait(e, (b.dsem, b.dcnt))

    def phase(self):
        return _Phase(self)

    def finish(self, out_bufs):
        for b in out_bufs:
            self._wait("sp", b.w)
        for k in self.eng:
            if k != "sp":
                self._wait("sp", self._last_ev(k))
        self.stack.close()


class _Phase:
    def __init__(self, kb):
        self.kb = kb
        self.st = ExitStack()
        self.bufs = []

    def __enter__(self):
        self.kb.phases.append(self)
        return self

    def sb(self, shape, dt, name=None):
        b = self.kb.sb(shape, dt, name, self.st)
        self.bufs.append(b)
        return b

    def ps(self, name=None):
        b = self.kb.ps(name, self.st)
        self.bufs.append(b)
        return b

    def __exit__(self, *a):
        assert self.kb.phases.pop() is self
        if a[0] is not None:
            return False
        self.kb.barrier(self.bufs)
        for b in self.bufs:
            if b.dsem is not None:
                self.kb.sem_pool.append((b.dsem, b.dcnt))
                b.dsem = None
        self.st.close()
        return False


def mm(kb, out_buf, out_ap, a_buf, lhsT, b_buf, rhs, start, stop, skip=False):
    kw = {"skip_group_check": True} if skip else {}
    return kb.op("pe", lambda e: e.matmul(out_ap, lhsT=lhsT, rhs=rhs, start=start, stop=stop, **kw),
                 [a_buf, b_buf], [out_buf])


def make_ident(kb, dt=F32):
    ident = kb.sb([128, 128], dt, "ident")
    kb.op("pool", lambda e: e.memset(ident[:], 1.0), [], [ident])
    kb.op("pool", lambda e: e.affine_select(out=ident[:], in_=ident[:], pattern=[[1, 128]],
                                            compare_op=ALU.is_equal, fill=0.0, base=0, channel_multiplier=-1),
          [ident], [ident])
    return ident


def emit_mod(kb, ph, modT, ccol, adaw, adab, psA, psB):
    cs = ph.sb([128, 8], F32, "cs")
    cond = ph.sb([128, 8], F32, "cond")
    brow = ph.sb([1, 6144], F32, "brow")
    one = ph.sb([1, 1], F32, "one")
    wb = [ph.sb([128, 8, 512], F32, "adawb") for _ in range(2)]
    modrow = ph.sb([1, 6144], F32, "modrow")
    kb.dma("sp", cs, cs[:], ccol, ccol[:])
    kb.dma("sp", brow, brow[:], adab, adab[:])
    kb.op("act", lambda e: e.activation(out=cond[:], in_=cs[:], func=AF.Silu), [cs], [cond])
    kb.op("dve", lambda e: e.memset(one[:], 1.0), [], [one])
    adaw_v = adaw.t.rearrange("(kt p) n -> p kt n", p=128)
    for cc in range(12):
        w = wb[cc % 2]
        kb.dma("sp", w, w[:], adaw, adaw_v[:, :, cc * 512:(cc + 1) * 512])
        for kt in range(8):
            mm(kb, psA, psA[0:1, :], cond, cond[:, kt:kt + 1], w, w[:, kt, :], kt == 0, kt == 7)
        kb.op("dve", lambda e: e.tensor_tensor(out=modrow[0:1, cc * 512:(cc + 1) * 512], in0=psA[0:1, :],
                                               in1=brow[0:1, cc * 512:(cc + 1) * 512], op=ALU.add),
              [psA, brow], [modrow])
    for j in range(48):
        mm(kb, psB, psB[:, j:j + 1], modrow, modrow[0:1, j * 128:(j + 1) * 128], one, one[0:1, 0:1], True, True)
    kb.op("dve", lambda e: e.tensor_copy(out=modT[:], in_=psB[:, 0:48]), [psB], [modT])
    return modrow, modT


def emit_bcast_row(kb, dst, modrow, c0, ones_row, psA):
    for cc in range(2):
        mm(kb, psA, psA[:, :], ones_row, ones_row[0:1, :], modrow, modrow[0:1, c0 + cc * 512:c0 + (cc + 1) * 512],
           True, True)
        kb.op("dve", lambda e: e.tensor_copy(out=dst[:, cc * 512:(cc + 1) * 512], in_=psA[:, :]), [psA], [dst])


def emit_gmod(kb, modT, gcol_d, jscale, name):
    g = kb.sb([128, 8], F32, name + "_g")
    gm = kb.sb([128, 8], F32, name)
    kb.dma("sp", g, g[:], gcol_d, gcol_d[:])
    kb.op("dve", lambda e: e.tensor_scalar(out=gm[:], in0=modT[:, jscale:jscale + 8], scalar1=1.0, scalar2=None,
                                           op0=ALU.add), [modT], [gm])
    kb.op("dve", lambda e: e.tensor_tensor(out=gm[:], in0=gm[:], in1=g[:], op=ALU.mult), [gm, g], [gm])
    return gm


def emit_norm_T(kb, src, src_ap, gm, modT, jshift, ident, pT, aT_views, aTf_views, tmp):
    junk, xs, stt = tmp["junk"], tmp["xs"], tmp["st"]
    kb.op("act", lambda e: e.activation(out=junk[:], in_=src_ap, func=AF.Square, accum_out=stt[:, 0:1]),
          [src], [junk, stt])
    kb.op("dve", lambda e: e.tensor_scalar(out=stt[:, 1:2], in0=stt[:, 0:1], scalar1=1.0 / D, scalar2=1e-6,
                                           op0=ALU.mult, op1=ALU.add), [stt], [stt])
    kb.op("act", lambda e: e.activation(out=stt[:, 2:3], in_=stt[:, 1:2], func=AF.Sqrt), [stt], [stt])
    kb.op("dve", lambda e: e.reciprocal(out=stt[:, 3:4], in_=stt[:, 2:3]), [stt], [stt])
    kb.op("dve", lambda e: e.tensor_scalar(out=xs[:], in0=src_ap, scalar1=stt[:, 3:4], scalar2=None, op0=ALU.mult),
          [src, stt], [xs])
    for kt in range(8):
        pb = pT[kt // 4]
        kb.op("pe", lambda e: e.transpose(out=pb[:, (kt % 4) * 128:(kt % 4 + 1) * 128],
                                          in_=xs[:, kt * 128:(kt + 1) * 128], identity=ident[:]),
              [xs, ident], [pb])
    for kt in range(8):
        pb = pT[kt // 4]
        pin = pb[:, (kt % 4) * 128:(kt % 4 + 1) * 128]
        ob, oap = aT_views[kt]
        if kt % 2 == 0:
            kb.op("dve", lambda e: e.tensor_scalar(out=oap, in0=pin, scalar1=gm[:, kt:kt + 1],
                                                   scalar2=modT[:, jshift + kt:jshift + kt + 1],
                                                   op0=ALU.mult, op1=ALU.add), [pb, gm, modT], [ob])
        else:
            kb.op("act", lambda e: e.activation(out=oap, in_=pin, func=AF.Identity,
                                                scale=gm[:, kt:kt + 1], bias=modT[:, jshift + kt:jshift + kt + 1]),
                  [pb, gm, modT], [ob])
        if aTf_views is not None:
            fb, fap = aTf_views[kt]
            if kt % 2 == 1:
                kb.op("dve", lambda e: e.tensor_scalar(out=fap, in0=pin, scalar1=gm[:, kt:kt + 1],
                                                       scalar2=modT[:, jshift + kt:jshift + kt + 1],
                                                       op0=ALU.mult, op1=ALU.add), [pb, gm, modT], [fb])
            else:
                kb.op("act", lambda e: e.activation(out=fap, in_=pin, func=AF.Identity,
                                                    scale=gm[:, kt:kt + 1],
                                                    bias=modT[:, jshift + kt:jshift + kt + 1]),
                      [pb, gm, modT], [fb])


def build_P(ntok=4096):
    kb = KB()
    NT = ntok // 128
    x = kb.dram("x", [ntok, D], F32, "ExternalInput")
    ccol = kb.dram("ccol", [128, 8], F32, "ExternalInput")
    adaw = kb.dram("adaw", [D, 6 * D], F32, "ExternalInput")
    adab = kb.dram("adab", [1, 6 * D], F32, "ExternalInput")
    gcol = kb.dram("gcol", [128, 8], F32, "ExternalInput")
    win = kb.dram("win", [D, N_IN], F32, "ExternalInput")
    z = kb.dram("z", [ntok, N_IN], F32, "ExternalOutput")

    ps = [kb.ps(f"P{i}") for i in range(6)]
    modT = kb.sb([128, 48], F32, "modT")
    with kb.phase() as ph:
        modrow, modT = emit_mod(kb, ph, modT, ccol, adaw, adab, ps[0], ps[1])
    gm = emit_gmod(kb, modT, gcol, 8, "gm1")
    ident = make_ident(kb)
    wb = kb.sb([128, 8, N_IN], BF16, "winb")
    win_v = win.t.rearrange("(kt p) n -> p kt n", p=128)
    for kt in range(8):
        kb.dma("pool", wb, wb[:, kt, :], win, win_v[:, kt, :])
    xt = [kb.sb([128, D], F32, "xt") for _ in range(2)]
    aT = [kb.sb([128, 8, 128], BF16, "aT") for _ in range(2)]
    zt = [kb.sb([128, N_IN], F32, "zt") for _ in range(2)]
    tmp = {"junk": kb.sb([128, D], F32, "junk"), "xs": kb.sb([128, D], F32, "xs"), "st": kb.sb([128, 4], F32, "st")}
    chunks = [(c0, min(512, N_IN - c0)) for c0 in range(0, N_IN, 512)]
    for i in range(NT):
        xb, ab, zb = xt[i % 2], aT[i % 2], zt[i % 2]
        kb.dma("sp", xb, xb[:], x, x[i * 128:(i + 1) * 128, :])
        emit_norm_T(kb, xb, xb[:], gm, modT, 0, ident, ps[0:2], [(ab, ab[:, kt, :]) for kt in range(8)], None, tmp)
        for ci, (c0, cw) in enumerate(chunks):
            pz = ps[2 + ci % 4]
            for kt in range(8):
                mm(kb, pz, pz[:, 0:cw], ab, ab[:, kt, :], wb, wb[:, kt, c0:c0 + cw], kt == 0, kt == 7)
            if ci % 2 == 0:
                kb.op("act", lambda e: e.copy(zb[:, c0:c0 + cw], pz[:, 0:cw]), [pz], [zb])
            else:
                kb.op("dve", lambda e: e.tensor_copy(out=zb[:, c0:c0 + cw], in_=pz[:, 0:cw]), [pz], [zb])
        kb.dma("sp", z, z[i * 128:(i + 1) * 128, :], zb, zb[:], anchor=zb)
    kb.finish([z])
    return kb


def emit_F(kb, ps, ident, moe, ntok, x, yT, ccol, adaw, adab, gcol, wglu, wout, wg, wu, wd, router, hout, h1s,
           gather=None):
    CH = 1024
    NCH = ntok // CH
    NE = NEXP if moe else 1
    with kb.phase():
        Gmix = kb.sb([128, D], F32, "Gmix")
        Gffn = kb.sb([128, D], F32, "Gffn")
        modT = kb.sb([128, 48], F32, "modT")
        with kb.phase() as ph:
            modrow, modT = emit_mod(kb, ph, modT, ccol, adaw, adab, ps[0], ps[1])
            ones_row = ph.sb([1, 128], F32, "ones_row")
            kb.op("dve", lambda e: e.memset(ones_row[:], 1.0), [], [ones_row])
            emit_bcast_row(kb, Gmix, modrow, 2 * D, ones_row, ps[0])
            emit_bcast_row(kb, Gffn, modrow, 5 * D, ones_row, ps[0])
        gm = emit_gmod(kb, modT, gcol, 32, "gm2")

        woutb = kb.sb([128, 8, D], BF16, "woutb")
        wout_v = wout.t.rearrange("(kt p) n -> p kt n", p=128)
        for kt in range(8):
            kb.dma("pool", woutb, woutb[:, kt, :], wout, wout_v[:, kt, :])
        wglub = kb.sb([128, 2, 256], BF16, "wglub")
        kb.dma("pool", wglub, wglub[:], wglu, wglu.t.rearrange("(kt p) n -> p kt n", p=128))
        if moe:
            routf = kb.sb([128, 8, NEXP], F32, "routf")
            kb.dma("sp", routf, routf[:], router, router.t.rearrange("(kt p) n -> p kt n", p=128))
            gates = kb.sb([128, 8, NEXP], F32, "gates")
            a2f = [kb.sb([128, 8, 128], F32, "a2f") for _ in range(2)]
            rt = {k: kb.sb([128, 8], F32, "rt_" + k) for k in ("lg", "m8", "d", "mask")}
            rs = kb.sb([128, 4], F32, "rt_s")

        yTb = kb.sb([128, 8, 512], BF16, "yTb")
        ys = kb.sb([128, 2, 512], F32, "ys")
        t1 = kb.sb([128, 2, 512], F32, "t1")
        t2 = kb.sb([128, 2, 512], F32, "t2")
        gb = kb.sb([128, 2, 512], BF16, "gb")
        xt = [kb.sb([128, D], F32, "xt") for _ in range(2)]
        tmpm = kb.sb([128, D], F32, "tmpm")
        h1t = [kb.sb([128, D], F32, "h1t") for _ in range(2)]
        tmp = {"junk": kb.sb([128, D], F32, "junk"), "xs": kb.sb([128, D], F32, "xs"), "st": kb.sb([128, 4], F32, "st")}
        a2T = kb.sb([128, 8, CH], BF16, "a2T")
        acc = kb.sb([128, CH // 128, D], F32, "acc")
        hT = kb.sb([128, 4, CH], BF16, "hT")
        sg = [kb.sb([128, 512], F32, "sg") for _ in range(2)]
        wgb = [kb.sb([128, 8, 512], BF16, "wgb") for _ in range(2)]
        wub = [kb.sb([128, 8, 512], BF16, "wub") for _ in range(2)]
        wdb = [kb.sb([128, 4, D], BF16, "wdb") for _ in range(2)]
        ot = kb.sb([128, D], F32, "ot")

        yT_v = yT.t.rearrange("(ct p) t -> p ct t", p=128) if gather is None else None
        if gather is not None:
            gidx = kb.sb([128, ntok // 128], I32, "gidx")
            kb.dma("sp", gidx, gidx[:], gather["idx"], gather["idx"][:])
            gidx_u = gidx[:].bitcast(mybir.dt.uint32)
            ytile = [kb.sb([128, D], F32, "ytile") for _ in range(2)]
            ytok = gather["ytok"]
        units = [(e, fg) for e in range(NE) for fg in range(DFF // 512)]

        def load_unit(ui, slot):
            e, fg = units[ui]
            gv = wg.t[e].rearrange("(kt p) f -> p kt f", p=128)
            uv = wu.t[e].rearrange("(kt p) f -> p kt f", p=128)
            dv = wd.t[e].rearrange("(ft p) d -> p ft d", p=128)
            kb.dma("pool", wgb[slot], wgb[slot][:], wg, gv[:, :, fg * 512:(fg + 1) * 512])
            kb.dma("pool", wub[slot], wub[slot][:], wu, uv[:, :, fg * 512:(fg + 1) * 512])
            kb.dma("pool", wdb[slot], wdb[slot][:], wd, dv[:, fg * 4:(fg + 1) * 4, :])

        for c in range(NCH):
            t0 = c * CH
            for hc in range(CH // 512):
                ta = t0 + hc * 512
                if gather is None:
                    kb.dma("sp", ys, ys[:], yT, yT_v[:, 2:4, ta:ta + 512])
                    kb.dma("pool", yTb, yTb[:, 0:2, :], yT, yT_v[:, 0:2, ta:ta + 512])
                    kb.dma("pool", yTb, yTb[:, 4:8, :], yT, yT_v[:, 4:8, ta:ta + 512])
                else:
                    for tt in range(4):
                        gi = ta // 128 + tt
                        yb_ = ytile[tt % 2]
                        kb.gather(yb_, yb_[:], ytok, ytok.t[:, :], gidx, gidx_u[:, gi:gi + 1])
                        for ct in range(8):
                            pb = ps[4 + ct // 4]
                            kb.op("pe", lambda e: e.transpose(out=pb[:, (ct % 4) * 128:(ct % 4 + 1) * 128],
                                                              in_=yb_[:, ct * 128:(ct + 1) * 128], identity=ident[:]),
                                  [yb_, ident], [pb])
                        for ct in range(8):
                            pb = ps[4 + ct // 4]
                            pin = pb[:, (ct % 4) * 128:(ct % 4 + 1) * 128]
                            if ct in (2, 3):
                                kb.op("dve", lambda e: e.tensor_copy(out=ys[:, ct - 2, tt * 128:(tt + 1) * 128], in_=pin),
                                      [pb], [ys])
                            else:
                                kb.op("act", lambda e: e.copy(yTb[:, ct, tt * 128:(tt + 1) * 128], pin), [pb], [yTb])
                kb.op("pool", lambda e: e.tensor_tensor(out=t1[:], in0=ys[:], in1=ys[:], op=ALU.mult), [ys], [t1])
                kb.op("dve", lambda e: e.tensor_scalar(out=t1[:], in0=t1[:], scalar1=0.044715, scalar2=1.0,
                                                       op0=ALU.mult, op1=ALU.add), [t1], [t1])
                kb.op("pool", lambda e: e.tensor_tensor(out=t1[:], in0=t1[:], in1=ys[:], op=ALU.mult), [t1, ys], [t1])
                kb.op("act", lambda e: e.activation(out=t2[:], in_=t1[:], func=AF.Sigmoid, scale=GELU_K), [t1], [t2])
                kb.op("dve", lambda e: e.tensor_tensor(out=t1[:], in0=t2[:], in1=ys[:], op=ALU.mult), [t2, ys], [t1])
                kb.op("act", lambda e: e.copy(gb[:], t1[:]), [t1], [gb])
                for jt in range(2):
                    pp = ps[jt]
                    for ct in range(2):
                        mm(kb, pp, pp[:, :], wglub, wglub[:, ct, jt * 128:(jt + 1) * 128], gb, gb[:, ct, :],
                           ct == 0, ct == 1)
                    kb.op("act", lambda e: e.activation(out=t2[:, jt, :], in_=pp[:, :], func=AF.Sigmoid), [pp], [t2])
                    kb.op("dve", lambda e: e.tensor_tensor(out=yTb[:, 2 + jt, :], in0=t1[:, jt, :], in1=t2[:, jt, :],
                                                           op=ALU.mult), [t1, t2], [yTb])
                def tileA(tt):
                    ti = hc * 4 + tt
                    tg = (t0 // 128) + ti
                    xb = xt[ti % 2]
                    hb = h1t[ti % 2]
                    if gather is None:
                        kb.dma("sp", xb, xb[:], x, x[tg * 128:(tg + 1) * 128, :])
                    else:
                        kb.gather(xb, xb[:], x, x.t[:, :], gidx, gidx_u[:, tg:tg + 1])
                    for cc in range(2):
                        pm = ps[2 + cc]
                        for ct in range(8):
                            mm(kb, pm, pm[:, :], yTb, yTb[:, ct, tt * 128:(tt + 1) * 128], woutb,
                               woutb[:, ct, cc * 512:(cc + 1) * 512], ct == 0, ct == 7)
                        kb.op("dve", lambda e: e.tensor_tensor(out=tmpm[:, cc * 512:(cc + 1) * 512], in0=pm[:, :],
                                                               in1=Gmix[:, cc * 512:(cc + 1) * 512], op=ALU.mult),
                              [pm, Gmix], [tmpm])
                    kb.op("pool", lambda e: e.tensor_tensor(out=hb[:], in0=tmpm[:], in1=xb[:], op=ALU.add),
                          [tmpm, xb], [hb])
                    kb.dma("act", h1s, h1s[tg * 128:(tg + 1) * 128, :], hb, hb[:], anchor=hb)

                def tileB(tt):
                    ti = hc * 4 + tt
                    hb = h1t[ti % 2]
                    af = a2f[ti % 2] if moe else None
                    emit_norm_T(kb, hb, hb[:], gm, modT, 24, ident, ps[4:6],
                                [(a2T, a2T[:, kt, ti * 128:(ti + 1) * 128]) for kt in range(8)],
                                [(af, af[:, kt, :]) for kt in range(8)] if moe else None, tmp)
                    if moe:
                        pr = ps[6]
                        for kt in range(8):
                            mm(kb, pr, pr[:, 0:NEXP], af, af[:, kt, :], routf, routf[:, kt, :], kt == 0, kt == 7)
                        lg, m8, dd, mask = rt["lg"], rt["m8"], rt["d"], rt["mask"]
                        kb.op("dve", lambda e: e.tensor_copy(out=lg[:], in_=pr[:, 0:NEXP]), [pr], [lg])
                        kb.op("dve", lambda e: e.max(out=m8[:], in_=lg[:]), [lg], [m8])
                        kb.op("dve", lambda e: e.tensor_scalar(out=rs[:, 0:1], in0=m8[:, 0:1], scalar1=-1.0, scalar2=None,
                                                               op0=ALU.mult), [m8], [rs])
                        kb.op("act", lambda e: e.activation(out=dd[:], in_=lg[:], func=AF.Exp, bias=rs[:, 0:1]),
                              [lg, rs], [dd])
                        kb.op("act", lambda e: e.activation(out=rs[:, 1:2], in_=m8[:, 1:2], func=AF.Exp, bias=rs[:, 0:1]),
                              [m8, rs], [rs])
                        kb.op("dve", lambda e: e.tensor_scalar(out=rs[:, 2:3], in0=rs[:, 1:2], scalar1=1.0, scalar2=None,
                                                               op0=ALU.add), [rs], [rs])
                        kb.op("dve", lambda e: e.reciprocal(out=rs[:, 3:4], in_=rs[:, 2:3]), [rs], [rs])
                        kb.op("dve", lambda e: e.tensor_scalar(out=mask[:], in0=lg[:], scalar1=m8[:, 1:2], scalar2=None,
                                                               op0=ALU.is_ge), [lg, m8], [mask])
                        kb.op("dve", lambda e: e.scalar_tensor_tensor(out=gates[:, ti, :], in0=dd[:], scalar=rs[:, 3:4],
                                                                      in1=mask[:], op0=ALU.mult, op1=ALU.mult),
                              [dd, rs, mask], [gates])

                tileA(0)
                for tt in range(4):
                    if tt + 1 < 4:
                        tileA(tt + 1)
                    tileB(tt)
            if c == 0:
                load_unit(0, 0)
            for ui, (ex, fg) in enumerate(units):
                gu = c * len(units) + ui
                slot = gu % 2
                if ui + 1 < len(units):
                    load_unit(ui + 1, (gu + 1) % 2)
                elif c + 1 < NCH:
                    load_unit(0, (gu + 1) % 2)
                g_b, u_b, d_b = wgb[slot], wub[slot], wdb[slot]
                k = 0
                for th in range(CH // 512):
                    for ft in range(4):
                        pg = ps[(k % 2) * 2]
                        pu = ps[(k % 2) * 2 + 1]
                        sgb = sg[k % 2]
                        k += 1
                        for kt in range(8):
                            mm(kb, pg, pg[:, :], g_b, g_b[:, kt, ft * 128:(ft + 1) * 128], a2T,
                               a2T[:, kt, th * 512:(th + 1) * 512], kt == 0, kt == 7)
                        for kt in range(8):
                            mm(kb, pu, pu[:, :], u_b, u_b[:, kt, ft * 128:(ft + 1) * 128], a2T,
                               a2T[:, kt, th * 512:(th + 1) * 512], kt == 0, kt == 7)
                        kb.op("act", lambda e: e.activation(out=sgb[:], in_=pg[:, :], func=AF.Silu), [pg], [sgb])
                        kb.op("dve", lambda e: e.tensor_tensor(out=hT[:, ft, th * 512:(th + 1) * 512], in0=sgb[:],
                                                               in1=pu[:, :], op=ALU.mult), [sgb, pu], [hT])
                k = 0
                for ti in range(CH // 128):
                    for cc in range(2):
                        pd = ps[4 + k % 4]
                        k += 1
                        for ft in range(4):
                            mm(kb, pd, pd[:, :], hT, hT[:, ft, ti * 128:(ti + 1) * 128], d_b,
                               d_b[:, ft, cc * 512:(cc + 1) * 512], ft == 0, ft == 3)
                        dst = acc[:, ti, cc * 512:(cc + 1) * 512]
                        gsc = gates[:, ti, ex:ex + 1] if moe else 1.0
                        gdeps = [gates] if moe else []
                        if ui == 0:
                            kb.op("dve", lambda e: e.tensor_scalar(out=dst, in0=pd[:, :], scalar1=gsc, scalar2=None,
                                                                   op0=ALU.mult), [pd] + gdeps, [acc])
                        else:
                            kb.op("dve", lambda e: e.scalar_tensor_tensor(out=dst, in0=pd[:, :], scalar=gsc, in1=dst,
                                                                          op0=ALU.mult, op1=ALU.add),
                                  [pd, acc] + gdeps, [acc])
            for ti in range(CH // 128):
                tg = (t0 // 128) + ti
                hb = h1t[ti % 2]
                kb.dma("pool", hb, hb[:], h1s, h1s[tg * 128:(tg + 1) * 128, :])
                kb.op("dve", lambda e: e.tensor_tensor(out=ot[:], in0=acc[:, ti, :], in1=Gffn[:], op=ALU.mult),
                      [acc, Gffn], [ot])
                kb.op("pool", lambda e: e.tensor_tensor(out=ot[:], in0=ot[:], in1=hb[:], op=ALU.add), [ot, hb], [ot])
                kb.dma("sp", hout, hout[tg * 128:(tg + 1) * 128, :], ot, ot[:], anchor=ot)


def build_F(moe, ntok=4096):
    kb = KB()
    x = kb.dram("x", [ntok, D], F32, "ExternalInput")
    yT = kb.dram("yT", [D, ntok], F32, "ExternalInput")
    ccol = kb.dram("ccol", [128, 8], F32, "ExternalInput")
    adaw = kb.dram("adaw", [D, 6 * D], F32, "ExternalInput")
    adab = kb.dram("adab", [1, 6 * D], F32, "ExternalInput")
    gcol = kb.dram("gcol", [128, 8], F32, "ExternalInput")
    wglu = kb.dram("wglu", [256, 256], F32, "ExternalInput")
    wout = kb.dram("wout", [D, D], F32, "ExternalInput")
    NE = NEXP if moe else 1
    wg = kb.dram("wg", [NE, D, DFF], F32, "ExternalInput")
    wu = kb.dram("wu", [NE, D, DFF], F32, "ExternalInput")
    wd = kb.dram("wd", [NE, DFF, D], F32, "ExternalInput")
    router = kb.dram("router", [D, NEXP], F32, "ExternalInput") if moe else None
    hout = kb.dram("hout", [ntok, D], F32, "ExternalOutput")
    h1s = kb.dram("h1s", [ntok, D], F32, "Internal")
    ps = [kb.ps(f"P{i}") for i in range(8)]
    ident = make_ident(kb)
    emit_F(kb, ps, ident, moe, ntok, x, yT, ccol, adaw, adab, gcol, wglu, wout, wg, wu, wd, router, hout, h1s)
    kb.finish([hout])
    return kb


TWO_PI = 2.0 * math.pi


def emit_tok_store(kb, ps, ident, y, n, t0, ytok, stg):
    for q in range(n // 512):
        pb = ps[6 + q % 2]
        for k in range(4):
            blk = q * 4 + k
            kb.op("pe", lambda e: e.transpose(out=pb[:, k * 128:(k + 1) * 128], in_=y[:, blk * 128:(blk + 1) * 128],
                                              identity=ident[:]), [y, ident], [pb])
        sg_ = stg[q % 2]
        kb.op("act", lambda e: e.copy(sg_[:].rearrange("p i e -> p (i e)"), pb[:, :]), [pb], [sg_])
        r0 = t0 + q * 512
        kb.dma("sp", ytok, ytok[r0:r0 + 512, :].rearrange("(i p) e -> p i e", p=128), sg_, sg_[:], anchor=sg_)


def emit_conv(kb, ps, conv_in, cw, yconvT, ident=None, ytok=None):
    CH = 2048
    with kb.phase() as ph:
        cwt = ph.sb([128, 3], F32, "cwt")
        kb.dma("sp", cwt, cwt[:], cw, cw[:])
        gbt = [ph.sb([128, CH], F32, "gbt") for _ in range(2)]
        gct = [ph.sb([128, CH], F32, "gct") for _ in range(2)]
        ut = [ph.sb([128, CH], F32, "ut") for _ in range(2)]
        vb = [ph.sb([128, CH + 2], F32, "vb") for _ in range(2)]
        yb = [ph.sb([128, CH], F32, "yb") for _ in range(2)]
        kb.op("pool", lambda e: e.memset(vb[1][:, CH:CH + 2], 0.0), [], [vb[1]])
        stg = [ph.sb([128, 4, 128], F32, "stg") for _ in range(2)] if ytok is not None else None
        for c in range(T // CH):
            k = c % 2
            sl = slice(c * CH, (c + 1) * CH)
            kb.dma("act", gbt[k], gbt[k][:], conv_in, conv_in[0, :, sl])
            kb.dma("act", gct[k], gct[k][:], conv_in, conv_in[1, :, sl])
            kb.dma("act", ut[k], ut[k][:], conv_in, conv_in[2, :, sl])
            v, vp, y = vb[k], vb[1 - k], yb[k]
            kb.op("pool", lambda e: e.tensor_copy(out=v[:, 0:2], in_=vp[:, CH:CH + 2]), [vp], [v])
            kb.op("pool", lambda e: e.tensor_tensor(out=v[:, 2:CH + 2], in0=gct[k][:], in1=ut[k][:], op=ALU.mult),
                  [gct[k], ut[k]], [v])
            kb.op("dve", lambda e: e.tensor_scalar(out=y[:], in0=v[:, 2:CH + 2], scalar1=cwt[:, 2:3], scalar2=None,
                                                   op0=ALU.mult), [v, cwt], [y])
            kb.op("dve", lambda e: e.scalar_tensor_tensor(out=y[:], in0=v[:, 1:CH + 1], scalar=cwt[:, 1:2], in1=y[:],
                                                          op0=ALU.mult, op1=ALU.add), [v, cwt, y], [y])
            kb.op("dve", lambda e: e.scalar_tensor_tensor(out=y[:], in0=v[:, 0:CH], scalar=cwt[:, 0:1], in1=y[:],
                                                          op0=ALU.mult, op1=ALU.add), [v, cwt, y], [y])
            kb.op("pool", lambda e: e.tensor_tensor(out=y[:], in0=y[:], in1=gbt[k][:], op=ALU.mult), [y, gbt[k]], [y])
            if ytok is None:
                kb.dma("sp", yconvT, yconvT[:, sl], y, y[:], anchor=y)
            else:
                emit_tok_store(kb, ps, ident, y, CH, c * CH, ytok, stg)


def emit_ssm(kb, ps, ident, uT, sp_lam, sp_ldt, sp_b, sp_c, sp_d, yssmT, ytok=None):
    L = 512
    NS = 4
    NL = 10
    with kb.phase() as ph:
        lam = ph.sb([128, NS, 2], F32, "lam")
        ldt = ph.sb([128, NS], F32, "ldt")
        bb = ph.sb([128, NS, 2, 128], F32, "bb")
        cc = ph.sb([128, NS, 2, 128], F32, "cc")
        dsk = ph.sb([128, 1], F32, "dsk")
        kb.dma("sp", lam, lam[:], sp_lam, sp_lam[:])
        kb.dma("sp", ldt, ldt[:], sp_ldt, sp_ldt[:])
        kb.dma("sp", bb, bb[:], sp_b, sp_b[:])
        kb.dma("sp", cc, cc[:], sp_c, sp_c[:])
        kb.dma("sp", dsk, dsk[:], sp_d, sp_d[:])
        S = {k: ph.sb([128, NS], F32, "s_" + k) for k in
             ("lr", "li", "dt", "a", "th", "r", "m", "ab", "nr", "ni", "l2", "inv", "kr", "ki", "t1", "t2", "nki")}
        Pc = ph.sb([128, NL, NS], F32, "Pc")
        Ps = ph.sb([128, NL, NS], F32, "Ps")

        def V(e, fn, reads, writes):
            kb.op(e, fn, reads, writes)

        lr, li, dt, a, th, r = S["lr"], S["li"], S["dt"], S["a"], S["th"], S["r"]
        V("dve", lambda e: e.tensor_scalar(out=lr[:], in0=lam[:, :, 0], scalar1=-1e-4, scalar2=None, op0=ALU.min),
          [lam], [lr])
        V("dve", lambda e: e.tensor_copy(out=li[:], in_=lam[:, :, 1]), [lam], [li])
        V("act", lambda e: e.activation(out=dt[:], in_=ldt[:], func=AF.Exp), [ldt], [dt])
        V("dve", lambda e: e.tensor_tensor(out=a[:], in0=lr[:], in1=dt[:], op=ALU.mult), [lr, dt], [a])
        V("dve", lambda e: e.tensor_tensor(out=th[:], in0=li[:], in1=dt[:], op=ALU.mult), [li, dt], [th])
        V("act", lambda e: e.activation(out=r[:], in_=a[:], func=AF.Exp), [a], [r])
        m = S["m"]
        for _ in range(8):
            V("dve", lambda e: e.tensor_scalar(out=m[:], in0=th[:], scalar1=math.pi, scalar2=None, op0=ALU.is_gt), [th], [m])
            V("dve", lambda e: e.scalar_tensor_tensor(out=th[:], in0=m[:], scalar=-TWO_PI, in1=th[:], op0=ALU.mult,
                                                      op1=ALU.add), [th, m], [th])
        ab = S["ab"]
        for _ in range(2):
            V("dve", lambda e: e.tensor_scalar(out=ab[:], in0=th[:], scalar1=-1.0, scalar2=None, op0=ALU.mult), [th], [ab])
            V("dve", lambda e: e.tensor_scalar(out=m[:], in0=ab[:], scalar1=math.pi, scalar2=None, op0=ALU.is_gt), [ab], [m])
            V("dve", lambda e: e.scalar_tensor_tensor(out=th[:], in0=m[:], scalar=TWO_PI, in1=th[:], op0=ALU.mult,
                                                      op1=ALU.add), [th, m], [th])
        halfpi = ph.sb([128, 1], F32, "halfpi")
        V("dve", lambda e: e.memset(halfpi[:], math.pi / 2), [], [halfpi])
        V("dve", lambda e: e.tensor_scalar(out=ab[:], in0=th[:], scalar1=-1.0, scalar2=None, op0=ALU.mult), [th], [ab])
        V("dve", lambda e: e.tensor_tensor(out=ab[:], in0=ab[:], in1=th[:], op=ALU.max), [ab, th], [ab])
        V("act", lambda e: e.activation(out=Pc[:, 0, :], in_=ab[:], func=AF.Sin, scale=-1.0, bias=halfpi[:, 0:1]),
          [ab, halfpi], [Pc])
        V("act", lambda e: e.activation(out=Ps[:, 0, :], in_=th[:], func=AF.Sin), [th], [Ps])
        t1, t2 = S["t1"], S["t2"]
        for lv in range(1, NL):
            V("dve", lambda e: e.tensor_tensor(out=t1[:], in0=Pc[:, lv - 1, :], in1=Pc[:, lv - 1, :], op=ALU.mult), [Pc], [t1])
            V("dve", lambda e: e.tensor_tensor(out=t2[:], in0=Ps[:, lv - 1, :], in1=Ps[:, lv - 1, :], op=ALU.mult), [Ps], [t2])
            V("dve", lambda e: e.tensor_tensor(out=Pc[:, lv, :], in0=t1[:], in1=t2[:], op=ALU.subtract), [t1, t2], [Pc])
            V("dve", lambda e: e.tensor_tensor(out=t1[:], in0=Pc[:, lv - 1, :], in1=Ps[:, lv - 1, :], op=ALU.mult), [Pc, Ps], [t1])
            V("dve", lambda e: e.tensor_scalar(out=Ps[:, lv, :], in0=t1[:], scalar1=2.0, scalar2=None, op0=ALU.mult), [t1], [Ps])
        nr, ni, l2, inv, kr, ki, nki = S["nr"], S["ni"], S["l2"], S["inv"], S["kr"], S["ki"], S["nki"]
        V("dve", lambda e: e.tensor_tensor(out=nr[:], in0=r[:], in1=Pc[:, 0, :], op=ALU.mult), [r, Pc], [nr])
        V("dve", lambda e: e.tensor_scalar(out=nr[:], in0=nr[:], scalar1=-1.0, scalar2=None, op0=ALU.add), [nr], [nr])
        V("dve", lambda e: e.tensor_tensor(out=ni[:], in0=r[:], in1=Ps[:, 0, :], op=ALU.mult), [r, Ps], [ni])
        V("dve", lambda e: e.tensor_tensor(out=t1[:], in0=lr[:], in1=lr[:], op=ALU.mult), [lr], [t1])
        V("dve", lambda e: e.tensor_tensor(out=t2[:], in0=li[:], in1=li[:], op=ALU.mult), [li], [t2])
        V("dve", lambda e: e.tensor_tensor(out=l2[:], in0=t1[:], in1=t2[:], op=ALU.add), [t1, t2], [l2])
        V("dve", lambda e: e.reciprocal(out=inv[:], in_=l2[:]), [l2], [inv])
        V("dve", lambda e: e.tensor_tensor(out=t1[:], in0=nr[:], in1=lr[:], op=ALU.mult), [nr, lr], [t1])
        V("dve", lambda e: e.tensor_tensor(out=t2[:], in0=ni[:], in1=li[:], op=ALU.mult), [ni, li], [t2])
        V("dve", lambda e: e.tensor_tensor(out=kr[:], in0=t1[:], in1=t2[:], op=ALU.add), [t1, t2], [kr])
        V("dve", lambda e: e.tensor_tensor(out=kr[:], in0=kr[:], in1=inv[:], op=ALU.mult), [kr, inv], [kr])
        V("dve", lambda e: e.tensor_tensor(out=t1[:], in0=ni[:], in1=lr[:], op=ALU.mult), [ni, lr], [t1])
        V("dve", lambda e: e.tensor_tensor(out=t2[:], in0=nr[:], in1=li[:], op=ALU.mult), [nr, li], [t2])
        V("dve", lambda e: e.tensor_tensor(out=ki[:], in0=t1[:], in1=t2[:], op=ALU.subtract), [t1, t2], [ki])
        V("dve", lambda e: e.tensor_tensor(out=ki[:], in0=ki[:], in1=inv[:], op=ALU.mult), [ki, inv], [ki])
        Bre = ph.sb([128, NS, 128], F32, "Bre")
        Bim = ph.sb([128, NS, 128], F32, "Bim")
        tb = ph.sb([128, 128], F32, "tb")
        BT = ph.sb([128, NS, 2, 128], F32, "BT")
        nCim = ph.sb([128, NS, 128], F32, "nCim")
        for s in range(NS):
            V("dve", lambda e: e.tensor_scalar(out=tb[:], in0=bb[:, s, 1, :], scalar1=ki[:, s:s + 1], scalar2=None,
                                               op0=ALU.mult), [bb, ki], [tb])
            V("dve", lambda e: e.scalar_tensor_tensor(out=Bre[:, s, :], in0=bb[:, s, 0, :], scalar=kr[:, s:s + 1],
                                                      in1=tb[:], op0=ALU.mult, op1=ALU.subtract), [bb, kr, tb], [Bre])
            V("dve", lambda e: e.tensor_scalar(out=tb[:], in0=bb[:, s, 0, :], scalar1=ki[:, s:s + 1], scalar2=None,
                                               op0=ALU.mult), [bb, ki], [tb])
            V("dve", lambda e: e.scalar_tensor_tensor(out=Bim[:, s, :], in0=bb[:, s, 1, :], scalar=kr[:, s:s + 1],
                                                      in1=tb[:], op0=ALU.mult, op1=ALU.add), [bb, kr, tb], [Bim])
            for ri, Bsrc in enumerate((Bre, Bim)):
                pb = ps[ri]
                V("pe", lambda e: e.transpose(out=pb[:, 0:128], in_=Bsrc[:, s, :], identity=ident[:]), [Bsrc, ident], [pb])
                V("dve", lambda e: e.tensor_copy(out=BT[:, s, ri, :], in_=pb[:, 0:128]), [pb], [BT])
            V("dve", lambda e: e.tensor_scalar(out=nCim[:, s, :], in0=cc[:, s, 1, :], scalar1=-1.0, scalar2=None,
                                               op0=ALU.mult), [cc], [nCim])
        cosT = ph.sb([128, NS, L], F32, "cosT")
        sinT = ph.sb([128, NS, L], F32, "sinT")
        tt = ph.sb([128, L // 2], F32, "tt")
        V("dve", lambda e: e.memset(cosT[:, :, 0:1], 1.0), [], [cosT])
        V("dve", lambda e: e.memset(sinT[:, :, 0:1], 0.0), [], [sinT])
        for s in range(NS):
            for lv in range(NL - 1):
                mlen = 1 << lv
                cm = Pc[:, lv, s:s + 1]
                sm = Ps[:, lv, s:s + 1]
                V("pool", lambda e: e.tensor_scalar(out=tt[:, 0:mlen], in0=sinT[:, s, 0:mlen], scalar1=sm, scalar2=None,
                                                    op0=ALU.mult), [sinT, Ps], [tt])
                V("dve", lambda e: e.scalar_tensor_tensor(out=cosT[:, s, mlen:2 * mlen], in0=cosT[:, s, 0:mlen], scalar=cm,
                                                          in1=tt[:, 0:mlen], op0=ALU.mult, op1=ALU.subtract),
                  [cosT, Pc, tt], [cosT])
                V("pool", lambda e: e.tensor_scalar(out=tt[:, 0:mlen], in0=cosT[:, s, 0:mlen], scalar1=sm, scalar2=None,
                                                    op0=ALU.mult), [cosT, Ps], [tt])
                V("dve", lambda e: e.scalar_tensor_tensor(out=sinT[:, s, mlen:2 * mlen], in0=sinT[:, s, 0:mlen], scalar=cm,
                                                          in1=tt[:, 0:mlen], op0=ALU.mult, op1=ALU.add),
                  [sinT, Pc, tt], [sinT])
        uc = [ph.sb([128, L], F32, "uc") for _ in range(3)]
        gre = [[ph.sb([128, L], F32, "gre") for _ in range(2)] for _ in range(NS)]
        gim = [[ph.sb([128, L], F32, "gim") for _ in range(2)] for _ in range(NS)]
        NBF = 2
        m1 = [ph.sb([128, L], F32, "m1") for _ in range(NBF)]
        m2 = [ph.sb([128, L], F32, "m2") for _ in range(NBF)]
        m3 = [ph.sb([128, L], F32, "m3") for _ in range(NBF)]
        m4 = [ph.sb([128, L], F32, "m4") for _ in range(NBF)]
        xre = [ph.sb([128, L], F32, "xre") for _ in range(NBF)]
        xim = [ph.sb([128, L], F32, "xim") for _ in range(NBF)]
        q1 = ph.sb([128, L], F32, "q1")
        q2 = ph.sb([128, L], F32, "q2")
        hre = [ph.sb([128, L], F32, "hre") for _ in range(2)]
        him = [ph.sb([128, L], F32, "him") for _ in range(2)]
        ini = [ph.sb([128, 4], F32, "ini") for _ in range(2)]
        yo = [ph.sb([128, L], F32, "yo") for _ in range(2)]
        EL = NL - 1
        NCH = T // L
        its = [(c, s_) for c in range(NCH) for s_ in range(NS)]

        def stA(j):
            c, s_ = its[j]
            u = uc[c % 3]
            if s_ == 0:
                kb.dma("pool", u, u[:], uT, uT[:, c * L:(c + 1) * L])
            pa, pb = ps[(j % 2) * 2], ps[(j % 2) * 2 + 1]
            mm(kb, pa, pa[:, :], BT, BT[:, s_, 0, :], u, u[:], True, True)
            mm(kb, pb, pb[:, :], BT, BT[:, s_, 1, :], u, u[:], True, True)

        def stB(j):
            c, s_ = its[j]
            k = j % NBF
            pa, pb = ps[(j % 2) * 2], ps[(j % 2) * 2 + 1]
            cs_, sn_ = cosT[:, s_, :], sinT[:, s_, :]
            V("dve", lambda e: e.tensor_tensor(out=m1[k][:], in0=pa[:, :], in1=cs_, op=ALU.mult), [pa, cosT], [m1[k]])
            V("dve", lambda e: e.tensor_tensor(out=m4[k][:], in0=pa[:, :], in1=sn_, op=ALU.mult), [pa, sinT], [m4[k]])
            V("dve", lambda e: e.tensor_tensor(out=m2[k][:], in0=pb[:, :], in1=sn_, op=ALU.mult), [pb, sinT], [m2[k]])
            V("dve", lambda e: e.tensor_tensor(out=m3[k][:], in0=pb[:, :], in1=cs_, op=ALU.mult), [pb, cosT], [m3[k]])
            V("pool", lambda e: e.tensor_tensor(out=xre[k][:], in0=m1[k][:], in1=m2[k][:], op=ALU.add), [m1[k], m2[k]], [xre[k]])
            V("pool", lambda e: e.tensor_tensor(out=xim[k][:], in0=m3[k][:], in1=m4[k][:], op=ALU.subtract),
              [m3[k], m4[k]], [xim[k]])

        def stC(j):
            c, s_ = its[j]
            k = j % NBF
            gr, gi = gre[s_][c % 2], gim[s_][c % 2]
            if c == 0:
                i_re, i_im, ideps = 0.0, 0.0, []
            else:
                gpr, gpi = gre[s_][1 - c % 2], gim[s_][1 - c % 2]
                elc, els = Pc[:, EL, s_:s_ + 1], Ps[:, EL, s_:s_ + 1]
                ii = ini[j % 2]
                V("dve", lambda e: e.tensor_scalar(out=ii[:, 2:3], in0=gpi[:, L - 1:L], scalar1=els, scalar2=None,
                                                   op0=ALU.mult), [gpi, Ps], [ii])
                V("dve", lambda e: e.scalar_tensor_tensor(out=ii[:, 0:1], in0=gpr[:, L - 1:L], scalar=elc,
                                                          in1=ii[:, 2:3], op0=ALU.mult, op1=ALU.subtract),
                  [gpr, Pc, ii], [ii])
                V("dve", lambda e: e.tensor_scalar(out=ii[:, 3:4], in0=gpr[:, L - 1:L], scalar1=els, scalar2=None,
                                                   op0=ALU.mult), [gpr, Ps], [ii])
                V("dve", lambda e: e.scalar_tensor_tensor(out=ii[:, 1:2], in0=gpi[:, L - 1:L], scalar=elc,
                                                          in1=ii[:, 3:4], op0=ALU.mult, op1=ALU.add),
                  [gpi, Pc, ii], [ii])
                i_re, i_im, ideps = ii[:, 0:1], ii[:, 1:2], [ii]
            rb = r[:, s_:s_ + 1].to_broadcast([128, L])
            V("dve", lambda e: e.tensor_tensor_scan(out=gr[:], data0=rb, data1=xre[k][:], initial=i_re, op0=ALU.mult,
                                                    op1=ALU.add), [r, xre[k]] + ideps, [gr])
            V("dve", lambda e: e.tensor_tensor_scan(out=gi[:], data0=rb, data1=xim[k][:], initial=i_im, op0=ALU.mult,
                                                    op1=ALU.add), [r, xim[k]] + ideps, [gi])

        def stD(j):
            c, s_ = its[j]
            gr, gi = gre[s_][c % 2], gim[s_][c % 2]
            cs_, sn_ = cosT[:, s_, :], sinT[:, s_, :]
            hr, hi = hre[j % 2], him[j % 2]
            V("pool", lambda e: e.tensor_tensor(out=q1[:], in0=gr[:], in1=cs_, op=ALU.mult), [gr, cosT], [q1])
            V("pool", lambda e: e.tensor_tensor(out=q2[:], in0=gi[:], in1=sn_, op=ALU.mult), [gi, sinT], [q2])
            V("pool", lambda e: e.tensor_tensor(out=hr[:], in0=q1[:], in1=q2[:], op=ALU.subtract), [q1, q2], [hr])
            V("pool", lambda e: e.tensor_tensor(out=q1[:], in0=gr[:], in1=sn_, op=ALU.mult), [gr, sinT], [q1])
            V("pool", lambda e: e.tensor_tensor(out=q2[:], in0=gi[:], in1=cs_, op=ALU.mult), [gi, cosT], [q2])
            V("pool", lambda e: e.tensor_tensor(out=hi[:], in0=q1[:], in1=q2[:], op=ALU.add), [q1, q2], [hi])

        def stE(j):
            c, s_ = its[j]
            hr, hi = hre[j % 2], him[j % 2]
            py = ps[4 + c % 2]
            mm(kb, py, py[:, :], cc, cc[:, s_, 0, :], hr, hr[:], s_ == 0, False)
            mm(kb, py, py[:, :], nCim, nCim[:, s_, :], hi, hi[:], False, s_ == NS - 1)
            if s_ == NS - 1:
                u = uc[c % 3]
                y = yo[c % 2]
                V("dve", lambda e: e.scalar_tensor_tensor(out=y[:], in0=u[:], scalar=dsk[:, 0:1], in1=py[:, :],
                                                          op0=ALU.mult, op1=ALU.add), [u, dsk, py], [y])
                if ytok is None:
                    kb.dma("sp", yssmT, yssmT[:, c * L:(c + 1) * L], y, y[:], anchor=y)
                else:
                    emit_tok_store(kb, ps, ident, y, L, c * L, ytok, stg)

        stg = [ph.sb([128, 4, 128], F32, "stg") for _ in range(2)] if ytok is not None else None
        stages = (stA, stB, stC, stD, stE)
        n = len(its)
        for step in range(n + len(stages) - 1):
            for si, st in enumerate(stages):
                j = step - si
                if 0 <= j < n:
                    st(j)


def emit_sb(kb, ps, ident, qT, kT, v, ysbT, ytok=None):
    NKT = T // 128
    with kb.phase() as ph:
        tri = ph.sb([128, 128], BF16, "tri")
        onesm = ph.sb([128, 128], BF16, "onesm")
        kb.op("pool", lambda e: e.memset(onesm[:], 1.0), [], [onesm])
        kb.op("pool", lambda e: e.memset(tri[:], 1.0), [], [tri])
        kb.op("pool", lambda e: e.affine_select(out=tri[:], in_=tri[:], pattern=[[-1, 128]], compare_op=ALU.is_ge,
                                                fill=0.0, base=0, channel_multiplier=1), [tri], [tri])
        qb = ph.sb([128, T], BF16, "qb")
        kbf = ph.sb([128, T], BF16, "kbf")
        kb.op("pool", lambda e: e.memset(qb[64:128, :], 0.0), [], [qb])
        kb.op("pool", lambda e: e.memset(kbf[64:128, :], 0.0), [], [kbf])
        vb = ph.sb([128, NKT, 64], BF16, "vb")
        NB3 = 4
        Eb = [ph.sb([128, 512], F32, "Eb") for _ in range(NB3)]
        SPb = [ph.sb([128, 512], BF16, "SPb") for _ in range(NB3)]
        Ctb = [ph.sb([128, 512], F32, "Ct") for _ in range(2)]
        Xbb = [ph.sb([128, 512], F32, "Xb") for _ in range(2)]
        Wb = [ph.sb([128, 512], BF16, "Wb") for _ in range(3)]
        Racc = ph.sb([128, 512], F32, "Racc")
        ot = [ph.sb([128, 256], F32, "ot") for _ in range(2)]
        otT = [ph.sb([64, 512], F32, "otT") for _ in range(2)]
        psA, psB, psC, psO = ps[0:2], ps[2:4], ps[4:5], ps[6:8]
        psT = ps[5]
        for h in range(2):
            for q4 in range(4):
                sl = slice(q4 * 2048, (q4 + 1) * 2048)
                kb.dma("pool", qb, qb[0:64, sl], qT, qT[h, :, sl])
                kb.dma("pool", kbf, kbf[0:64, sl], kT, kT[h, :, sl])
            kb.dma("pool", vb, vb[:], v, v.t.rearrange("(kt p) e -> p kt e", p=128)[:, :, h * 64:(h + 1) * 64])
            its = []
            for c in range(T // 512):
                nk = 4 * c + 4
                for ki in range(nk):
                    kt = nk - 1 - ki
                    its.append((c, kt, ki == 0, kt == 0, kt >= 4 * c))

            def stA(i):
                c, kt, first, last, diag = its[i]
                pa, E, SP = psA[i % 2], Eb[i % NB3], SPb[i % NB3]
                mm(kb, pa, pa[:, :], kbf, kbf[:, kt * 128:(kt + 1) * 128], qb, qb[:, c * 512:(c + 1) * 512], True, True)
                kb.op("act", lambda e: e.activation(out=E[:], in_=pa[:, :], func=AF.Exp, scale=0.125), [pa], [E])
                kb.op("act", lambda e: e.activation(out=SP[:], in_=E[:], func=AF.Ln, bias=1.0), [E], [SP])
                if diag:
                    kb.op("pool", lambda e: e.affine_select(out=SP[:], in_=SP[:], pattern=[[1, 512]],
                                                            compare_op=ALU.is_gt, fill=0.0, base=512 * c - 128 * kt,
                                                            channel_multiplier=-1), [SP], [SP])

            def stB1(i):
                c, kt, first, last, diag = its[i]
                pb, pc, SP, Ct = psB[i % 2], psC[0], SPb[i % NB3], Ctb[i % 2]
                mm(kb, pb, pb[:, :], tri, tri[:], SP, SP[:], True, True)
                if not last:
                    mm(kb, pc, pc[:, :], onesm, onesm[:], SP, SP[:], True, True)
                if first:
                    kb.op("dve", lambda e: e.tensor_copy(out=Ct[:], in_=pb[:, :]), [pb], [Ct])
                else:
                    kb.op("dve", lambda e: e.tensor_tensor(out=Ct[:], in0=pb[:, :], in1=Racc[:], op=ALU.add),
                          [pb, Racc], [Ct])
                if not last:
                    if first:
                        kb.op("dve", lambda e: e.tensor_copy(out=Racc[:], in_=pc[:, :]), [pc], [Racc])
                    else:
                        kb.op("dve", lambda e: e.tensor_tensor(out=Racc[:], in0=pc[:, :], in1=Racc[:], op=ALU.add),
                              [pc, Racc], [Racc])

            def stB2(i):
                c, kt, first, last, diag = its[i]
                E, Ct, W, Xb = Eb[i % NB3], Ctb[i % 2], Wb[i % 3], Xbb[i % 2]
                kb.op("act", lambda e: e.activation(out=Xb[:], in_=Ct[:], func=AF.Exp, scale=-1.0), [Ct], [Xb])
                kb.op("dve", lambda e: e.tensor_tensor(out=W[:], in0=E[:], in1=Xb[:], op=ALU.mult), [E, Xb], [W])
                if diag:
                    kb.op("pool", lambda e: e.affine_select(out=W[:], in_=W[:], pattern=[[1, 512]],
                                                            compare_op=ALU.is_gt, fill=0.0, base=512 * c - 128 * kt,
                                                            channel_multiplier=-1), [W], [W])

            def stC(i):
                c, kt, first, last, diag = its[i]
                W, po = Wb[i % 3], psO[c % 2]
                for sub in range(4):
                    mm(kb, po, po[:, sub * 64:(sub + 1) * 64], W, W[:, sub * 128:(sub + 1) * 128], vb, vb[:, kt, :],
                       first and sub == 0, last, skip=True)
                if last:
                    o = ot[c % 2]
                    kb.op("act", lambda e: e.copy(o[:], po[:, 0:256]), [po], [o])
                    if ytok is not None:
                        kb.dma("sp", ytok, ytok[c * 512:(c + 1) * 512, h * 64:(h + 1) * 64].rearrange("(s p) d -> p s d", p=128),
                               o, o[:].rearrange("p (s d) -> p s d", s=4), anchor=o)
                        return
                    for sub in range(4):
                        kb.op("pe", lambda e: e.transpose(out=psT[0:64, sub * 128:(sub + 1) * 128],
                                                          in_=o[:, sub * 64:(sub + 1) * 64], identity=ident[:]),
                              [o, ident], [psT])
                    oT = otT[c % 2]
                    kb.op("dve", lambda e: e.tensor_copy(out=oT[:], in_=psT[0:64, :]), [psT], [oT])
                    kb.dma("sp", ysbT, ysbT[h * 64:(h + 1) * 64, c * 512:(c + 1) * 512], oT, oT[:], anchor=oT)

            stages = (stA, stB1, stB2, stC)
            n = len(its)
            for step in range(n + len(stages) - 1):
                for si, st in enumerate(stages):
                    i = step - si
                    if 0 <= i < n:
                        st(i)


def build_M(parts=("conv", "ssm", "sb", "nsa"), nsa_stage=9, nsa_dbg=False):
    kb = KB()
    ps = [kb.ps(f"P{i}") for i in range(8)]
    ident = make_ident(kb)
    outs = []
    if "conv" in parts:
        conv_in = kb.dram("conv_in", [3, 128, T], F32, "ExternalInput")
        cw = kb.dram("cw", [128, 3], F32, "ExternalInput")
        yconvT = kb.dram("yconvT", [128, T], F32, "ExternalOutput")
        emit_conv(kb, ps, conv_in, cw, yconvT)
        outs.append(yconvT)
    if "ssm" in parts:
        uT = kb.dram("ssm_uT", [128, T], F32, "ExternalInput")
        sp_lam = kb.dram("sp_lam", [128, 4, 2], F32, "ExternalInput")
        sp_ldt = kb.dram("sp_ldt", [128, 4], F32, "ExternalInput")
        sp_b = kb.dram("sp_b", [128, 4, 2, 128], F32, "ExternalInput")
        sp_c = kb.dram("sp_c", [128, 4, 2, 128], F32, "ExternalInput")
        sp_d = kb.dram("sp_d", [128, 1], F32, "ExternalInput")
        yssmT = kb.dram("yssmT", [128, T], F32, "ExternalOutput")
        emit_ssm(kb, ps, ident, uT, sp_lam, sp_ldt, sp_b, sp_c, sp_d, yssmT)
        outs.append(yssmT)
    if "sb" in parts:
        qT = kb.dram("sb_qT", [2, 64, T], F32, "ExternalInput")
        kT = kb.dram("sb_kT", [2, 64, T], F32, "ExternalInput")
        v = kb.dram("sb_v", [T, 128], F32, "ExternalInput")
        ysb = kb.dram("ysb", [128, T], F32, "ExternalOutput")
        emit_sb(kb, ps, ident, qT, kT, v, ysb)
        outs.append(ysb)
    if "nsa" in parts:
        I = {}
        for name, shape, dt in (("q", [T, 128], F32), ("kcT", [64, T], F32), ("vcT", [64, T], F32), ("ks", [T, 64], F32),
                                ("vs", [T, 64], F32), ("kw", [T, 64], F32), ("vw", [T, 64], F32), ("gl", [T, 6], F32),
                                ("pos", [128, 64], I32), ("pos_cmp", [128, 4], I32), ("invf", [128, 32], F32),
                                ("qg", [128, 64], F32), ("kg", [128, 64], F32), ("pe_k", [64, 32], F32),
                                ("pe_v", [64, 32], F32), ("k_w1", [2048, 256], F32), ("v_w1", [2048, 256], F32),
                                ("k_w2", [256, 64], F32), ("v_w2", [256, 64], F32), ("ov", [128, 4, 128], F32),
                                ("e128", [128, 64, 128], F32)):
            I[name] = kb.dram("nsa_" + name, shape, dt, "ExternalInput")
        I["qn"] = kb.dram("nsa_qn", [T, 128], F32, "Internal")
        ynsa = kb.dram("ynsa", [128, T], F32, "ExternalOutput")
        dbg = None
        if nsa_dbg:
            dbg = {"ksT": kb.dram("dbg_ksT", [64, T], F32, "ExternalOutput"),
                   "kwT": kb.dram("dbg_kwT", [64, T], F32, "ExternalOutput"),
                   "cosA": kb.dram("dbg_cosA", [128, 64, 32], F32, "ExternalOutput"),
                   "sinA": kb.dram("dbg_sinA", [128, 64, 32], F32, "ExternalOutput"),
                   "kcT": kb.dram("dbg_kcT", [64, 512], F32, "ExternalOutput"),
                   "rhsc": kb.dram("dbg_rhsc", [128, 4, 194], F32, "ExternalOutput")}
        emit_nsa(kb, ps, ident, I, ynsa, stage=nsa_stage, dbg=dbg)
        outs.append(ynsa)
    kb.finish(outs)
    return kb


def nsa_consts():
    n = np.arange(512)[:, None]
    sblk = np.arange(128)[None, :]
    ov = ((16 * n < 64 * sblk + 64) & (16 * n + 32 > 64 * sblk) & (n < 511)).astype(np.float32)
    ov = np.ascontiguousarray(ov.reshape(4, 128, 128).transpose(1, 0, 2))
    e128 = np.zeros((128, 64, 128), np.float32)
    for kt in range(64):
        e128[2 * kt, kt, 0:64] = 1.0
        e128[2 * kt + 1, kt, 64:128] = 1.0
    invf = np.power(np.float32(10000.0), -np.arange(32, dtype=np.float32) / np.float32(32)).astype(np.float32)
    invf = np.ascontiguousarray(np.broadcast_to(invf[None, :], (128, 32)))
    return ov, e128, invf


def nsa_layout(inp, l, b, p, z):
    ov, e128, invf = nsa_consts()
    kv0 = 2048
    pos = inp["positions"][b].astype(np.int32)
    pc = np.zeros(512, np.int32)
    pc[:511] = pos[16 * np.arange(511) + 31]
    m = {
        "nsa_q": z[:, 1792 + 128 * p:1792 + 128 * p + 128],
        "nsa_kcT": z[:, kv0 + 0 * 128 + 64 * p:kv0 + 0 * 128 + 64 * p + 64].T,
        "nsa_vcT": z[:, kv0 + 1 * 128 + 64 * p:kv0 + 1 * 128 + 64 * p + 64].T,
        "nsa_ks": z[:, kv0 + 2 * 128 + 64 * p:kv0 + 2 * 128 + 64 * p + 64],
        "nsa_vs": z[:, kv0 + 3 * 128 + 64 * p:kv0 + 3 * 128 + 64 * p + 64],
        "nsa_kw": z[:, kv0 + 4 * 128 + 64 * p:kv0 + 4 * 128 + 64 * p + 64],
        "nsa_vw": z[:, kv0 + 5 * 128 + 64 * p:kv0 + 5 * 128 + 64 * p + 64],
        "nsa_gl": z[:, 2816 + 6 * p:2816 + 6 * p + 6],
        "nsa_pos": pos.reshape(64, 128).T,
        "nsa_pos_cmp": pc.reshape(4, 128).T,
        "nsa_invf": invf,
        "nsa_qg": np.broadcast_to(inp["nsa_q_norm_g"][l][None, :], (128, 64)),
        "nsa_kg": np.broadcast_to(inp["nsa_k_norm_g"][l][None, :], (128, 64)),
        "nsa_pe_k": inp["cmp_pos_k"][l].T,
        "nsa_pe_v": inp["cmp_pos_v"][l].T,
        "nsa_k_w1": inp["cmp_k_w1"][l], "nsa_v_w1": inp["cmp_v_w1"][l],
        "nsa_k_w2": inp["cmp_k_w2"][l], "nsa_v_w2": inp["cmp_v_w2"][l],
        "nsa_ov": ov, "nsa_e128": e128,
    }
    return {k: np.ascontiguousarray(v) for k, v in m.items()}


def ssm_layout(inp, l, p):
    lam = np.zeros((128, 4, 2), np.float32)
    ldt = np.zeros((128, 4), np.float32)
    bb = np.zeros((128, 4, 2, 128), np.float32)
    cc = np.zeros((128, 4, 2, 128), np.float32)
    for s in range(4):
        for gl in range(2):
            g = 8 * p + 2 * s + gl
            rows = slice(gl * 64, (gl + 1) * 64)
            cols = slice((2 * s + gl) * 16, (2 * s + gl + 1) * 16)
            lam[rows, s, 0] = inp["ssm_lam_re"][l, g]
            lam[rows, s, 1] = inp["ssm_lam_im"][l, g]
            ldt[rows, s] = inp["ssm_log_dt"][l, g]
            bb[rows, s, 0, cols] = inp["ssm_b_re"][l, g]
            bb[rows, s, 1, cols] = inp["ssm_b_im"][l, g]
            cc[rows, s, 0, cols] = inp["ssm_c_re"][l, g].T
            cc[rows, s, 1, cols] = inp["ssm_c_im"][l, g].T
    d = np.ascontiguousarray(inp["ssm_d"][l, 128 * p:128 * (p + 1)].reshape(128, 1))
    return {"sp_lam": lam, "sp_ldt": ldt, "sp_b": bb, "sp_c": cc, "sp_d": d}


CW1 = 6.28125
CW2 = TWO_PI - CW1


def emit_rope_tables(kb, ph0, posf_ap, posf_buf, n, invf, cos, sin):
    with kb.phase() as ph:
        ang = ph.sb([128, n, 32], F32, "ang")
        y = ph.sb([128, n, 32], F32, "angy")
        yi = ph.sb([128, n, 32], I32, "angi")
        m = ph.sb([128, n, 32], F32, "angm")
        r = ph.sb([128, n, 32], F32, "angr")
        kb.op("dve", lambda e: e.tensor_tensor(out=ang[:], in0=posf_ap.unsqueeze(2).to_broadcast([128, n, 32]),
                                               in1=invf[:].unsqueeze(1).to_broadcast([128, n, 32]), op=ALU.mult),
              [posf_buf, invf], [ang])
        kb.op("dve", lambda e: e.tensor_scalar(out=y[:], in0=ang[:], scalar1=1.0 / TWO_PI, scalar2=None, op0=ALU.mult),
              [ang], [y])
        kb.op("dve", lambda e: e.tensor_copy(out=yi[:], in_=y[:]), [y], [yi])
        kb.op("dve", lambda e: e.tensor_copy(out=y[:], in_=yi[:]), [yi], [y])
        kb.op("dve", lambda e: e.scalar_tensor_tensor(out=r[:], in0=y[:], scalar=-CW1, in1=ang[:], op0=ALU.mult,
                                                      op1=ALU.add), [y, ang], [r])
        kb.op("dve", lambda e: e.scalar_tensor_tensor(out=r[:], in0=y[:], scalar=-CW2, in1=r[:], op0=ALU.mult,
                                                      op1=ALU.add), [y, r], [r])

        def fix(buf):
            for _ in range(2):
                kb.op("dve", lambda e: e.tensor_scalar(out=m[:], in0=buf[:], scalar1=math.pi, scalar2=None, op0=ALU.is_gt),
                      [buf], [m])
                kb.op("dve", lambda e: e.scalar_tensor_tensor(out=buf[:], in0=m[:], scalar=-TWO_PI, in1=buf[:],
                                                              op0=ALU.mult, op1=ALU.add), [m, buf], [buf])
            for _ in range(2):
                kb.op("dve", lambda e: e.tensor_scalar(out=m[:], in0=buf[:], scalar1=-math.pi, scalar2=None, op0=ALU.is_gt),
                      [buf], [m])
                kb.op("dve", lambda e: e.tensor_scalar(out=m[:], in0=m[:], scalar1=-TWO_PI, scalar2=TWO_PI, op0=ALU.mult,
                                                       op1=ALU.add), [m], [m])
                kb.op("dve", lambda e: e.tensor_tensor(out=buf[:], in0=buf[:], in1=m[:], op=ALU.add), [m, buf], [buf])

        fix(r)
        kb.op("act", lambda e: e.activation(out=sin[:], in_=r[:], func=AF.Sin), [r], [sin])
        kb.op("dve", lambda e: e.tensor_scalar(out=r[:], in0=r[:], scalar1=math.pi / 2, scalar2=None, op0=ALU.add), [r], [r])
        fix(r)
        kb.op("act", lambda e: e.activation(out=cos[:], in_=r[:], func=AF.Sin), [r], [cos])


def emit_normrope(kb, X, xap, n, g, cos, cosap, sin, sinap, Y, yap, tmp, qscale=None):
    sq, ss, xn, t1, t2 = tmp["sq"], tmp["ss"], tmp["xn"], tmp["t1"], tmp["t2"]
    sqv, xnv = sq[:, 0:n, :], xn[:, 0:n, :]
    t1v, t2v = t1[:, 0:n, :], t2[:, 0:n, :]
    kb.op("pool", lambda e: e.tensor_tensor(out=sqv, in0=xap, in1=xap, op=ALU.mult), [X], [sq])
    kb.op("dve", lambda e: e.tensor_reduce(out=ss[:, 0:n], in_=sqv, axis=AX.X, op=ALU.add), [sq], [ss])
    kb.op("dve", lambda e: e.tensor_scalar(out=ss[:, 0:n], in0=ss[:, 0:n], scalar1=1.0 / 64, scalar2=1e-6, op0=ALU.mult,
                                           op1=ALU.add), [ss], [ss])
    kb.op("act", lambda e: e.activation(out=ss[:, 0:n], in_=ss[:, 0:n], func=AF.Sqrt), [ss], [ss])
    kb.op("dve", lambda e: e.reciprocal(out=ss[:, 0:n], in_=ss[:, 0:n]), [ss], [ss])
    if qscale is not None:
        kb.op("dve", lambda e: e.tensor_scalar(out=ss[:, 0:n], in0=ss[:, 0:n], scalar1=qscale, scalar2=None, op0=ALU.mult),
              [ss], [ss])
    kb.op("dve", lambda e: e.tensor_tensor(out=xnv, in0=xap, in1=ss[:, 0:n].unsqueeze(2).to_broadcast([128, n, 64]),
                                           op=ALU.mult), [X, ss], [xn])
    kb.op("pool", lambda e: e.tensor_tensor(out=xnv, in0=xnv, in1=g[:].unsqueeze(1).to_broadcast([128, n, 64]),
                                            op=ALU.mult), [xn, g], [xn])
    x1, x2 = xn[:, 0:n, 0:32], xn[:, 0:n, 32:64]
    kb.op("dve", lambda e: e.tensor_tensor(out=t1v, in0=x1, in1=cosap, op=ALU.mult), [xn, cos], [t1])
    kb.op("pool", lambda e: e.tensor_tensor(out=t2v, in0=x2, in1=sinap, op=ALU.mult), [xn, sin], [t2])
    kb.op("dve", lambda e: e.tensor_tensor(out=yap[:, :, 0:32], in0=t1v, in1=t2v, op=ALU.subtract), [t1, t2], [Y])
    kb.op("pool", lambda e: e.tensor_tensor(out=t1v, in0=x2, in1=cosap, op=ALU.mult), [xn, cos], [t1])
    kb.op("dve", lambda e: e.tensor_tensor(out=t2v, in0=x1, in1=sinap, op=ALU.mult), [xn, sin], [t2])
    kb.op("pool", lambda e: e.tensor_tensor(out=yap[:, :, 32:64], in0=t1v, in1=t2v, op=ALU.add), [t1, t2], [Y])


def emit_nsa(kb, ps, ident, I, ynsa, stage=9, dbg=None, ytok=None):
    NT = T // 128
    with kb.phase() as ph:
        invf = ph.sb([128, 32], F32, "invf")
        qg = ph.sb([128, 64], F32, "qg")
        kg = ph.sb([128, 64], F32, "kg")
        posi = ph.sb([128, NT], I32, "posi")
        posf = ph.sb([128, NT], F32, "posf")
        pci = ph.sb([128, 4], I32, "pci")
        pcf = ph.sb([128, 4], F32, "pcf")
        for dst, src in ((invf, "invf"), (qg, "qg"), (kg, "kg"), (posi, "pos"), (pci, "pos_cmp")):
            kb.dma("sp", dst, dst[:], I[src], I[src][:])
        kb.op("dve", lambda e: e.tensor_copy(out=posf[:], in_=posi[:]), [posi], [posf])
        kb.op("dve", lambda e: e.tensor_copy(out=pcf[:], in_=pci[:]), [pci], [pcf])
        cosA = ph.sb([128, NT, 32], F32, "cosA")
        sinA = ph.sb([128, NT, 32], F32, "sinA")
        cosC = ph.sb([128, 4, 32], F32, "cosC")
        sinC = ph.sb([128, 4, 32], F32, "sinC")
        emit_rope_tables(kb, ph, posf[:], posf, NT, invf, cosA, sinA)
        emit_rope_tables(kb, ph, pcf[:], pcf, 4, invf, cosC, sinC)

        ksT = ph.sb([128, T], BF16, "ksT")
        kwT = ph.sb([128, T], BF16, "kwT")
        kb.op("pool", lambda e: e.memset(ksT[64:128, :], 0.0), [], [ksT])
        kb.op("pool", lambda e: e.memset(kwT[64:128, :], 0.0), [], [kwT])
        vsa = ph.sb([128, NT, 65], BF16, "vsa")
        vwa = ph.sb([128, NT, 65], BF16, "vwa")
        kcT = ph.sb([64, 512], F32, "kcT")
        rhsc = ph.sb([128, 4, 194], F32, "rhsc")
        e128 = ph.sb([128, NT, 128], BF16, "e128")
        for g4 in range(4):
            kb.dma("pool", e128, e128[:, g4 * 16:(g4 + 1) * 16, :], I["e128"], I["e128"][:, g4 * 16:(g4 + 1) * 16, :])
        kb.dma("sp", rhsc, rhsc[:, :, 64:192], I["ov"], I["ov"][:])
        kb.op("dve", lambda e: e.memset(rhsc[:, :, 192:194], 1.0), [], [rhsc])
        for va, src in ((vsa, "vs"), (vwa, "vw")):
            kb.op("pool", lambda e: e.memset(va[:, :, 64:65], 1.0), [], [va])
            kb.dma("pool", va, va[:, :, 0:64], I[src], I[src].t.rearrange("(kt p) d -> p kt d", p=128))
        nr_tmp = {"sq": ph.sb([128, 16, 64], F32, "nr_sq"), "ss": ph.sb([128, 16], F32, "nr_ss"),
                  "xn": ph.sb([128, 16, 64], F32, "nr_xn"), "t1": ph.sb([128, 16, 32], F32, "nr_t1"),
                  "t2": ph.sb([128, 16, 32], F32, "nr_t2")}
        Xk = [ph.sb([128, 16, 64], F32, "Xk") for _ in range(2)]
        Yk = [ph.sb([128, 16, 64], F32, "Yk") for _ in range(2)]
        it = 0
        for dstT, src in ((ksT, "ks"), (kwT, "kw")):
            sv = I[src].t.rearrange("(tt p) d -> p tt d", p=128)
            for st in range(NT // 16):
                X, Y = Xk[it % 2], Yk[it % 2]
                it += 1
                kb.dma("sp", X, X[:], I[src], sv[:, st * 16:(st + 1) * 16, :])
                emit_normrope(kb, X, X[:], 16, kg, cosA, cosA[:, st * 16:(st + 1) * 16, :], sinA,
                              sinA[:, st * 16:(st + 1) * 16, :], Y, Y[:], nr_tmp)
                for g4 in range(4):
                    pb = ps[g4 % 2]
                    for k in range(4):
                        tt = g4 * 4 + k
                        kb.op("pe", lambda e: e.transpose(out=pb[0:64, k * 128:(k + 1) * 128], in_=Y[:, tt, :],
                                                          identity=ident[:]), [Y, ident], [pb])
                    c0 = (st * 16 + g4 * 4) * 128
                    if g4 % 2 == 0:
                        kb.op("act", lambda e: e.copy(dstT[0:64, c0:c0 + 512], pb[0:64, :]), [pb], [dstT])
                    else:
                        kb.op("dve", lambda e: e.tensor_copy(out=dstT[0:64, c0:c0 + 512], in_=pb[0:64, :]), [pb], [dstT])
        qv = I["q"].t.rearrange("(tt p) e -> p tt e", p=128)
        qnv = I["qn"].t.rearrange("(tt p) e -> p tt e", p=128)
        with kb.phase() as phq:
            Xq16 = [phq.sb([128, 16, 128], F32, "Xq16") for _ in range(2)]
            Yq16 = [phq.sb([128, 16, 128], F32, "Yq16") for _ in range(2)]
            for st in range(NT // 16):
                X, Y = Xq16[st % 2], Yq16[st % 2]
                kb.dma("sp", X, X[:], I["q"], qv[:, st * 16:(st + 1) * 16, :])
                for h in range(2):
                    emit_normrope(kb, X, X[:, :, h * 64:(h + 1) * 64], 16, qg, cosA, cosA[:, st * 16:(st + 1) * 16, :],
                                  sinA, sinA[:, st * 16:(st + 1) * 16, :], Y, Y[:, :, h * 64:(h + 1) * 64], nr_tmp,
                                  qscale=0.125)
                kb.dma("sp", I["qn"], qnv[:, st * 16:(st + 1) * 16, :], Y, Y[:], anchor=Y)
        gs_all = ph.sb([128, NT, 6], F32, "gs_all")
        glv = I["gl"].t.rearrange("(tt p) e -> p tt e", p=128)
        for g4 in range(4):
            kb.dma("sp", gs_all, gs_all[:, g4 * 16:(g4 + 1) * 16, :], I["gl"], glv[:, g4 * 16:(g4 + 1) * 16, :])
        kb.op("act", lambda e: e.activation(out=gs_all[:], in_=gs_all[:], func=AF.Sigmoid), [gs_all], [gs_all])
        with kb.phase() as ph2:
            xT = ph2.sb([64, T], F32, "xcT")
            w1 = ph2.sb([64, 32, 256], F32, "cw1")
            w2 = ph2.sb([128, 2, 64], F32, "cw2")
            peT = ph2.sb([64, 32], F32, "peT")
            cb = ph2.sb([128, 2], F32, "cb")
            hidT = ph2.sb([128, 2, 512], F32, "hidT")
            craw = ph2.sb([128, 4, 64], F32, "craw")
            kcn = ph2.sb([128, 4, 64], F32, "kcn")
            kb.op("dve", lambda e: e.memset(hidT[:, :, 511:512], 0.0), [], [hidT])
            for which in ("k", "v"):
                kb.dma("sp", xT, xT[:], I[which + "cT"], I[which + "cT"][:])
                w1v = I[which + "_w1"].t.rearrange("(r d) j -> d r j", d=64)
                for r4 in range(4):
                    kb.dma("sp", w1, w1[:, r4 * 8:(r4 + 1) * 8, :], I[which + "_w1"], w1v[:, r4 * 8:(r4 + 1) * 8, :])
                kb.dma("sp", w2, w2[:], I[which + "_w2"], I[which + "_w2"].t.rearrange("(jt p) d -> p jt d", p=128))
                kb.dma("sp", peT, peT[:], I["pe_" + which], I["pe_" + which][:])
                pcb = ps[2]
                for jt in range(2):
                    for r in range(32):
                        mm(kb, pcb, pcb[:, jt:jt + 1], w1, w1[:, r, jt * 128:(jt + 1) * 128], peT, peT[:, r:r + 1],
                           r == 0, r == 31)
                kb.op("dve", lambda e: e.tensor_copy(out=cb[:], in_=pcb[:, 0:2]), [pcb], [cb])
                xT3 = xT[:].rearrange("d (c s) -> d c s", s=16)
                for jt in range(2):
                    phd = ps[3 + jt]
                    for r in range(32):
                        mm(kb, phd, phd[:, 0:511], w1, w1[:, r, jt * 128:(jt + 1) * 128], xT,
                           xT3[:, (r // 16):(r // 16) + 511, r % 16], r == 0, r == 31)
                    kb.op("act", lambda e: e.activation(out=hidT[:, jt, 0:511], in_=phd[:, 0:511], func=AF.Silu,
                                                        bias=cb[:, jt:jt + 1]), [phd, cb], [hidT])
                pk = ps[5]
                for nt in range(4):
                    for jt in range(2):
                        mm(kb, pk, pk[:, nt * 64:(nt + 1) * 64], hidT, hidT[:, jt, nt * 128:(nt + 1) * 128], w2, w2[:, jt, :],
                           nt == 0 and jt == 0, jt == 1, skip=True)
                if which == "k":
                    kb.op("dve", lambda e: e.tensor_copy(out=craw[:].rearrange("p a d -> p (a d)"), in_=pk[:, 0:256]),
                          [pk], [craw])
                    emit_normrope(kb, craw, craw[:], 4, kg, cosC, cosC[:], sinC, sinC[:], kcn, kcn[:], nr_tmp)
                    pb = ps[0]
                    for nt in range(4):
                        kb.op("pe", lambda e: e.transpose(out=pb[0:64, nt * 128:(nt + 1) * 128], in_=kcn[:, nt, :],
                                                          identity=ident[:]), [kcn, ident], [pb])
                    kb.op("dve", lambda e: e.tensor_copy(out=kcT[:], in_=pb[0:64, :]), [pb], [kcT])
                else:
                    kb.op("dve", lambda e: e.tensor_copy(out=rhsc[:, :, 0:64],
                                                         in_=pk[:, 0:256].rearrange("p (a d) -> p a d", d=64)),
                          [pk], [rhsc])
        NC_ = T // 256
        Xq = [ph.sb([128, 2, 128], F32, "Xq") for _ in range(2)]
        qTf = ph.sb([64, 512], F32, "qTf")
        qTb = [ph.sb([128, 512], BF16, "qTb") for _ in range(2)]
        for q_ in qTb:
            kb.op("pool", lambda e: e.memset(q_[64:128, :], 0.0), [], [q_])
        PcT = [ph.sb([128, 512], F32, "PcT") for _ in range(4)]
        ocs = [ph.sb([128, 4, 64], F32, "ocs") for _ in range(2)]
        imph = ph.sb([128, 4, 128], F32, "imph")
        imp = ph.sb([128, 2, 128], F32, "imp")
        scr = ph.sb([128, 128], F32, "scr")
        m16 = ph.sb([128, 16], F32, "m16")
        self_ = ph.sb([128, 128], F32, "self")
        selT = [ph.sb([128, 2, 256], BF16, "selT") for _ in range(2)]
        rdF = ph.sb([128, 4], F32, "rdF")
        rdB = ph.sb([128, 2, 2, 2], F32, "rdB")
        Pb = [ph.sb([128, 512], BF16, "Pb") for _ in range(4)]
        yt = [ph.sb([128, 2, 128], F32, "yt") for _ in range(2)]
        ytT = [ph.sb([128, 256], F32, "ytT") for _ in range(2)]
        pF0, pF1 = ps[5], ps[6]
        pOs, pOw = ps[7], ps[0]

        def front(c):
            th = []
            X, q_b, sT, oc_ = Xq[c % 2], qTb[c % 2], selT[c % 2], ocs[c % 2]

            th.append(lambda: kb.dma("pool", X, X[:], I["qn"], qnv[:, 2 * c:2 * c + 2, :]))

            def t_qT():
                for h in range(2):
                    for tt in range(2):
                        c0 = h * 256 + tt * 128
                        kb.op("pe", lambda e: e.transpose(out=pF0[0:64, c0:c0 + 128], in_=X[:, tt, h * 64:(h + 1) * 64],
                                                          identity=ident[:]), [X, ident], [pF0])
                kb.op("act", lambda e: e.copy(qTf[:], pF0[0:64, :]), [pF0], [qTf])
                kb.op("dve", lambda e: e.tensor_copy(out=q_b[0:64, :], in_=qTf[:]), [qTf], [q_b])
            th.append(t_qT)
            nts = [nt for nt in range(4) if 2048 * nt + 31 <= 256 * c + 255]

            def t_cmpS(nt):
                mm(kb, pF0, pF0[:, :], kcT, kcT[:, nt * 128:(nt + 1) * 128], qTf, qTf[:], True, True)
                P = PcT[nt]
                kb.op("act", lambda e: e.activation(out=P[:], in_=pF0[:, :], func=AF.Exp), [pF0], [P])
                for hh_ in range(2):
                    Ph = P[:, hh_ * 256:(hh_ + 1) * 256]
                    kb.op("pool", lambda e: e.affine_select(out=Ph, in_=Ph, pattern=[[1, 256]], compare_op=ALU.is_ge,
                                                            fill=0.0, base=256 * c - 31 - 2048 * nt,
                                                            channel_multiplier=-16), [P], [P])
            for nt in nts:
                th.append(lambda nt=nt: t_cmpS(nt))

            def t_cmpO(half):
                for s2 in range(2):
                    sub = 2 * half + s2
                    o0 = s2 * 194
                    for i, nt in enumerate(nts):
                        mm(kb, pF1, pF1[:, o0:o0 + 194], PcT[nt], PcT[nt][:, sub * 128:(sub + 1) * 128], rhsc, rhsc[:, nt, :],
                           s2 == 0 and i == 0, i == len(nts) - 1, skip=True)
                for s2 in range(2):
                    sub = 2 * half + s2
                    o0 = s2 * 194
                    kb.op("dve", lambda e: e.tensor_scalar(out=rdF[:, sub:sub + 1], in0=pF1[:, o0 + 192:o0 + 193],
                                                           scalar1=1e-30, scalar2=None, op0=ALU.max), [pF1], [rdF])
                    kb.op("dve", lambda e: e.reciprocal(out=rdF[:, sub:sub + 1], in_=rdF[:, sub:sub + 1]), [rdF], [rdF])
                    kb.op("dve", lambda e: e.tensor_scalar(out=oc_[:, sub, :], in0=pF1[:, o0:o0 + 64],
                                                           scalar1=rdF[:, sub:sub + 1], scalar2=None, op0=ALU.mult),
                          [pF1, rdF], [oc_])
                    kb.op("dve", lambda e: e.tensor_scalar(out=imph[:, sub, :], in0=pF1[:, o0 + 64:o0 + 192],
                                                           scalar1=rdF[:, sub:sub + 1], scalar2=None, op0=ALU.mult),
                          [pF1, rdF], [imph])
            for half in range(2):
                th.append(lambda half=half: t_cmpO(half))
            th.append(lambda: kb.op("pool", lambda e: e.tensor_tensor(out=imp[:], in0=imph[:, 0:2, :], in1=imph[:, 2:4, :],
                                                                      op=ALU.add), [imph], [imp]))

            def t_edit(tt):
                for hf in range(2):
                    blk = 4 * c + 2 * tt + hf
                    rows = slice(64 * hf, 64 * hf + 64)
                    forced = sorted({0, blk} | ({blk - 1} if blk >= 1 else set()))
                    runs = []
                    for s_ in forced:
                        if runs and runs[-1][1] == s_:
                            runs[-1][1] = s_ + 1
                        else:
                            runs.append([s_, s_ + 1])
                    for a, b in runs:
                        kb.op("pool", lambda e: e.tensor_scalar(out=imp[rows, tt, a:b], in0=imp[rows, tt, a:b], scalar1=1e4,
                                                                scalar2=None, op0=ALU.add), [imp], [imp])
                    if blk < 127:
                        kb.op("pool", lambda e: e.memset(imp[rows, tt, blk + 1:128], -1e30), [], [imp])

            def t_topk(tt):
                kb.op("dve", lambda e: e.max(out=m16[:, 0:8], in_=imp[:, tt, :]), [imp], [m16])
                kb.op("dve", lambda e: e.match_replace(out=scr[:], in_to_replace=m16[:, 0:8], in_values=imp[:, tt, :],
                                                       imm_value=-3e38), [imp, m16], [scr])
                kb.op("dve", lambda e: e.max(out=m16[:, 8:16], in_=scr[:]), [scr], [m16])
                kb.op("dve", lambda e: e.tensor_scalar(out=self_[:], in0=imp[:, tt, :], scalar1=m16[:, 15:16], scalar2=None,
                                                       op0=ALU.is_ge), [imp, m16], [self_])
                kb.op("pe", lambda e: e.transpose(out=pF0[:, 0:128], in_=self_[:], identity=ident[:]), [self_, ident], [pF0])
                kb.op("act", lambda e: e.copy(sT[:, 0, tt * 128:(tt + 1) * 128], pF0[:, 0:128]), [pF0], [sT])
                kb.op("act", lambda e: e.copy(sT[:, 1, tt * 128:(tt + 1) * 128], pF0[:, 0:128]), [pF0], [sT])
            for tt in range(2):
                th.append(lambda tt=tt: t_edit(tt))
                th.append(lambda tt=tt: t_topk(tt))
            return th

        def back(c, pending):
            q_b, sT, oc_ = qTb[c % 2], selT[c % 2], ocs[c % 2]
            g_ = gs_all[:, 2 * c:2 * c + 2, :]
            sT2 = sT[:].rearrange("s h t -> s (h t)")
            wk = [kt for kt in range(2 * c - 4, 2 * c + 2) if kt >= 0]
            nk = 2 * c + 2
            its = [("w", kt, i, len(wk)) for i, kt in enumerate(wk)] + [("s", kt, kt, nk) for kt in range(nk)]

            def stA1(j):
                kind, kt, i, n = its[j]
                pS = ps[1 + j % 2]
                src = kwT if kind == "w" else ksT
                mm(kb, pS, pS[:, :], src, src[:, kt * 128:(kt + 1) * 128], q_b, q_b[:], True, True)
                if kind == "s":
                    pM = ps[3 + j % 2]
                    mm(kb, pM, pM[:, :], e128, e128[:, kt, :], sT, sT2, True, True)

            def stA2(j):
                kind, kt, i, n = its[j]
                pS, P = ps[1 + j % 2], Pb[j % 4]
                kb.op("act", lambda e: e.activation(out=P[:], in_=pS[:, :], func=AF.Exp), [pS], [P])

            def stB(j):
                kind, kt, i, n = its[j]
                P = Pb[j % 4]
                if kind == "s":
                    pM = ps[3 + j % 2]
                    kb.op("dve", lambda e: e.tensor_tensor(out=P[:], in0=P[:], in1=pM[:, :], op=ALU.mult), [P, pM], [P])
                for hh_ in range(2):
                    Ph = P[:, hh_ * 256:(hh_ + 1) * 256]
                    if kt >= 2 * c:
                        kb.op("pool", lambda e: e.affine_select(out=Ph, in_=Ph, pattern=[[1, 256]], compare_op=ALU.is_ge,
                                                                fill=0.0, base=256 * c - 128 * kt, channel_multiplier=-1),
                              [P], [P])
                    if kind == "w" and kt <= 2 * c - 3:
                        kb.op("pool", lambda e: e.affine_select(out=Ph, in_=Ph, pattern=[[-1, 256]], compare_op=ALU.is_gt,
                                                                fill=0.0, base=128 * kt - 256 * c + 512,
                                                                channel_multiplier=1), [P], [P])

            def stC(j):
                kind, kt, i, n = its[j]
                P = Pb[j % 4]
                pO, va = (pOw, vwa) if kind == "w" else (pOs, vsa)
                for sub in range(4):
                    mm(kb, pO, pO[:, sub * 65:(sub + 1) * 65], P, P[:, sub * 128:(sub + 1) * 128], va, va[:, kt, :],
                       i == 0 and sub == 0, i == n - 1, skip=True)

            stages = (stA1, stA2, stB, stC)
            n = len(its)
            for step in range(n + len(stages) - 1):
                for si in reversed(range(len(stages))):
                    j = step - si
                    if 0 <= j < n:
                        stages[si](j)
                for _ in range(1):
                    if pending:
                        pending.pop(0)()
            while pending:
                pending.pop(0)()
            y = yt[c % 2]
            g4 = g_.rearrange("p t (h b) -> p t h b", b=3)
            for bi, pO in ((1, pOs), (2, pOw)):
                den = pO[:, 0:260].rearrange("p (h t e) -> p t h e", h=2, t=2, e=65)[:, :, :, 64]
                rb_ = rdB[:, bi - 1, :, :]
                kb.op("dve", lambda e: e.tensor_scalar(out=rb_, in0=den, scalar1=1e-30, scalar2=None, op0=ALU.max), [pO], [rdB])
                kb.op("dve", lambda e: e.reciprocal(out=rb_, in_=rb_), [rdB], [rdB])
                kb.op("dve", lambda e: e.tensor_tensor(out=rb_, in0=rb_, in1=g4[:, :, :, bi], op=ALU.mult), [rdB, gs_all], [rdB])
            for sub in range(4):
                h, tt = sub // 2, sub % 2
                yo = y[:, tt, h * 64:(h + 1) * 64]
                kb.op("dve", lambda e: e.tensor_scalar(out=yo, in0=oc_[:, sub, :], scalar1=g_[:, tt, h * 3:h * 3 + 1],
                                                       scalar2=None, op0=ALU.mult), [oc_, gs_all], [y])
                for bi, pO in ((1, pOs), (2, pOw)):
                    kb.op("dve", lambda e: e.scalar_tensor_tensor(out=yo, in0=pO[:, sub * 65:sub * 65 + 64],
                                                                  scalar=rdB[:, bi - 1, tt, h:h + 1], in1=yo, op0=ALU.mult,
                                                                  op1=ALU.add), [pO, rdB, y], [y])
            if ytok is not None:
                kb.dma("sp", ytok, ytok[c * 256:(c + 1) * 256, :].rearrange("(t p) e -> p t e", p=128), y, y[:], anchor=y)
                return
            pyT = ps[0]
            for tt in range(2):
                kb.op("pe", lambda e: e.transpose(out=pyT[:, tt * 128:(tt + 1) * 128], in_=y[:, tt, :], identity=ident[:]),
                      [y, ident], [pyT])
            yT_ = ytT[c % 2]
            kb.op("act", lambda e: e.copy(yT_[:], pyT[:, 0:256]), [pyT], [yT_])
            kb.dma("sp", ynsa, ynsa[:, c * 256:(c + 1) * 256], yT_, yT_[:], anchor=yT_)

        for t_ in front(0):
            t_()
        for c in range(NC_):
            back(c, front(c + 1) if c + 1 < NC_ else [])


NF = 1792
NK = 1036


def win_perm():
    kv0 = 2048
    f = list(range(0, 1024)) + list(range(1024, 1536)) + list(range(kv0, kv0 + 128)) + list(range(kv0 + 128, kv0 + 256))
    k = (list(range(1536, 1792)) + list(range(1792, 2048)) + list(range(kv0 + 256, kv0 + 768)) + list(range(2816, 2828)))
    assert len(f) == NF and len(k) == NK
    return np.array(f + k, np.int64)


def emit_P2(kb, ps, ident, x, ntok, ccol, adaw, adab, gcol, winp, zT, ztok):
    with kb.phase():
        modT = kb.sb([128, 48], F32, "modT")
        with kb.phase() as ph:
            emit_mod(kb, ph, modT, ccol, adaw, adab, ps[0], ps[1])
        gm = emit_gmod(kb, modT, gcol, 8, "gm1")
        wb = kb.sb([128, 8, N_IN], BF16, "winb")
        win_v = winp.t.rearrange("(kt p) n -> p kt n", p=128)
        for kt in range(8):
            kb.dma("pool", wb, wb[:, kt, :], winp, win_v[:, kt, :])
        xt = [kb.sb([128, D], F32, "xt") for _ in range(2)]
        aT = [kb.sb([128, 8, 512], BF16, "aT") for _ in range(2)]
        zf = [kb.sb([128, 512], F32, "zf") for _ in range(3)]
        zk = [kb.sb([128, NK], F32, "zk") for _ in range(2)]
        tmps = [{"junk": kb.sb([128, D], F32, "junk"), "xs": kb.sb([128, D], F32, "xs"), "st": kb.sb([128, 4], F32, "st")}
                for _ in range(2)]
        kchunks = [(0, 512), (512, 512), (1024, NK - 1024)]
        NG = ntok // 512

        def norm_thunk(g, tt):
            def f():
                ab = aT[g % 2]
                i = 4 * g + tt
                xb = xt[i % 2]
                kb.dma("pool", xb, xb[:], x, x[i * 128:(i + 1) * 128, :])
                emit_norm_T(kb, xb, xb[:], gm, modT, 0, ident, ps[0:2],
                            [(ab, ab[:, kt, tt * 128:(tt + 1) * 128]) for kt in range(8)], None, tmps[i % 2])
            return f

        for tt in range(4):
            norm_thunk(0, tt)()
        ev = 0
        for g in range(NG):
            ab = aT[g % 2]
            nxt = [norm_thunk(g + 1, tt) for tt in range(4)] if g + 1 < NG else []
            for ct in range(NF // 128):
                pz = ps[2 + ev % 4]
                zb = zf[ev % 3]
                for kt in range(8):
                    mm(kb, pz, pz[:, :], wb, wb[:, kt, ct * 128:(ct + 1) * 128], ab, ab[:, kt, :], kt == 0, kt == 7)
                if ev % 2 == 0:
                    kb.op("act", lambda e: e.copy(zb[:], pz[:, :]), [pz], [zb])
                else:
                    kb.op("dve", lambda e: e.tensor_copy(out=zb[:], in_=pz[:, :]), [pz], [zb])
                ev += 1
                kb.dma("sp", zT, zT[ct * 128:(ct + 1) * 128, g * 512:(g + 1) * 512], zb, zb[:], anchor=zb)
                if ct % 4 == 3 and nxt:
                    nxt.pop(0)()
            for tt in range(4):
                i = 4 * g + tt
                zb = zk[i % 2]
                for (c0, cw) in kchunks:
                    pz = ps[2 + ev % 4]
                    for kt in range(8):
                        mm(kb, pz, pz[:, 0:cw], ab, ab[:, kt, tt * 128:(tt + 1) * 128], wb, wb[:, kt, NF + c0:NF + c0 + cw],
                           kt == 0, kt == 7)
                    if ev % 2 == 0:
                        kb.op("act", lambda e: e.copy(zb[:, c0:c0 + cw], pz[:, 0:cw]), [pz], [zb])
                    else:
                        kb.op("dve", lambda e: e.tensor_copy(out=zb[:, c0:c0 + cw], in_=pz[:, 0:cw]), [pz], [zb])
                    ev += 1
                kb.dma("sp", ztok, ztok[i * 128:(i + 1) * 128, :], zb, zb[:], anchor=zb)
                if tt == 1 and nxt:
                    nxt.pop(0)()
            while nxt:
                nxt.pop(0)()


def build_fused(depth=2):
    kb = KB()
    EI = "ExternalInput"
    x = kb.dram("x", [T, D], F32, EI)
    ccol = kb.dram("ccol", [128, 8], F32, EI)
    L = []
    for l in range(depth):
        d = {}
        for name, shape in (("adaw", [D, 6 * D]), ("adab", [1, 6 * D]), ("gcol1", [128, 8]), ("gcol2", [128, 8]),
                            ("winp", [D, N_IN]), ("wglu", [256, 256]), ("wout", [D, D]),
                            ("qg", [128, 64]), ("kg", [128, 64]), ("pe_k", [64, 32]), ("pe_v", [64, 32]),
                            ("k_w1", [2048, 256]), ("v_w1", [2048, 256]), ("k_w2", [256, 64]), ("v_w2", [256, 64])):
            d[name] = kb.dram(f"{name}_{l}", shape, F32, EI)
        for p in range(2):
            for name, shape in (("cw", [128, 3]), ("sp_lam", [128, 4, 2]), ("sp_ldt", [128, 4]), ("sp_b", [128, 4, 2, 128]),
                                ("sp_c", [128, 4, 2, 128]), ("sp_d", [128, 1])):
                d[f"{name}{p}"] = kb.dram(f"{name}_{l}_{p}", shape, F32, EI)
        moe = (l % 2 == 1)
        NE = NEXP if moe else 1
        d["wg"] = kb.dram(f"wg_{l}", [NE, D, DFF], F32, EI)
        d["wu"] = kb.dram(f"wu_{l}", [NE, D, DFF], F32, EI)
        d["wd"] = kb.dram(f"wd_{l}", [NE, DFF, D], F32, EI)
        d["router"] = kb.dram(f"router_{l}", [D, NEXP], F32, EI) if moe else None
        L.append(d)
    C = {}
    for name, shape, dt in (("pos", [128, 64], I32), ("pos_cmp", [128, 4], I32), ("invf", [128, 32], F32),
                            ("ov", [128, 4, 128], F32), ("e128", [128, 64, 128], F32)):
        C[name] = kb.dram("nsa_" + name, shape, dt, EI)
    HT = T // 2
    out = kb.dram("out", [HT, D], F32, "ExternalOutput")
    tokidx = kb.dram("tokidx", [128, HT // 128], I32, EI)
    ytokd = kb.dram("ytok_last", [T, D], F32)
    zT = kb.dram("zT", [NF, T], F32)
    ztok = kb.dram("ztok", [T, NK], F32)
    yT = kb.dram("yT", [D, T], F32)
    h1s = kb.dram("h1s", [T, D], F32)
    hmid = [kb.dram(f"hmid{l}", [T, D], F32) for l in range(depth - 1)]
    C["qn"] = kb.dram("nsa_qn", [T, 128], F32)

    ps = [kb.ps(f"P{i}") for i in range(8)]
    ident = make_ident(kb)
    hin = x
    for l in range(depth):
        d = L[l]
        moe = (l % 2 == 1)
        hout = out if l == depth - 1 else hmid[l]
        emit_P2(kb, ps, ident, hin, T, ccol, d["adaw"], d["adab"], d["gcol1"], d["winp"], zT, ztok)
        last = (l == depth - 1)

        def yv(c0):
            return View(ytokd, ytokd.t[:, c0:c0 + 128]) if last else None

        for p in range(2):
            conv_in = View(zT, zT.t[0:768, :].rearrange("(w c) t -> w c t", w=3)[:, 128 * p:128 * p + 128, :])
            emit_conv(kb, ps, conv_in, d[f"cw{p}"], View(yT, yT.t[128 * p:128 * p + 128, :]), ident=ident, ytok=yv(128 * p))
            emit_ssm(kb, ps, ident, View(zT, zT.t[768 + 128 * p:768 + 128 * p + 128, :]), d[f"sp_lam{p}"], d[f"sp_ldt{p}"],
                     d[f"sp_b{p}"], d[f"sp_c{p}"], d[f"sp_d{p}"], View(yT, yT.t[256 + 128 * p:256 + 128 * p + 128, :]),
                     ytok=yv(256 + 128 * p))
            qT = View(zT, zT.t[1024 + 128 * p:1024 + 128 * p + 128, :].rearrange("(h d) t -> h d t", h=2))
            kT = View(zT, zT.t[1280 + 128 * p:1280 + 128 * p + 128, :].rearrange("(h d) t -> h d t", h=2))
            emit_sb(kb, ps, ident, qT, kT, View(ztok, ztok.t[:, 128 * p:128 * p + 128]),
                    View(yT, yT.t[512 + 128 * p:512 + 128 * p + 128, :]), ytok=yv(512 + 128 * p))
            I = dict(C)
            I.update({"q": View(ztok, ztok.t[:, 256 + 128 * p:256 + 128 * p + 128]),
                      "kcT": View(zT, zT.t[1536 + 64 * p:1536 + 64 * p + 64, :]),
                      "vcT": View(zT, zT.t[1664 + 64 * p:1664 + 64 * p + 64, :]),
                      "ks": View(ztok, ztok.t[:, 512 + 64 * p:512 + 64 * p + 64]),
                      "vs": View(ztok, ztok.t[:, 640 + 64 * p:640 + 64 * p + 64]),
                      "kw": View(ztok, ztok.t[:, 768 + 64 * p:768 + 64 * p + 64]),
                      "vw": View(ztok, ztok.t[:, 896 + 64 * p:896 + 64 * p + 64]),
                      "gl": View(ztok, ztok.t[:, 1024 + 6 * p:1024 + 6 * p + 6])})
            for nm in ("qg", "kg", "pe_k", "pe_v", "k_w1", "v_w1", "k_w2", "v_w2"):
                I[nm] = d[nm]
            emit_nsa(kb, ps, ident, I, View(yT, yT.t[768 + 128 * p:768 + 128 * p + 128, :]), ytok=yv(768 + 128 * p))
        if last:
            emit_F(kb, ps, ident, moe, HT, hin, None, ccol, d["adaw"], d["adab"], d["gcol2"], d["wglu"], d["wout"],
                   d["wg"], d["wu"], d["wd"], d["router"], hout, h1s, gather={"idx": tokidx, "ytok": ytokd})
        else:
            emit_F(kb, ps, ident, moe, T, hin, yT, ccol, d["adaw"], d["adab"], d["gcol2"], d["wglu"], d["wout"],
                   d["wg"], d["wu"], d["wd"], d["router"], hout, h1s)
        hin = hout
    kb.finish([out])
    return kb


_PROGS = {}


def _col128(v):
    return np.ascontiguousarray(np.asarray(v).reshape(-1, 128).T)


def _C(a):
    return np.ascontiguousarray(a)


def kernel(**inp):
    inp = {k: np.asarray(v) for k, v in inp.items()}
    depth = inp["ada_w"].shape[0]
    if "fused" not in _PROGS:
        _PROGS["fused"] = build_fused(depth)
    kb = _PROGS["fused"]
    ov, e128, invf = nsa_consts()
    perm = win_perm()
    shared = {"nsa_invf": invf, "nsa_ov": ov, "nsa_e128": e128}
    for l in range(depth):
        moe = (l % 2 == 1)
        i = l // 2
        shared[f"adaw_{l}"] = inp["ada_w"][l]
        shared[f"adab_{l}"] = _C(inp["ada_b"][l][None, :])
        shared[f"gcol1_{l}"] = _col128(inp["norm_mix_g"][l])
        shared[f"gcol2_{l}"] = _col128(inp["norm_ffn_g"][l])
        shared[f"winp_{l}"] = _C(inp["w_in"][l][:, perm])
        shared[f"wglu_{l}"] = inp["ssm_w_glu"][l]
        shared[f"wout_{l}"] = inp["w_out"][l]
        shared[f"qg_{l}"] = _C(np.broadcast_to(inp["nsa_q_norm_g"][l][None, :], (128, 64)))
        shared[f"kg_{l}"] = _C(np.broadcast_to(inp["nsa_k_norm_g"][l][None, :], (128, 64)))
        shared[f"pe_k_{l}"] = _C(inp["cmp_pos_k"][l].T)
        shared[f"pe_v_{l}"] = _C(inp["cmp_pos_v"][l].T)
        shared[f"k_w1_{l}"] = inp["cmp_k_w1"][l]
        shared[f"v_w1_{l}"] = inp["cmp_v_w1"][l]
        shared[f"k_w2_{l}"] = inp["cmp_k_w2"][l]
        shared[f"v_w2_{l}"] = inp["cmp_v_w2"][l]
        for p in range(2):
            shared[f"cw_{l}_{p}"] = _C(inp["conv_w"][l][:, 128 * p:128 * p + 128].T)
            for k, v in ssm_layout(inp, l, p).items():
                shared[f"{k}_{l}_{p}"] = v
        if moe:
            shared[f"wg_{l}"] = inp["moe_w_gate"][i]
            shared[f"wu_{l}"] = inp["moe_w_up"][i]
            shared[f"wd_{l}"] = inp["moe_w_down"][i]
            shared[f"router_{l}"] = inp["moe_router"][i]
        else:
            shared[f"wg_{l}"] = inp["ffn_w_gate"][i:i + 1]
            shared[f"wu_{l}"] = inp["ffn_w_up"][i:i + 1]
            shared[f"wd_{l}"] = inp["ffn_w_down"][i:i + 1]
    maps = []
    for c in range(8):
        b = c // 2
        pos = inp["positions"][b].astype(np.int32)
        pc = np.zeros(512, np.int32)
        pc[:511] = pos[16 * np.arange(511) + 31]
        m = dict(shared)
        m["x"] = _C(inp["x"][b].astype(np.float32))
        m["ccol"] = _col128(inp["c"][b])
        m["nsa_pos"] = _C(pos.reshape(64, 128).T)
        m["nsa_pos_cmp"] = _C(pc.reshape(4, 128).T)
        half = c % 2
        m["tokidx"] = _C((half * (T // 2) + np.arange(T // 2, dtype=np.int32)).reshape(T // 256, 128).T)
        maps.append({k: _C(v) for k, v in m.items()})
    res = run_bass_kernel_spmd(kb.nc, maps, core_ids=list(range(8)))
    out = np.stack([np.concatenate([res.results[2 * b]["out"], res.results[2 * b + 1]["out"]], axis=0) for b in range(NB)])
    return out.astype(np.float32)
```

```python
import math
from contextlib import ExitStack

import numpy as np
import concourse.bass as bass
import concourse.mybir as mybir
from concourse.bass_utils import run_bass_kernel_spmd

F32 = mybir.dt.float32
BF16 = mybir.dt.bfloat16
I32 = mybir.dt.int32
AF = mybir.ActivationFunctionType
ALU = mybir.AluOpType
AX = mybir.AxisListType

D = 1024
T = 8192
NB = 4
DFF = 3584
NEXP = 8
N_IN = 2828
GELU_K = 1.5957691216057308


class Buf:
    __slots__ = ("t", "name", "w", "r", "dsem", "dcnt", "ssem", "scnt", "excl")

    def __init__(self, t, name):
        self.t = t
        self.name = name
        self.w = None
        self.r = {}
        self.dsem = None
        self.dcnt = 0
        self.ssem = None
        self.scnt = 0
        self.excl = False

    def __getitem__(self, idx):
        return self.t[idx]


class View:
    def __init__(self, parent, ap):
        object.__setattr__(self, "parent", parent)
        object.__setattr__(self, "t", ap)

    def __getitem__(self, idx):
        return self.t[idx]

    def __getattr__(self, k):
        return getattr(object.__getattribute__(self, "parent"), k)

    def __setattr__(self, k, v):
        setattr(object.__getattribute__(self, "parent"), k, v)


SEM_LIMIT = 30000


class KB:
    def __init__(self, strict=True, num_devices=None):
        if num_devices is None:
            self.nc = bass.Bass("TRN2", target_bir_lowering=False)
        else:
            self.nc = bass.Bass("TRN2", target_bir_lowering=False, num_devices=num_devices)
        nc = self.nc
        self.eng = {"pe": nc.tensor, "act": nc.scalar, "dve": nc.vector, "pool": nc.gpsimd, "sp": nc.sync}
        self.esem = {k: nc.alloc_semaphore(name="es_" + k) for k in self.eng}
        self.ecnt = {k: 0 for k in self.eng}
        self.eold = {}
        self.waited = {k: {} for k in self.eng}
        self.strict = strict
        self.nbuf = 0
        self.ninst = 0
        self.nsem = len(self.eng)
        self.stack = ExitStack()
        self.phases = []
        self.sem_pool = []
        self.ssem_pool = []

    def dram(self, name, shape, dt, kind="Internal"):
        t = self.nc.dram_tensor(name, list(shape), dt, kind=kind)
        return Buf(t.ap(), name)

    def _alloc(self, ctx, name, stack):
        ph = self.phases[-1] if (self.phases and stack is None) else None
        st = stack or (ph.st if ph is not None else self.stack)
        b = Buf(st.enter_context(ctx), name)
        if ph is not None:
            ph.bufs.append(b)
        return b

    def sb(self, shape, dt, name=None, stack=None):
        self.nbuf += 1
        name = (name or "sb") + f"_{self.nbuf}"
        return self._alloc(self.nc.sbuf_tensor(name, list(shape), dt), name, stack)

    def ps(self, name=None, stack=None):
        self.nbuf += 1
        name = (name or "ps") + f"_{self.nbuf}"
        b = self._alloc(self.nc.psum_tensor(name, [128, 512], F32), name, stack)
        b.excl = True
        return b

    def _new_sem(self, name):
        self.nsem += 1
        return self.nc.alloc_semaphore(name=f"{name}_{self.nsem}")

    def _wait(self, e, ev):
        if ev is None:
            return
        sem, val = ev
        if sem is self.esem[e] and (e == "pe" or not self.strict):
            return
        k = id(sem)
        if self.waited[e].get(k, 0) >= val:
            return
        self.eng[e].wait_ge(sem, val)
        self.waited[e][k] = val

    def _deps(self, e, reads, writes):
        for b in reads:
            self._wait(e, b.w)
            if b.excl:
                for ev in b.r.values():
                    self._wait(e, ev)
        for b in writes:
            self._wait(e, b.w)
            for ev in b.r.values():
                self._wait(e, ev)

    @staticmethod
    def _record(ev, reads, writes):
        for b in reads:
            b.r[id(ev[0])] = ev
        for b in writes:
            b.w = ev
            b.r = {}

    def op(self, e, fn, reads=(), writes=()):
        self._deps(e, reads, writes)
        ins = fn(self.eng[e])
        self.ecnt[e] += 1
        ins.then_inc(self.esem[e], 1)
        self._record((self.esem[e], self.ecnt[e]), reads, writes)
        self.ninst += 1
        if self.ecnt[e] >= SEM_LIMIT:
            self.eold[e] = (self.esem[e], self.ecnt[e])
            self.esem[e] = self._new_sem("es_" + e)
            self.ecnt[e] = 0
        return ins

    def _dma_sem(self, anchor, soft):
        sem_a, cnt_a, pool = ("ssem", "scnt", self.ssem_pool) if soft else ("dsem", "dcnt", self.sem_pool)
        if getattr(anchor, sem_a) is None or getattr(anchor, cnt_a) >= SEM_LIMIT:
            got = False
            while pool:
                sem, cnt = pool.pop()
                if cnt < SEM_LIMIT - 4096:
                    setattr(anchor, sem_a, sem)
                    setattr(anchor, cnt_a, cnt)
                    got = True
                    break
            if not got:
                setattr(anchor, sem_a, self._new_sem("ss" if soft else "ds"))
                setattr(anchor, cnt_a, 0)
        setattr(anchor, cnt_a, getattr(anchor, cnt_a) + 16)
        return getattr(anchor, sem_a), getattr(anchor, cnt_a)

    def dma(self, q, out_buf, out_ap, in_buf, in_ap, anchor=None, **kw):
        if anchor is None:
            anchor = out_buf
        sem, val = self._dma_sem(anchor, q == "pool")
        self._deps(q, [in_buf], [out_buf])
        ins = self.eng[q].dma_start(out=out_ap, in_=in_ap, **kw)
        ins.then_inc(sem, 16)
        self._record((sem, val), [in_buf], [out_buf])
        self.ninst += 1
        return ins

    def gather(self, out_buf, out_ap, in_buf, in_ap, idx_buf, idx_ap):
        sem, val = self._dma_sem(out_buf, True)
        self._deps("pool", [in_buf, idx_buf], [out_buf])
        ins = self.nc.gpsimd.indirect_dma_start(out=out_ap, out_offset=None, in_=in_ap,
                                                in_offset=bass.IndirectOffsetOnAxis(ap=idx_ap, axis=0))
        ins.then_inc(sem, 16)
        self._record((sem, val), [in_buf, idx_buf], [out_buf])
        self.ninst += 1
        return ins

    def _last_ev(self, k):
        if self.ecnt[k] > 0:
            return (self.esem[k], self.ecnt[k])
        return self.eold.get(k)

    def barrier(self, bufs=()):
        for e in self.eng:
            for k in self.eng:
                if k != e:
                    self._wait(e, self._last_ev(k))
            for b in bufs:
                if b.dsem is not None and b.dcnt > 0:
                    self._wait(e, (b.dsem, b.dcnt))
                if b.ssem is not None and b.scnt > 0:
                    self._wait(e, (b.ssem, b.scnt))

    def phase(self):
        return _Phase(self)

    def finish(self, out_bufs):
        for b in out_bufs:
            self._wait("sp", b.w)
        for k in self.eng:
            if k != "sp":
                self._wait("sp", self._last_ev(k))
        self.stack.close()


class _Phase:
    def __init__(self, kb):
        self.kb = kb
        self.st = ExitStack()
        self.bufs = []

    def __enter__(self):
        self.kb.phases.append(self)
        return self

    def sb(self, shape, dt, name=None):
        b = self.kb.sb(shape, dt, name, self.st)
        self.bufs.append(b)
        return b

    def ps(self, name=None):
        b = self.kb.ps(name, self.st)
        self.bufs.append(b)
        return b

    def __exit__(self, *a):
        assert self.kb.phases.pop() is self
        if a[0] is not None:
            return False
        self.kb.barrier(self.bufs)
        for b in self.bufs:
            if b.dsem is not None:
                self.kb.sem_pool.append((b.dsem, b.dcnt))
                b.dsem = None
            if b.ssem is not None:
                self.kb.ssem_pool.append((b.ssem, b.scnt))
                b.ssem = None
        self.st.close()
        return False


def mm(kb, out_buf, out_ap, a_buf, lhsT, b_buf, rhs, start, stop, skip=False):
    kw = {"skip_group_check": True} if skip else {}
    return kb.op("pe", lambda e: e.matmul(out_ap, lhsT=lhsT, rhs=rhs, start=start, stop=stop, **kw),
                 [a_buf, b_buf], [out_buf])


def make_ident(kb, dt=F32):
    ident = kb.sb([128, 128], dt, "ident")
    kb.op("pool", lambda e: e.memset(ident[:], 1.0), [], [ident])
    kb.op("pool", lambda e: e.affine_select(out=ident[:], in_=ident[:], pattern=[[1, 128]],
                                            compare_op=ALU.is_equal, fill=0.0, base=0, channel_multiplier=-1),
          [ident], [ident])
    return ident


def emit_mod(kb, ph, modT, ccol, adaw, adab, psA, psB):
    cs = ph.sb([128, 8], F32, "cs")
    cond = ph.sb([128, 8], F32, "cond")
    brow = ph.sb([1, 6144], F32, "brow")
    one = ph.sb([1, 1], F32, "one")
    wb = [ph.sb([128, 8, 512], F32, "adawb") for _ in range(2)]
    modrow = ph.sb([1, 6144], F32, "modrow")
    kb.dma("sp", cs, cs[:], ccol, ccol[:])
    kb.dma("sp", brow, brow[:], adab, adab[:])
    kb.op("act", lambda e: e.activation(out=cond[:], in_=cs[:], func=AF.Silu), [cs], [cond])
    kb.op("dve", lambda e: e.memset(one[:], 1.0), [], [one])
    adaw_v = adaw.t.rearrange("(kt p) n -> p kt n", p=128)
    for cc in range(12):
        w = wb[cc % 2]
        kb.dma("sp", w, w[:], adaw, adaw_v[:, :, cc * 512:(cc + 1) * 512])
        for kt in range(8):
            mm(kb, psA, psA[0:1, :], cond, cond[:, kt:kt + 1], w, w[:, kt, :], kt == 0, kt == 7)
        kb.op("dve", lambda e: e.tensor_tensor(out=modrow[0:1, cc * 512:(cc + 1) * 512], in0=psA[0:1, :],
                                               in1=brow[0:1, cc * 512:(cc + 1) * 512], op=ALU.add),
              [psA, brow], [modrow])
    for j in range(48):
        mm(kb, psB, psB[:, j:j + 1], modrow, modrow[0:1, j * 128:(j + 1) * 128], one, one[0:1, 0:1], True, True)
    kb.op("dve", lambda e: e.tensor_copy(out=modT[:], in_=psB[:, 0:48]), [psB], [modT])
    return modrow, modT


def emit_bcast_row(kb, dst, modrow, c0, ones_row, psA):
    for cc in range(2):
        mm(kb, psA, psA[:, :], ones_row, ones_row[0:1, :], modrow, modrow[0:1, c0 + cc * 512:c0 + (cc + 1) * 512],
           True, True)
        kb.op("dve", lambda e: e.tensor_copy(out=dst[:, cc * 512:(cc + 1) * 512], in_=psA[:, :]), [psA], [dst])


def emit_gmod(kb, modT, gcol_d, jscale, name):
    g = kb.sb([128, 8], F32, name + "_g")
    gm = kb.sb([128, 8], F32, name)
    kb.dma("sp", g, g[:], gcol_d, gcol_d[:])
    kb.op("dve", lambda e: e.tensor_scalar(out=gm[:], in0=modT[:, jscale:jscale + 8], scalar1=1.0, scalar2=None,
                                           op0=ALU.add), [modT], [gm])
    kb.op("dve", lambda e: e.tensor_tensor(out=gm[:], in0=gm[:], in1=g[:], op=ALU.mult), [gm, g], [gm])
    return gm


def emit_norm_T(kb, src, src_ap, gm, modT, jshift, ident, pT, aT_views, aTf_views, tmp):
    junk, xs, stt = tmp["junk"], tmp["xs"], tmp["st"]
    kb.op("act", lambda e: e.activation(out=junk[:], in_=src_ap, func=AF.Square, accum_out=stt[:, 0:1]),
          [src], [junk, stt])
    kb.op("dve", lambda e: e.tensor_scalar(out=stt[:, 1:2], in0=stt[:, 0:1], scalar1=1.0 / D, scalar2=1e-6,
                                           op0=ALU.mult, op1=ALU.add), [stt], [stt])
    kb.op("act", lambda e: e.activation(out=stt[:, 2:3], in_=stt[:, 1:2], func=AF.Sqrt), [stt], [stt])
    kb.op("dve", lambda e: e.reciprocal(out=stt[:, 3:4], in_=stt[:, 2:3]), [stt], [stt])
    kb.op("dve", lambda e: e.tensor_scalar(out=xs[:], in0=src_ap, scalar1=stt[:, 3:4], scalar2=None, op0=ALU.mult),
          [src, stt], [xs])
    for kt in range(8):
        pb = pT[kt // 4]
        kb.op("pe", lambda e: e.transpose(out=pb[:, (kt % 4) * 128:(kt % 4 + 1) * 128],
                                          in_=xs[:, kt * 128:(kt + 1) * 128], identity=ident[:]),
              [xs, ident], [pb])
    for kt in range(8):
        pb = pT[kt // 4]
        pin = pb[:, (kt % 4) * 128:(kt % 4 + 1) * 128]
        ob, oap = aT_views[kt]
        if kt % 2 == 0:
            kb.op("dve", lambda e: e.tensor_scalar(out=oap, in0=pin, scalar1=gm[:, kt:kt + 1],
                                                   scalar2=modT[:, jshift + kt:jshift + kt + 1],
                                                   op0=ALU.mult, op1=ALU.add), [pb, gm, modT], [ob])
        else:
            kb.op("act", lambda e: e.activation(out=oap, in_=pin, func=AF.Identity,
                                                scale=gm[:, kt:kt + 1], bias=modT[:, jshift + kt:jshift + kt + 1]),
                  [pb, gm, modT], [ob])
        if aTf_views is not None:
            fb, fap = aTf_views[kt]
            if kt % 2 == 1:
                kb.op("dve", lambda e: e.tensor_scalar(out=fap, in0=pin, scalar1=gm[:, kt:kt + 1],
                                                       scalar2=modT[:, jshift + kt:jshift + kt + 1],
                                                       op0=ALU.mult, op1=ALU.add), [pb, gm, modT], [fb])
            else:
                kb.op("act", lambda e: e.activation(out=fap, in_=pin, func=AF.Identity,
                                                    scale=gm[:, kt:kt + 1],
                                                    bias=modT[:, jshift + kt:jshift + kt + 1]),
                      [pb, gm, modT], [fb])


def build_P(ntok=4096):
    kb = KB()
    NT = ntok // 128
    x = kb.dram("x", [ntok, D], F32, "ExternalInput")
    ccol = kb.dram("ccol", [128, 8], F32, "ExternalInput")
    adaw = kb.dram("adaw", [D, 6 * D], F32, "ExternalInput")
    adab = kb.dram("adab", [1, 6 * D], F32, "ExternalInput")
    gcol = kb.dram("gcol", [128, 8], F32, "ExternalInput")
    win = kb.dram("win", [D, N_IN], F32, "ExternalInput")
    z = kb.dram("z", [ntok, N_IN], F32, "ExternalOutput")

    ps = [kb.ps(f"P{i}") for i in range(6)]
    modT = kb.sb([128, 48], F32, "modT")
    with kb.phase() as ph:
        modrow, modT = emit_mod(kb, ph, modT, ccol, adaw, adab, ps[0], ps[1])
    gm = emit_gmod(kb, modT, gcol, 8, "gm1")
    ident = make_ident(kb)
    wb = kb.sb([128, 8, N_IN], BF16, "winb")
    win_v = win.t.rearrange("(kt p) n -> p kt n", p=128)
    for kt in range(8):
        kb.dma("pool", wb, wb[:, kt, :], win, win_v[:, kt, :])
    xt = [kb.sb([128, D], F32, "xt") for _ in range(2)]
    aT = [kb.sb([128, 8, 128], BF16, "aT") for _ in range(2)]
    zt = [kb.sb([128, N_IN], F32, "zt") for _ in range(2)]
    tmp = {"junk": kb.sb([128, D], F32, "junk"), "xs": kb.sb([128, D], F32, "xs"), "st": kb.sb([128, 4], F32, "st")}
    chunks = [(c0, min(512, N_IN - c0)) for c0 in range(0, N_IN, 512)]
    for i in range(NT):
        xb, ab, zb = xt[i % 2], aT[i % 2], zt[i % 2]
        kb.dma("sp", xb, xb[:], x, x[i * 128:(i + 1) * 128, :])
        emit_norm_T(kb, xb, xb[:], gm, modT, 0, ident, ps[0:2], [(ab, ab[:, kt, :]) for kt in range(8)], None, tmp)
        for ci, (c0, cw) in enumerate(chunks):
            pz = ps[2 + ci % 4]
            for kt in range(8):
                mm(kb, pz, pz[:, 0:cw], ab, ab[:, kt, :], wb, wb[:, kt, c0:c0 + cw], kt == 0, kt == 7)
            if ci % 2 == 0:
                kb.op("act", lambda e: e.copy(zb[:, c0:c0 + cw], pz[:, 0:cw]), [pz], [zb])
            else:
                kb.op("dve", lambda e: e.tensor_copy(out=zb[:, c0:c0 + cw], in_=pz[:, 0:cw]), [pz], [zb])
        kb.dma("sp", z, z[i * 128:(i + 1) * 128, :], zb, zb[:], anchor=zb)
    kb.finish([z])
    return kb


def emit_F(kb, ps, ident, moe, ntok, x, yT, ccol, adaw, adab, gcol, wglu, wout, wg, wu, wd, router, hout, h1s,
           gather=None):
    CH = 1024
    NCH = ntok // CH
    NE = NEXP if moe else 1
    with kb.phase():
        Gmix = kb.sb([128, D], F32, "Gmix")
        Gffn = kb.sb([128, D], F32, "Gffn")
        modT = kb.sb([128, 48], F32, "modT")
        with kb.phase() as ph:
            modrow, modT = emit_mod(kb, ph, modT, ccol, adaw, adab, ps[0], ps[1])
            ones_row = ph.sb([1, 128], F32, "ones_row")
            kb.op("dve", lambda e: e.memset(ones_row[:], 1.0), [], [ones_row])
            emit_bcast_row(kb, Gmix, modrow, 2 * D, ones_row, ps[0])
            emit_bcast_row(kb, Gffn, modrow, 5 * D, ones_row, ps[0])
        gm = emit_gmod(kb, modT, gcol, 32, "gm2")

        woutb = kb.sb([128, 8, D], BF16, "woutb")
        wout_v = wout.t.rearrange("(kt p) n -> p kt n", p=128)
        for kt in range(8):
            kb.dma("pool", woutb, woutb[:, kt, :], wout, wout_v[:, kt, :])
        wglub = kb.sb([128, 2, 256], BF16, "wglub")
        kb.dma("pool", wglub, wglub[:], wglu, wglu.t.rearrange("(kt p) n -> p kt n", p=128))
        if moe:
            routf = kb.sb([128, 8, NEXP], F32, "routf")
            kb.dma("sp", routf, routf[:], router, router.t.rearrange("(kt p) n -> p kt n", p=128))
            gates = kb.sb([128, 8, NEXP], F32, "gates")
            a2f = [kb.sb([128, 8, 128], F32, "a2f") for _ in range(2)]
            rt = {k: kb.sb([128, 8], F32, "rt_" + k) for k in ("lg", "m8", "d", "mask")}
            rs = kb.sb([128, 4], F32, "rt_s")

        yTb = kb.sb([128, 8, 512], BF16, "yTb")
        ys = kb.sb([128, 2, 512], F32, "ys")
        t1 = kb.sb([128, 2, 512], F32, "t1")
        t2 = kb.sb([128, 2, 512], F32, "t2")
        gb = kb.sb([128, 2, 512], BF16, "gb")
        xt = [kb.sb([128, D], F32, "xt") for _ in range(2)]
        tmpm = kb.sb([128, D], F32, "tmpm")
        h1t = [kb.sb([128, D], F32, "h1t") for _ in range(2)]
        tmp = {"junk": kb.sb([128, D], F32, "junk"), "xs": kb.sb([128, D], F32, "xs"), "st": kb.sb([128, 4], F32, "st")}
        a2T = kb.sb([128, 8, CH], BF16, "a2T")
        acc = kb.sb([128, CH // 128, D], F32, "acc")
        hT = kb.sb([128, 4, CH], BF16, "hT")
        sg = [kb.sb([128, 512], F32, "sg") for _ in range(2)]
        wgb = [kb.sb([128, 8, 512], BF16, "wgb") for _ in range(2)]
        wub = [kb.sb([128, 8, 512], BF16, "wub") for _ in range(2)]
        wdb = [kb.sb([128, 4, D], BF16, "wdb") for _ in range(2)]
        ot = kb.sb([128, D], F32, "ot")

        yT_v = yT.t.rearrange("(ct p) t -> p ct t", p=128) if gather is None else None
        if gather is not None:
            gidx = kb.sb([128, ntok // 128], I32, "gidx")
            kb.dma("sp", gidx, gidx[:], gather["idx"], gather["idx"][:])
            gidx_u = gidx[:].bitcast(mybir.dt.uint32)
            ytile = [kb.sb([128, D], F32, "ytile") for _ in range(2)]
            ytok = gather["ytok"]
        units = [(e, fg) for e in range(NE) for fg in range(DFF // 512)]

        def load_unit(ui, slot):
            e, fg = units[ui]
            gv = wg.t[e].rearrange("(kt p) f -> p kt f", p=128)
            uv = wu.t[e].rearrange("(kt p) f -> p kt f", p=128)
            dv = wd.t[e].rearrange("(ft p) d -> p ft d", p=128)
            kb.dma("pool", wgb[slot], wgb[slot][:], wg, gv[:, :, fg * 512:(fg + 1) * 512])
            kb.dma("pool", wub[slot], wub[slot][:], wu, uv[:, :, fg * 512:(fg + 1) * 512])
            kb.dma("pool", wdb[slot], wdb[slot][:], wd, dv[:, fg * 4:(fg + 1) * 4, :])

        for c in range(NCH):
            t0 = c * CH
            for hc in range(CH // 512):
                ta = t0 + hc * 512
                if gather is None:
                    kb.dma("sp", ys, ys[:], yT, yT_v[:, 2:4, ta:ta + 512])
                    kb.dma("pool", yTb, yTb[:, 0:2, :], yT, yT_v[:, 0:2, ta:ta + 512])
                    kb.dma("pool", yTb, yTb[:, 4:8, :], yT, yT_v[:, 4:8, ta:ta + 512])
                else:
                    for tt in range(4):
                        gi = ta // 128 + tt
                        yb_ = ytile[tt % 2]
                        kb.gather(yb_, yb_[:], ytok, ytok.t[:, :], gidx, gidx_u[:, gi:gi + 1])
                        for ct in range(8):
                            pb = ps[4 + ct // 4]
                            kb.op("pe", lambda e: e.transpose(out=pb[:, (ct % 4) * 128:(ct % 4 + 1) * 128],
                                                              in_=yb_[:, ct * 128:(ct + 1) * 128], identity=ident[:]),
                                  [yb_, ident], [pb])
                        for ct in range(8):
                            pb = ps[4 + ct // 4]
                            pin = pb[:, (ct % 4) * 128:(ct % 4 + 1) * 128]
                            if ct in (2, 3):
                                kb.op("dve", lambda e: e.tensor_copy(out=ys[:, ct - 2, tt * 128:(tt + 1) * 128], in_=pin),
                                      [pb], [ys])
                            else:
                                kb.op("act", lambda e: e.copy(yTb[:, ct, tt * 128:(tt + 1) * 128], pin), [pb], [yTb])
                kb.op("pool", lambda e: e.tensor_tensor(out=t1[:], in0=ys[:], in1=ys[:], op=ALU.mult), [ys], [t1])
                kb.op("dve", lambda e: e.tensor_scalar(out=t1[:], in0=t1[:], scalar1=0.044715, scalar2=1.0,
                                                       op0=ALU.mult, op1=ALU.add), [t1], [t1])
                kb.op("pool", lambda e: e.tensor_tensor(out=t1[:], in0=t1[:], in1=ys[:], op=ALU.mult), [t1, ys], [t1])
                kb.op("act", lambda e: e.activation(out=t2[:], in_=t1[:], func=AF.Sigmoid, scale=GELU_K), [t1], [t2])
                kb.op("dve", lambda e: e.tensor_tensor(out=t1[:], in0=t2[:], in1=ys[:], op=ALU.mult), [t2, ys], [t1])
                kb.op("act", lambda e: e.copy(gb[:], t1[:]), [t1], [gb])
                for jt in range(2):
                    pp = ps[jt]
                    for ct in range(2):
                        mm(kb, pp, pp[:, :], wglub, wglub[:, ct, jt * 128:(jt + 1) * 128], gb, gb[:, ct, :],
                           ct == 0, ct == 1)
                    kb.op("act", lambda e: e.activation(out=t2[:, jt, :], in_=pp[:, :], func=AF.Sigmoid), [pp], [t2])
                    kb.op("dve", lambda e: e.tensor_tensor(out=yTb[:, 2 + jt, :], in0=t1[:, jt, :], in1=t2[:, jt, :],
                                                           op=ALU.mult), [t1, t2], [yTb])
                def tileA(tt):
                    ti = hc * 4 + tt
                    tg = (t0 // 128) + ti
                    xb = xt[ti % 2]
                    hb = h1t[ti % 2]
                    if gather is None:
                        kb.dma("sp", xb, xb[:], x, x[tg * 128:(tg + 1) * 128, :])
                    else:
                        kb.gather(xb, xb[:], x, x.t[:, :], gidx, gidx_u[:, tg:tg + 1])
                    for cc in range(2):
                        pm = ps[2 + cc]
                        for ct in range(8):
                            mm(kb, pm, pm[:, :], yTb, yTb[:, ct, tt * 128:(tt + 1) * 128], woutb,
                               woutb[:, ct, cc * 512:(cc + 1) * 512], ct == 0, ct == 7)
                        kb.op("dve", lambda e: e.tensor_tensor(out=tmpm[:, cc * 512:(cc + 1) * 512], in0=pm[:, :],
                                                               in1=Gmix[:, cc * 512:(cc + 1) * 512], op=ALU.mult),
                              [pm, Gmix], [tmpm])
                    kb.op("pool", lambda e: e.tensor_tensor(out=hb[:], in0=tmpm[:], in1=xb[:], op=ALU.add),
                          [tmpm, xb], [hb])
                    kb.dma("act", h1s, h1s[tg * 128:(tg + 1) * 128, :], hb, hb[:], anchor=hb)

                def tileB(tt):
                    ti = hc * 4 + tt
                    hb = h1t[ti % 2]
                    af = a2f[ti % 2] if moe else None
                    emit_norm_T(kb, hb, hb[:], gm, modT, 24, ident, ps[4:6],
                                [(a2T, a2T[:, kt, ti * 128:(ti + 1) * 128]) for kt in range(8)],
                                [(af, af[:, kt, :]) for kt in range(8)] if moe else None, tmp)
                    if moe:
                        pr = ps[6]
                        for kt in range(8):
                            mm(kb, pr, pr[:, 0:NEXP], af, af[:, kt, :], routf, routf[:, kt, :], kt == 0, kt == 7)
                        lg, m8, dd, mask = rt["lg"], rt["m8"], rt["d"], rt["mask"]
                        kb.op("dve", lambda e: e.tensor_copy(out=lg[:], in_=pr[:, 0:NEXP]), [pr], [lg])
                        kb.op("dve", lambda e: e.max(out=m8[:], in_=lg[:]), [lg], [m8])
                        kb.op("dve", lambda e: e.tensor_scalar(out=rs[:, 0:1], in0=m8[:, 0:1], scalar1=-1.0, scalar2=None,
                                                               op0=ALU.mult), [m8], [rs])
                        kb.op("act", lambda e: e.activation(out=dd[:], in_=lg[:], func=AF.Exp, bias=rs[:, 0:1]),
                              [lg, rs], [dd])
                        kb.op("act", lambda e: e.activation(out=rs[:, 1:2], in_=m8[:, 1:2], func=AF.Exp, bias=rs[:, 0:1]),
                              [m8, rs], [rs])
                        kb.op("dve", lambda e: e.tensor_scalar(out=rs[:, 2:3], in0=rs[:, 1:2], scalar1=1.0, scalar2=None,
                                                               op0=ALU.add), [rs], [rs])
                        kb.op("dve", lambda e: e.reciprocal(out=rs[:, 3:4], in_=rs[:, 2:3]), [rs], [rs])
                        kb.op("dve", lambda e: e.tensor_scalar(out=mask[:], in0=lg[:], scalar1=m8[:, 1:2], scalar2=None,
                                                               op0=ALU.is_ge), [lg, m8], [mask])
                        kb.op("dve", lambda e: e.scalar_tensor_tensor(out=gates[:, ti, :], in0=dd[:], scalar=rs[:, 3:4],
                                                                      in1=mask[:], op0=ALU.mult, op1=ALU.mult),
                              [dd, rs, mask], [gates])

                tileA(0)
                for tt in range(4):
                    if tt + 1 < 4:
                        tileA(tt + 1)
                    tileB(tt)
            if c == 0:
                load_unit(0, 0)
            for ui, (ex, fg) in enumerate(units):
                gu = c * len(units) + ui
                slot = gu % 2
                if ui + 1 < len(units):
                    load_unit(ui + 1, (gu + 1) % 2)
                elif c + 1 < NCH:
                    load_unit(0, (gu + 1) % 2)
                g_b, u_b, d_b = wgb[slot], wub[slot], wdb[slot]
                k = 0
                for th in range(CH // 512):
                    for ft in range(4):
                        pg = ps[(k % 2) * 2]
                        pu = ps[(k % 2) * 2 + 1]
                        sgb = sg[k % 2]
                        k += 1
                        for kt in range(8):
                            mm(kb, pg, pg[:, :], g_b, g_b[:, kt, ft * 128:(ft + 1) * 128], a2T,
                               a2T[:, kt, th * 512:(th + 1) * 512], kt == 0, kt == 7)
                        for kt in range(8):
                            mm(kb, pu, pu[:, :], u_b, u_b[:, kt, ft * 128:(ft + 1) * 128], a2T,
                               a2T[:, kt, th * 512:(th + 1) * 512], kt == 0, kt == 7)
                        kb.op("act", lambda e: e.activation(out=sgb[:], in_=pg[:, :], func=AF.Silu), [pg], [sgb])
                        kb.op("dve", lambda e: e.tensor_tensor(out=hT[:, ft, th * 512:(th + 1) * 512], in0=sgb[:],
                                                               in1=pu[:, :], op=ALU.mult), [sgb, pu], [hT])
                k = 0
                for ti in range(CH // 128):
                    for cc in range(2):
                        pd = ps[4 + k % 4]
                        k += 1
                        for ft in range(4):
                            mm(kb, pd, pd[:, :], hT, hT[:, ft, ti * 128:(ti + 1) * 128], d_b,
                               d_b[:, ft, cc * 512:(cc + 1) * 512], ft == 0, ft == 3)
                        dst = acc[:, ti, cc * 512:(cc + 1) * 512]
                        gsc = gates[:, ti, ex:ex + 1] if moe else 1.0
                        gdeps = [gates] if moe else []
                        if ui == 0:
                            kb.op("dve", lambda e: e.tensor_scalar(out=dst, in0=pd[:, :], scalar1=gsc, scalar2=None,
                                                                   op0=ALU.mult), [pd] + gdeps, [acc])
                        else:
                            kb.op("dve", lambda e: e.scalar_tensor_tensor(out=dst, in0=pd[:, :], scalar=gsc, in1=dst,
                                                                          op0=ALU.mult, op1=ALU.add),
                                  [pd, acc] + gdeps, [acc])
            for ti in range(CH // 128):
                tg = (t0 // 128) + ti
                hb = h1t[ti % 2]
                kb.dma("pool", hb, hb[:], h1s, h1s[tg * 128:(tg + 1) * 128, :])
                kb.op("dve", lambda e: e.tensor_tensor(out=ot[:], in0=acc[:, ti, :], in1=Gffn[:], op=ALU.mult),
                      [acc, Gffn], [ot])
                kb.op("pool", lambda e: e.tensor_tensor(out=ot[:], in0=ot[:], in1=hb[:], op=ALU.add), [ot, hb], [ot])
                kb.dma("sp", hout, hout[tg * 128:(tg + 1) * 128, :], ot, ot[:], anchor=ot)


def build_F(moe, ntok=4096):
    kb = KB()
    x = kb.dram("x", [ntok, D], F32, "ExternalInput")
    yT = kb.dram("yT", [D, ntok], F32, "ExternalInput")
    ccol = kb.dram("ccol", [128, 8], F32, "ExternalInput")
    adaw = kb.dram("adaw", [D, 6 * D], F32, "ExternalInput")
    adab = kb.dram("adab", [1, 6 * D], F32, "ExternalInput")
    gcol = kb.dram("gcol", [128, 8], F32, "ExternalInput")
    wglu = kb.dram("wglu", [256, 256], F32, "ExternalInput")
    wout = kb.dram("wout", [D, D], F32, "ExternalInput")
    NE = NEXP if moe else 1
    wg = kb.dram("wg", [NE, D, DFF], F32, "ExternalInput")
    wu = kb.dram("wu", [NE, D, DFF], F32, "ExternalInput")
    wd = kb.dram("wd", [NE, DFF, D], F32, "ExternalInput")
    router = kb.dram("router", [D, NEXP], F32, "ExternalInput") if moe else None
    hout = kb.dram("hout", [ntok, D], F32, "ExternalOutput")
    h1s = kb.dram("h1s", [ntok, D], F32, "Internal")
    ps = [kb.ps(f"P{i}") for i in range(8)]
    ident = make_ident(kb)
    emit_F(kb, ps, ident, moe, ntok, x, yT, ccol, adaw, adab, gcol, wglu, wout, wg, wu, wd, router, hout, h1s)
    kb.finish([hout])
    return kb


TWO_PI = 2.0 * math.pi


def emit_tok_store(kb, ps, ident, y, n, t0, ytok, stg):
    for q in range(n // 512):
        pb = ps[6 + q % 2]
        for k in range(4):
            blk = q * 4 + k
            kb.op("pe", lambda e: e.transpose(out=pb[:, k * 128:(k + 1) * 128], in_=y[:, blk * 128:(blk + 1) * 128],
                                              identity=ident[:]), [y, ident], [pb])
        sg_ = stg[q % 2]
        kb.op("act", lambda e: e.copy(sg_[:].rearrange("p i e -> p (i e)"), pb[:, :]), [pb], [sg_])
        r0 = t0 + q * 512
        kb.dma("sp", ytok, ytok[r0:r0 + 512, :].rearrange("(i p) e -> p i e", p=128), sg_, sg_[:], anchor=sg_)


def emit_conv(kb, ps, conv_in, cw, yconvT, ident=None, ytok=None):
    CH = 2048
    with kb.phase() as ph:
        cwt = ph.sb([128, 3], F32, "cwt")
        kb.dma("sp", cwt, cwt[:], cw, cw[:])
        gbt = [ph.sb([128, CH], F32, "gbt") for _ in range(2)]
        gct = [ph.sb([128, CH], F32, "gct") for _ in range(2)]
        ut = [ph.sb([128, CH], F32, "ut") for _ in range(2)]
        vb = [ph.sb([128, CH + 2], F32, "vb") for _ in range(2)]
        yb = [ph.sb([128, CH], F32, "yb") for _ in range(2)]
        kb.op("pool", lambda e: e.memset(vb[1][:, CH:CH + 2], 0.0), [], [vb[1]])
        stg = [ph.sb([128, 4, 128], F32, "stg") for _ in range(2)] if ytok is not None else None
        for c in range(T // CH):
            k = c % 2
            sl = slice(c * CH, (c + 1) * CH)
            kb.dma("act", gbt[k], gbt[k][:], conv_in, conv_in[0, :, sl])
            kb.dma("act", gct[k], gct[k][:], conv_in, conv_in[1, :, sl])
            kb.dma("act", ut[k], ut[k][:], conv_in, conv_in[2, :, sl])
            v, vp, y = vb[k], vb[1 - k], yb[k]
            kb.op("pool", lambda e: e.tensor_copy(out=v[:, 0:2], in_=vp[:, CH:CH + 2]), [vp], [v])
            kb.op("pool", lambda e: e.tensor_tensor(out=v[:, 2:CH + 2], in0=gct[k][:], in1=ut[k][:], op=ALU.mult),
                  [gct[k], ut[k]], [v])
            kb.op("dve", lambda e: e.tensor_scalar(out=y[:], in0=v[:, 2:CH + 2], scalar1=cwt[:, 2:3], scalar2=None,
                                                   op0=ALU.mult), [v, cwt], [y])
            kb.op("dve", lambda e: e.scalar_tensor_tensor(out=y[:], in0=v[:, 1:CH + 1], scalar=cwt[:, 1:2], in1=y[:],
                                                          op0=ALU.mult, op1=ALU.add), [v, cwt, y], [y])
            kb.op("dve", lambda e: e.scalar_tensor_tensor(out=y[:], in0=v[:, 0:CH], scalar=cwt[:, 0:1], in1=y[:],
                                                          op0=ALU.mult, op1=ALU.add), [v, cwt, y], [y])
            kb.op("pool", lambda e: e.tensor_tensor(out=y[:], in0=y[:], in1=gbt[k][:], op=ALU.mult), [y, gbt[k]], [y])
            if ytok is None:
                kb.dma("sp", yconvT, yconvT[:, sl], y, y[:], anchor=y)
            else:
                emit_tok_store(kb, ps, ident, y, CH, c * CH, ytok, stg)


def emit_ssm(kb, ps, ident, uT, sp_lam, sp_ldt, sp_b, sp_c, sp_d, yssmT, ytok=None):
    L = 512
    NS = 4
    NL = 10
    with kb.phase() as ph:
        lam = ph.sb([128, NS, 2], F32, "lam")
        ldt = ph.sb([128, NS], F32, "ldt")
        bb = ph.sb([128, NS, 2, 128], F32, "bb")
        cc = ph.sb([128, NS, 2, 128], F32, "cc")
        dsk = ph.sb([128, 1], F32, "dsk")
        kb.dma("sp", lam, lam[:], sp_lam, sp_lam[:])
        kb.dma("sp", ldt, ldt[:], sp_ldt, sp_ldt[:])
        kb.dma("sp", bb, bb[:], sp_b, sp_b[:])
        kb.dma("sp", cc, cc[:], sp_c, sp_c[:])
        kb.dma("sp", dsk, dsk[:], sp_d, sp_d[:])
        S = {k: ph.sb([128, NS], F32, "s_" + k) for k in
             ("lr", "li", "dt", "a", "th", "r", "m", "ab", "nr", "ni", "l2", "inv", "kr", "ki", "t1", "t2", "nki")}
        Pc = ph.sb([128, NL, NS], F32, "Pc")
        Ps = ph.sb([128, NL, NS], F32, "Ps")

        def V(e, fn, reads, writes):
            kb.op(e, fn, reads, writes)

        lr, li, dt, a, th, r = S["lr"], S["li"], S["dt"], S["a"], S["th"], S["r"]
        V("dve", lambda e: e.tensor_scalar(out=lr[:], in0=lam[:, :, 0], scalar1=-1e-4, scalar2=None, op0=ALU.min),
          [lam], [lr])
        V("dve", lambda e: e.tensor_copy(out=li[:], in_=lam[:, :, 1]), [lam], [li])
        V("act", lambda e: e.activation(out=dt[:], in_=ldt[:], func=AF.Exp), [ldt], [dt])
        V("dve", lambda e: e.tensor_tensor(out=a[:], in0=lr[:], in1=dt[:], op=ALU.mult), [lr, dt], [a])
        V("dve", lambda e: e.tensor_tensor(out=th[:], in0=li[:], in1=dt[:], op=ALU.mult), [li, dt], [th])
        V("act", lambda e: e.activation(out=r[:], in_=a[:], func=AF.Exp), [a], [r])
        m = S["m"]
        for _ in range(8):
            V("dve", lambda e: e.tensor_scalar(out=m[:], in0=th[:], scalar1=math.pi, scalar2=None, op0=ALU.is_gt), [th], [m])
            V("dve", lambda e: e.scalar_tensor_tensor(out=th[:], in0=m[:], scalar=-TWO_PI, in1=th[:], op0=ALU.mult,
                                                      op1=ALU.add), [th, m], [th])
        ab = S["ab"]
        for _ in range(2):
            V("dve", lambda e: e.tensor_scalar(out=ab[:], in0=th[:], scalar1=-1.0, scalar2=None, op0=ALU.mult), [th], [ab])
            V("dve", lambda e: e.tensor_scalar(out=m[:], in0=ab[:], scalar1=math.pi, scalar2=None, op0=ALU.is_gt), [ab], [m])
            V("dve", lambda e: e.scalar_tensor_tensor(out=th[:], in0=m[:], scalar=TWO_PI, in1=th[:], op0=ALU.mult,
                                                      op1=ALU.add), [th, m], [th])
        halfpi = ph.sb([128, 1], F32, "halfpi")
        V("dve", lambda e: e.memset(halfpi[:], math.pi / 2), [], [halfpi])
        V("dve", lambda e: e.tensor_scalar(out=ab[:], in0=th[:], scalar1=-1.0, scalar2=None, op0=ALU.mult), [th], [ab])
        V("dve", lambda e: e.tensor_tensor(out=ab[:], in0=ab[:], in1=th[:], op=ALU.max), [ab, th], [ab])
        V("act", lambda e: e.activation(out=Pc[:, 0, :], in_=ab[:], func=AF.Sin, scale=-1.0, bias=halfpi[:, 0:1]),
          [ab, halfpi], [Pc])
        V("act", lambda e: e.activation(out=Ps[:, 0, :], in_=th[:], func=AF.Sin), [th], [Ps])
        t1, t2 = S["t1"], S["t2"]
        for lv in range(1, NL):
            V("dve", lambda e: e.tensor_tensor(out=t1[:], in0=Pc[:, lv - 1, :], in1=Pc[:, lv - 1, :], op=ALU.mult), [Pc], [t1])
            V("dve", lambda e: e.tensor_tensor(out=t2[:], in0=Ps[:, lv - 1, :], in1=Ps[:, lv - 1, :], op=ALU.mult), [Ps], [t2])
            V("dve", lambda e: e.tensor_tensor(out=Pc[:, lv, :], in0=t1[:], in1=t2[:], op=ALU.subtract), [t1, t2], [Pc])
            V("dve", lambda e: e.tensor_tensor(out=t1[:], in0=Pc[:, lv - 1, :], in1=Ps[:, lv - 1, :], op=ALU.mult), [Pc, Ps], [t1])
            V("dve", lambda e: e.tensor_scalar(out=Ps[:, lv, :], in0=t1[:], scalar1=2.0, scalar2=None, op0=ALU.mult), [t1], [Ps])
        nr, ni, l2, inv, kr, ki, nki = S["nr"], S["ni"], S["l2"], S["inv"], S["kr"], S["ki"], S["nki"]
        V("dve", lambda e: e.tensor_tensor(out=nr[:], in0=r[:], in1=Pc[:, 0, :], op=ALU.mult), [r, Pc], [nr])
        V("dve", lambda e: e.tensor_scalar(out=nr[:], in0=nr[:], scalar1=-1.0, scalar2=None, op0=ALU.add), [nr], [nr])
        V("dve", lambda e: e.tensor_tensor(out=ni[:], in0=r[:], in1=Ps[:, 0, :], op=ALU.mult), [r, Ps], [ni])
        V("dve", lambda e: e.tensor_tensor(out=t1[:], in0=lr[:], in1=lr[:], op=ALU.mult), [lr], [t1])
        V("dve", lambda e: e.tensor_tensor(out=t2[:], in0=li[:], in1=li[:], op=ALU.mult), [li], [t2])
        V("dve", lambda e: e.tensor_tensor(out=l2[:], in0=t1[:], in1=t2[:], op=ALU.add), [t1, t2], [l2])
        V("dve", lambda e: e.reciprocal(out=inv[:], in_=l2[:]), [l2], [inv])
        V("dve", lambda e: e.tensor_tensor(out=t1[:], in0=nr[:], in1=lr[:], op=ALU.mult), [nr, lr], [t1])
        V("dve", lambda e: e.tensor_tensor(out=t2[:], in0=ni[:], in1=li[:], op=ALU.mult), [ni, li], [t2])
        V("dve", lambda e: e.tensor_tensor(out=kr[:], in0=t1[:], in1=t2[:], op=ALU.add), [t1, t2], [kr])
        V("dve", lambda e: e.tensor_tensor(out=kr[:], in0=kr[:], in1=inv[:], op=ALU.mult), [kr, inv], [kr])
        V("dve", lambda e: e.tensor_tensor(out=t1[:], in0=ni[:], in1=lr[:], op=ALU.mult), [ni, lr], [t1])
        V("dve", lambda e: e.tensor_tensor(out=t2[:], in0=nr[:], in1=li[:], op=ALU.mult), [nr, li], [t2])
        V("dve", lambda e: e.tensor_tensor(out=ki[:], in0=t1[:], in1=t2[:], op=ALU.subtract), [t1, t2], [ki])
        V("dve", lambda e: e.tensor_tensor(out=ki[:], in0=ki[:], in1=inv[:], op=ALU.mult), [ki, inv], [ki])
        Bre = ph.sb([128, NS, 128], F32, "Bre")
        Bim = ph.sb([128, NS, 128], F32, "Bim")
        tb = ph.sb([128, 128], F32, "tb")
        BT = ph.sb([128, NS, 2, 128], F32, "BT")
        nCim = ph.sb([128, NS, 128], F32, "nCim")
        for s in range(NS):
            V("dve", lambda e: e.tensor_scalar(out=tb[:], in0=bb[:, s, 1, :], scalar1=ki[:, s:s + 1], scalar2=None,
                                               op0=ALU.mult), [bb, ki], [tb])
            V("dve", lambda e: e.scalar_tensor_tensor(out=Bre[:, s, :], in0=bb[:, s, 0, :], scalar=kr[:, s:s + 1],
                                                      in1=tb[:], op0=ALU.mult, op1=ALU.subtract), [bb, kr, tb], [Bre])
            V("dve", lambda e: e.tensor_scalar(out=tb[:], in0=bb[:, s, 0, :], scalar1=ki[:, s:s + 1], scalar2=None,
                                               op0=ALU.mult), [bb, ki], [tb])
            V("dve", lambda e: e.scalar_tensor_tensor(out=Bim[:, s, :], in0=bb[:, s, 1, :], scalar=kr[:, s:s + 1],
                                                      in1=tb[:], op0=ALU.mult, op1=ALU.add), [bb, kr, tb], [Bim])
            for ri, Bsrc in enumerate((Bre, Bim)):
                pb = ps[ri]
                V("pe", lambda e: e.transpose(out=pb[:, 0:128], in_=Bsrc[:, s, :], identity=ident[:]), [Bsrc, ident], [pb])
                V("dve", lambda e: e.tensor_copy(out=BT[:, s, ri, :], in_=pb[:, 0:128]), [pb], [BT])
            V("dve", lambda e: e.tensor_scalar(out=nCim[:, s, :], in0=cc[:, s, 1, :], scalar1=-1.0, scalar2=None,
                                               op0=ALU.mult), [cc], [nCim])
        cosT = ph.sb([128, NS, L], F32, "cosT")
        sinT = ph.sb([128, NS, L], F32, "sinT")
        tt = ph.sb([128, L // 2], F32, "tt")
        V("dve", lambda e: e.memset(cosT[:, :, 0:1], 1.0), [], [cosT])
        V("dve", lambda e: e.memset(sinT[:, :, 0:1], 0.0), [], [sinT])
        for s in range(NS):
            for lv in range(NL - 1):
                mlen = 1 << lv
                cm = Pc[:, lv, s:s + 1]
                sm = Ps[:, lv, s:s + 1]
                V("pool", lambda e: e.tensor_scalar(out=tt[:, 0:mlen], in0=sinT[:, s, 0:mlen], scalar1=sm, scalar2=None,
                                                    op0=ALU.mult), [sinT, Ps], [tt])
                V("dve", lambda e: e.scalar_tensor_tensor(out=cosT[:, s, mlen:2 * mlen], in0=cosT[:, s, 0:mlen], scalar=cm,
                                                          in1=tt[:, 0:mlen], op0=ALU.mult, op1=ALU.subtract),
                  [cosT, Pc, tt], [cosT])
                V("pool", lambda e: e.tensor_scalar(out=tt[:, 0:mlen], in0=cosT[:, s, 0:mlen], scalar1=sm, scalar2=None,
                                                    op0=ALU.mult), [cosT, Ps], [tt])
                V("dve", lambda e: e.scalar_tensor_tensor(out=sinT[:, s, mlen:2 * mlen], in0=sinT[:, s, 0:mlen], scalar=cm,
                                                          in1=tt[:, 0:mlen], op0=ALU.mult, op1=ALU.add),
                  [sinT, Pc, tt], [sinT])
        uc = [ph.sb([128, L], F32, "uc") for _ in range(3)]
        gre = [[ph.sb([128, L], F32, "gre") for _ in range(2)] for _ in range(NS)]
        gim = [[ph.sb([128, L], F32, "gim") for _ in range(2)] for _ in range(NS)]
        NBF = 2
        m1 = [ph.sb([128, L], F32, "m1") for _ in range(NBF)]
        m2 = [ph.sb([128, L], F32, "m2") for _ in range(NBF)]
        m3 = [ph.sb([128, L], F32, "m3") for _ in range(NBF)]
        m4 = [ph.sb([128, L], F32, "m4") for _ in range(NBF)]
        xre = [ph.sb([128, L], F32, "xre") for _ in range(NBF)]
        xim = [ph.sb([128, L], F32, "xim") for _ in range(NBF)]
        q1 = ph.sb([128, L], F32, "q1")
        q2 = ph.sb([128, L], F32, "q2")
        hre = [ph.sb([128, L], F32, "hre") for _ in range(2)]
        him = [ph.sb([128, L], F32, "him") for _ in range(2)]
        ini = [ph.sb([128, 4], F32, "ini") for _ in range(2)]
        yo = [ph.sb([128, L], F32, "yo") for _ in range(2)]
        EL = NL - 1
        NCH = T // L
        its = [(c, s_) for c in range(NCH) for s_ in range(NS)]

        def stA(j):
            c, s_ = its[j]
            u = uc[c % 3]
            if s_ == 0:
                kb.dma("pool", u, u[:], uT, uT[:, c * L:(c + 1) * L])
            pa, pb = ps[(j % 2) * 2], ps[(j % 2) * 2 + 1]
            mm(kb, pa, pa[:, :], BT, BT[:, s_, 0, :], u, u[:], True, True)
            mm(kb, pb, pb[:, :], BT, BT[:, s_, 1, :], u, u[:], True, True)

        def stB(j):
            c, s_ = its[j]
            k = j % NBF
            pa, pb = ps[(j % 2) * 2], ps[(j % 2) * 2 + 1]
            cs_, sn_ = cosT[:, s_, :], sinT[:, s_, :]
            V("dve", lambda e: e.tensor_tensor(out=m1[k][:], in0=pa[:, :], in1=cs_, op=ALU.mult), [pa, cosT], [m1[k]])
            V("dve", lambda e: e.tensor_tensor(out=m4[k][:], in0=pa[:, :], in1=sn_, op=ALU.mult), [pa, sinT], [m4[k]])
            V("dve", lambda e: e.tensor_tensor(out=m2[k][:], in0=pb[:, :], in1=sn_, op=ALU.mult), [pb, sinT], [m2[k]])
            V("dve", lambda e: e.tensor_tensor(out=m3[k][:], in0=pb[:, :], in1=cs_, op=ALU.mult), [pb, cosT], [m3[k]])
            V("pool", lambda e: e.tensor_tensor(out=xre[k][:], in0=m1[k][:], in1=m2[k][:], op=ALU.add), [m1[k], m2[k]], [xre[k]])
            V("pool", lambda e: e.tensor_tensor(out=xim[k][:], in0=m3[k][:], in1=m4[k][:], op=ALU.subtract),
              [m3[k], m4[k]], [xim[k]])

        def stC(j):
            c, s_ = its[j]
            k = j % NBF
            gr, gi = gre[s_][c % 2], gim[s_][c % 2]
            if c == 0:
                i_re, i_im, ideps = 0.0, 0.0, []
            else:
                gpr, gpi = gre[s_][1 - c % 2], gim[s_][1 - c % 2]
                elc, els = Pc[:, EL, s_:s_ + 1], Ps[:, EL, s_:s_ + 1]
                ii = ini[j % 2]
                V("dve", lambda e: e.tensor_scalar(out=ii[:, 2:3], in0=gpi[:, L - 1:L], scalar1=els, scalar2=None,
                                                   op0=ALU.mult), [gpi, Ps], [ii])
                V("dve", lambda e: e.scalar_tensor_tensor(out=ii[:, 0:1], in0=gpr[:, L - 1:L], scalar=elc,
                                                          in1=ii[:, 2:3], op0=ALU.mult, op1=ALU.subtract),
                  [gpr, Pc, ii], [ii])
                V("dve", lambda e: e.tensor_scalar(out=ii[:, 3:4], in0=gpr[:, L - 1:L], scalar1=els, scalar2=None,
                                                   op0=ALU.mult), [gpr, Ps], [ii])
                V("dve", lambda e: e.scalar_tensor_tensor(out=ii[:, 1:2], in0=gpi[:, L - 1:L], scalar=elc,
                                                          in1=ii[:, 3:4], op0=ALU.mult, op1=ALU.add),
                  [gpi, Pc, ii], [ii])
                i_re, i_im, ideps = ii[:, 0:1], ii[:, 1:2], [ii]
            rb = r[:, s_:s_ + 1].to_broadcast([128, L])
            V("dve", lambda e: e.tensor_tensor_scan(out=gr[:], data0=rb, data1=xre[k][:], initial=i_re, op0=ALU.mult,
                                                    op1=ALU.add), [r, xre[k]] + ideps, [gr])
            V("dve", lambda e: e.tensor_tensor_scan(out=gi[:], data0=rb, data1=xim[k][:], initial=i_im, op0=ALU.mult,
                                                    op1=ALU.add), [r, xim[k]] + ideps, [gi])

        def stD(j):
            c, s_ = its[j]
            gr, gi = gre[s_][c % 2], gim[s_][c % 2]
            cs_, sn_ = cosT[:, s_, :], sinT[:, s_, :]
            hr, hi = hre[j % 2], him[j % 2]
            V("pool", lambda e: e.tensor_tensor(out=q1[:], in0=gr[:], in1=cs_, op=ALU.mult), [gr, cosT], [q1])
            V("pool", lambda e: e.tensor_tensor(out=q2[:], in0=gi[:], in1=sn_, op=ALU.mult), [gi, sinT], [q2])
            V("pool", lambda e: e.tensor_tensor(out=hr[:], in0=q1[:], in1=q2[:], op=ALU.subtract), [q1, q2], [hr])
            V("pool", lambda e: e.tensor_tensor(out=q1[:], in0=gr[:], in1=sn_, op=ALU.mult), [gr, sinT], [q1])
            V("pool", lambda e: e.tensor_tensor(out=q2[:], in0=gi[:], in1=cs_, op=ALU.mult), [gi, cosT], [q2])
            V("pool", lambda e: e.tensor_tensor(out=hi[:], in0=q1[:], in1=q2[:], op=ALU.add), [q1, q2], [hi])

        def stE(j):
            c, s_ = its[j]
            hr, hi = hre[j % 2], him[j % 2]
            py = ps[4 + c % 2]
            mm(kb, py, py[:, :], cc, cc[:, s_, 0, :], hr, hr[:], s_ == 0, False)
            mm(kb, py, py[:, :], nCim, nCim[:, s_, :], hi, hi[:], False, s_ == NS - 1)
            if s_ == NS - 1:
                u = uc[c % 3]
                y = yo[c % 2]
                V("dve", lambda e: e.scalar_tensor_tensor(out=y[:], in0=u[:], scalar=dsk[:, 0:1], in1=py[:, :],
                                                          op0=ALU.mult, op1=ALU.add), [u, dsk, py], [y])
                if ytok is None:
                    kb.dma("sp", yssmT, yssmT[:, c * L:(c + 1) * L], y, y[:], anchor=y)
                else:
                    emit_tok_store(kb, ps, ident, y, L, c * L, ytok, stg)

        stg = [ph.sb([128, 4, 128], F32, "stg") for _ in range(2)] if ytok is not None else None
        stages = (stA, stB, stC, stD, stE)
        n = len(its)
        for step in range(n + len(stages) - 1):
            for si, st in enumerate(stages):
                j = step - si
                if 0 <= j < n:
                    st(j)


def emit_sb(kb, ps, ident, qT, kT, v, ysbT, ytok=None):
    NKT = T // 128
    with kb.phase() as ph:
        tri = ph.sb([128, 128], BF16, "tri")
        onesm = ph.sb([128, 128], BF16, "onesm")
        kb.op("pool", lambda e: e.memset(onesm[:], 1.0), [], [onesm])
        kb.op("pool", lambda e: e.memset(tri[:], 1.0), [], [tri])
        kb.op("pool", lambda e: e.affine_select(out=tri[:], in_=tri[:], pattern=[[-1, 128]], compare_op=ALU.is_ge,
                                                fill=0.0, base=0, channel_multiplier=1), [tri], [tri])
        qb = ph.sb([128, T], BF16, "qb")
        kbf = ph.sb([128, T], BF16, "kbf")
        kb.op("pool", lambda e: e.memset(qb[64:128, :], 0.0), [], [qb])
        kb.op("pool", lambda e: e.memset(kbf[64:128, :], 0.0), [], [kbf])
        vb = ph.sb([128, NKT, 64], BF16, "vb")
        NB3 = 4
        Eb = [ph.sb([128, 512], F32, "Eb") for _ in range(NB3)]
        SPb = [ph.sb([128, 512], BF16, "SPb") for _ in range(NB3)]
        Ctb = [ph.sb([128, 512], F32, "Ct") for _ in range(2)]
        Xbb = [ph.sb([128, 512], F32, "Xb") for _ in range(2)]
        Wb = [ph.sb([128, 512], BF16, "Wb") for _ in range(3)]
        Racc = ph.sb([128, 512], F32, "Racc")
        ot = [ph.sb([128, 256], F32, "ot") for _ in range(2)]
        otT = [ph.sb([64, 512], F32, "otT") for _ in range(2)]
        psA, psB, psC, psO = ps[0:2], ps[2:4], ps[4:5], ps[6:8]
        psT = ps[5]
        for h in range(2):
            for q4 in range(4):
                sl = slice(q4 * 2048, (q4 + 1) * 2048)
                kb.dma("pool", qb, qb[0:64, sl], qT, qT[h, :, sl])
                kb.dma("pool", kbf, kbf[0:64, sl], kT, kT[h, :, sl])
            kb.dma("pool", vb, vb[:], v, v.t.rearrange("(kt p) e -> p kt e", p=128)[:, :, h * 64:(h + 1) * 64])
            its = []
            for c in range(T // 512):
                nk = 4 * c + 4
                for ki in range(nk):
                    kt = nk - 1 - ki
                    its.append((c, kt, ki == 0, kt == 0, kt >= 4 * c))

            def stA(i):
                c, kt, first, last, diag = its[i]
                pa, E, SP = psA[i % 2], Eb[i % NB3], SPb[i % NB3]
                mm(kb, pa, pa[:, :], kbf, kbf[:, kt * 128:(kt + 1) * 128], qb, qb[:, c * 512:(c + 1) * 512], True, True)
                kb.op("act", lambda e: e.activation(out=E[:], in_=pa[:, :], func=AF.Exp, scale=0.125), [pa], [E])
                kb.op("act", lambda e: e.activation(out=SP[:], in_=E[:], func=AF.Ln, bias=1.0), [E], [SP])
                if diag:
                    kb.op("pool", lambda e: e.affine_select(out=SP[:], in_=SP[:], pattern=[[1, 512]],
                                                            compare_op=ALU.is_gt, fill=0.0, base=512 * c - 128 * kt,
                                                            channel_multiplier=-1), [SP], [SP])

            def stB1(i):
                c, kt, first, last, diag = its[i]
                pb, pc, SP, Ct = psB[i % 2], psC[0], SPb[i % NB3], Ctb[i % 2]
                mm(kb, pb, pb[:, :], tri, tri[:], SP, SP[:], True, True)
                if not last:
                    mm(kb, pc, pc[:, :], onesm, onesm[:], SP, SP[:], True, True)
                if first:
                    kb.op("dve", lambda e: e.tensor_copy(out=Ct[:], in_=pb[:, :]), [pb], [Ct])
                else:
                    kb.op("dve", lambda e: e.tensor_tensor(out=Ct[:], in0=pb[:, :], in1=Racc[:], op=ALU.add),
                          [pb, Racc], [Ct])
                if not last:
                    if first:
                        kb.op("dve", lambda e: e.tensor_copy(out=Racc[:], in_=pc[:, :]), [pc], [Racc])
                    else:
                        kb.op("dve", lambda e: e.tensor_tensor(out=Racc[:], in0=pc[:, :], in1=Racc[:], op=ALU.add),
                              [pc, Racc], [Racc])

            def stB2(i):
                c, kt, first, last, diag = its[i]
                E, Ct, W, Xb = Eb[i % NB3], Ctb[i % 2], Wb[i % 3], Xbb[i % 2]
                kb.op("act", lambda e: e.activation(out=Xb[:], in_=Ct[:], func=AF.Exp, scale=-1.0), [Ct], [Xb])
                kb.op("dve", lambda e: e.tensor_tensor(out=W[:], in0=E[:], in1=Xb[:], op=ALU.mult), [E, Xb], [W])
                if diag:
                    kb.op("pool", lambda e: e.affine_select(out=W[:], in_=W[:], pattern=[[1, 512]],
                                                            compare_op=ALU.is_gt, fill=0.0, base=512 * c - 128 * kt,
                                                            channel_multiplier=-1), [W], [W])

            def stC(i):
                c, kt, first, last, diag = its[i]
                W, po = Wb[i % 3], psO[c % 2]
                for sub in range(4):
                    mm(kb, po, po[:, sub * 64:(sub + 1) * 64], W, W[:, sub * 128:(sub + 1) * 128], vb, vb[:, kt, :],
                       first and sub == 0, last, skip=True)
                if last:
                    o = ot[c % 2]
                    kb.op("act", lambda e: e.copy(o[:], po[:, 0:256]), [po], [o])
                    if ytok is not None:
                        kb.dma("sp", ytok, ytok[c * 512:(c + 1) * 512, h * 64:(h + 1) * 64].rearrange("(s p) d -> p s d", p=128),
                               o, o[:].rearrange("p (s d) -> p s d", s=4), anchor=o)
                        return
                    for sub in range(4):
                        kb.op("pe", lambda e: e.transpose(out=psT[0:64, sub * 128:(sub + 1) * 128],
                                                          in_=o[:, sub * 64:(sub + 1) * 64], identity=ident[:]),
                              [o, ident], [psT])
                    oT = otT[c % 2]
                    kb.op("dve", lambda e: e.tensor_copy(out=oT[:], in_=psT[0:64, :]), [psT], [oT])
                    kb.dma("sp", ysbT, ysbT[h * 64:(h + 1) * 64, c * 512:(c + 1) * 512], oT, oT[:], anchor=oT)

            stages = (stA, stB1, stB2, stC)
            n = len(its)
            for step in range(n + len(stages) - 1):
                for si, st in enumerate(stages):
                    i = step - si
                    if 0 <= i < n:
                        st(i)


def build_M(parts=("conv", "ssm", "sb", "nsa"), nsa_stage=9, nsa_dbg=False):
    kb = KB()
    ps = [kb.ps(f"P{i}") for i in range(8)]
    ident = make_ident(kb)
    outs = []
    if "conv" in parts:
        conv_in = kb.dram("conv_in", [3, 128, T], F32, "ExternalInput")
        cw = kb.dram("cw", [128, 3], F32, "ExternalInput")
        yconvT = kb.dram("yconvT", [128, T], F32, "ExternalOutput")
        emit_conv(kb, ps, conv_in, cw, yconvT)
        outs.append(yconvT)
    if "ssm" in parts:
        uT = kb.dram("ssm_uT", [128, T], F32, "ExternalInput")
        sp_lam = kb.dram("sp_lam", [128, 4, 2], F32, "ExternalInput")
        sp_ldt = kb.dram("sp_ldt", [128, 4], F32, "ExternalInput")
        sp_b = kb.dram("sp_b", [128, 4, 2, 128], F32, "ExternalInput")
        sp_c = kb.dram("sp_c", [128, 4, 2, 128], F32, "ExternalInput")
        sp_d = kb.dram("sp_d", [128, 1], F32, "ExternalInput")
        yssmT = kb.dram("yssmT", [128, T], F32, "ExternalOutput")
        emit_ssm(kb, ps, ident, uT, sp_lam, sp_ldt, sp_b, sp_c, sp_d, yssmT)
        outs.append(yssmT)
    if "sb" in parts:
        qT = kb.dram("sb_qT", [2, 64, T], F32, "ExternalInput")
        kT = kb.dram("sb_kT", [2, 64, T], F32, "ExternalInput")
        v = kb.dram("sb_v", [T, 128], F32, "ExternalInput")
        ysb = kb.dram("ysb", [128, T], F32, "ExternalOutput")
        emit_sb(kb, ps, ident, qT, kT, v, ysb)
        outs.append(ysb)
    if "nsa" in parts:
        I = {}
        for name, shape, dt in (("q", [T, 128], F32), ("kcT", [64, T], F32), ("vcT", [64, T], F32), ("ks", [T, 64], F32),
                                ("vs", [T, 64], F32), ("kw", [T, 64], F32), ("vw", [T, 64], F32), ("gl", [T, 6], F32),
                                ("pos", [128, 64], I32), ("pos_cmp", [128, 4], I32), ("invf", [128, 32], F32),
                                ("qg", [128, 64], F32), ("kg", [128, 64], F32), ("pe_k", [64, 32], F32),
                                ("pe_v", [64, 32], F32), ("k_w1", [2048, 256], F32), ("v_w1", [2048, 256], F32),
                                ("k_w2", [256, 64], F32), ("v_w2", [256, 64], F32), ("ov", [128, 4, 128], F32),
                                ("e128", [128, 64, 128], F32)):
            I[name] = kb.dram("nsa_" + name, shape, dt, "ExternalInput")
        I["qn"] = kb.dram("nsa_qn", [T, 128], F32, "Internal")
        ynsa = kb.dram("ynsa", [128, T], F32, "ExternalOutput")
        dbg = None
        if nsa_dbg:
            dbg = {"ksT": kb.dram("dbg_ksT", [64, T], F32, "ExternalOutput"),
                   "kwT": kb.dram("dbg_kwT", [64, T], F32, "ExternalOutput"),
                   "cosA": kb.dram("dbg_cosA", [128, 64, 32], F32, "ExternalOutput"),
                   "sinA": kb.dram("dbg_sinA", [128, 64, 32], F32, "ExternalOutput"),
                   "kcT": kb.dram("dbg_kcT", [64, 512], F32, "ExternalOutput"),
                   "rhsc": kb.dram("dbg_rhsc", [128, 4, 194], F32, "ExternalOutput")}
        emit_nsa(kb, ps, ident, I, ynsa, stage=nsa_stage, dbg=dbg)
        outs.append(ynsa)
    kb.finish(outs)
    return kb


def nsa_consts():
    n = np.arange(512)[:, None]
    sblk = np.arange(128)[None, :]
    ov = ((16 * n < 64 * sblk + 64) & (16 * n + 32 > 64 * sblk) & (n < 511)).astype(np.float32)
    ov = np.ascontiguousarray(ov.reshape(4, 128, 128).transpose(1, 0, 2))
    e128 = np.zeros((128, 64, 128), np.float32)
    for kt in range(64):
        e128[2 * kt, kt, 0:64] = 1.0
        e128[2 * kt + 1, kt, 64:128] = 1.0
    invf = np.power(np.float32(10000.0), -np.arange(32, dtype=np.float32) / np.float32(32)).astype(np.float32)
    invf = np.ascontiguousarray(np.broadcast_to(invf[None, :], (128, 32)))
    return ov, e128, invf


def nsa_layout(inp, l, b, p, z):
    ov, e128, invf = nsa_consts()
    kv0 = 2048
    pos = inp["positions"][b].astype(np.int32)
    pc = np.zeros(512, np.int32)
    pc[:511] = pos[16 * np.arange(511) + 31]
    m = {
        "nsa_q": z[:, 1792 + 128 * p:1792 + 128 * p + 128],
        "nsa_kcT": z[:, kv0 + 0 * 128 + 64 * p:kv0 + 0 * 128 + 64 * p + 64].T,
        "nsa_vcT": z[:, kv0 + 1 * 128 + 64 * p:kv0 + 1 * 128 + 64 * p + 64].T,
        "nsa_ks": z[:, kv0 + 2 * 128 + 64 * p:kv0 + 2 * 128 + 64 * p + 64],
        "nsa_vs": z[:, kv0 + 3 * 128 + 64 * p:kv0 + 3 * 128 + 64 * p + 64],
        "nsa_kw": z[:, kv0 + 4 * 128 + 64 * p:kv0 + 4 * 128 + 64 * p + 64],
        "nsa_vw": z[:, kv0 + 5 * 128 + 64 * p:kv0 + 5 * 128 + 64 * p + 64],
        "nsa_gl": z[:, 2816 + 6 * p:2816 + 6 * p + 6],
        "nsa_pos": pos.reshape(64, 128).T,
        "nsa_pos_cmp": pc.reshape(4, 128).T,
        "nsa_invf": invf,
        "nsa_qg": np.broadcast_to(inp["nsa_q_norm_g"][l][None, :], (128, 64)),
        "nsa_kg": np.broadcast_to(inp["nsa_k_norm_g"][l][None, :], (128, 64)),
        "nsa_pe_k": inp["cmp_pos_k"][l].T,
        "nsa_pe_v": inp["cmp_pos_v"][l].T,
        "nsa_k_w1": inp["cmp_k_w1"][l], "nsa_v_w1": inp["cmp_v_w1"][l],
        "nsa_k_w2": inp["cmp_k_w2"][l], "nsa_v_w2": inp["cmp_v_w2"][l],
        "nsa_ov": ov, "nsa_e128": e128,
    }
    return {k: np.ascontiguousarray(v) for k, v in m.items()}


def ssm_layout(inp, l, p):
    lam = np.zeros((128, 4, 2), np.float32)
    ldt = np.zeros((128, 4), np.float32)
    bb = np.zeros((128, 4, 2, 128), np.float32)
    cc = np.zeros((128, 4, 2, 128), np.float32)
    for s in range(4):
        for gl in range(2):
            g = 8 * p + 2 * s + gl
            rows = slice(gl * 64, (gl + 1) * 64)
            cols = slice((2 * s + gl) * 16, (2 * s + gl + 1) * 16)
            lam[rows, s, 0] = inp["ssm_lam_re"][l, g]
            lam[rows, s, 1] = inp["ssm_lam_im"][l, g]
            ldt[rows, s] = inp["ssm_log_dt"][l, g]
            bb[rows, s, 0, cols] = inp["ssm_b_re"][l, g]
            bb[rows, s, 1, cols] = inp["ssm_b_im"][l, g]
            cc[rows, s, 0, cols] = inp["ssm_c_re"][l, g].T
            cc[rows, s, 1, cols] = inp["ssm_c_im"][l, g].T
    d = np.ascontiguousarray(inp["ssm_d"][l, 128 * p:128 * (p + 1)].reshape(128, 1))
    return {"sp_lam": lam, "sp_ldt": ldt, "sp_b": bb, "sp_c": cc, "sp_d": d}


CW1 = 6.28125
CW2 = TWO_PI - CW1


def emit_rope_tables(kb, ph0, posf_ap, posf_buf, n, invf, cos, sin):
    with kb.phase() as ph:
        ang = ph.sb([128, n, 32], F32, "ang")
        y = ph.sb([128, n, 32], F32, "angy")
        yi = ph.sb([128, n, 32], I32, "angi")
        m = ph.sb([128, n, 32], F32, "angm")
        r = ph.sb([128, n, 32], F32, "angr")
        kb.op("dve", lambda e: e.tensor_tensor(out=ang[:], in0=posf_ap.unsqueeze(2).to_broadcast([128, n, 32]),
                                               in1=invf[:].unsqueeze(1).to_broadcast([128, n, 32]), op=ALU.mult),
              [posf_buf, invf], [ang])
        kb.op("dve", lambda e: e.tensor_scalar(out=y[:], in0=ang[:], scalar1=1.0 / TWO_PI, scalar2=None, op0=ALU.mult),
              [ang], [y])
        kb.op("dve", lambda e: e.tensor_copy(out=yi[:], in_=y[:]), [y], [yi])
        kb.op("dve", lambda e: e.tensor_copy(out=y[:], in_=yi[:]), [yi], [y])
        kb.op("dve", lambda e: e.scalar_tensor_tensor(out=r[:], in0=y[:], scalar=-CW1, in1=ang[:], op0=ALU.mult,
                                                      op1=ALU.add), [y, ang], [r])
        kb.op("dve", lambda e: e.scalar_tensor_tensor(out=r[:], in0=y[:], scalar=-CW2, in1=r[:], op0=ALU.mult,
                                                      op1=ALU.add), [y, r], [r])

        def fix(buf):
            for _ in range(2):
                kb.op("dve", lambda e: e.tensor_scalar(out=m[:], in0=buf[:], scalar1=math.pi, scalar2=None, op0=ALU.is_gt),
                      [buf], [m])
                kb.op("dve", lambda e: e.scalar_tensor_tensor(out=buf[:], in0=m[:], scalar=-TWO_PI, in1=buf[:],
                                                              op0=ALU.mult, op1=ALU.add), [m, buf], [buf])
            for _ in range(2):
                kb.op("dve", lambda e: e.tensor_scalar(out=m[:], in0=buf[:], scalar1=-math.pi, scalar2=None, op0=ALU.is_gt),
                      [buf], [m])
                kb.op("dve", lambda e: e.tensor_scalar(out=m[:], in0=m[:], scalar1=-TWO_PI, scalar2=TWO_PI, op0=ALU.mult,
                                                       op1=ALU.add), [m], [m])
                kb.op("dve", lambda e: e.tensor_tensor(out=buf[:], in0=buf[:], in1=m[:], op=ALU.add), [m, buf], [buf])

        fix(r)
        kb.op("act", lambda e: e.activation(out=sin[:], in_=r[:], func=AF.Sin), [r], [sin])
        kb.op("dve", lambda e: e.tensor_scalar(out=r[:], in0=r[:], scalar1=math.pi / 2, scalar2=None, op0=ALU.add), [r], [r])
        fix(r)
        kb.op("act", lambda e: e.activation(out=cos[:], in_=r[:], func=AF.Sin), [r], [cos])


def emit_normrope(kb, X, xap, n, g, cos, cosap, sin, sinap, Y, yap, tmp, qscale=None):
    sq, ss, xn, t1, t2 = tmp["sq"], tmp["ss"], tmp["xn"], tmp["t1"], tmp["t2"]
    sqv, xnv = sq[:, 0:n, :], xn[:, 0:n, :]
    t1v, t2v = t1[:, 0:n, :], t2[:, 0:n, :]
    kb.op("pool", lambda e: e.tensor_tensor(out=sqv, in0=xap, in1=xap, op=ALU.mult), [X], [sq])
    kb.op("dve", lambda e: e.tensor_reduce(out=ss[:, 0:n], in_=sqv, axis=AX.X, op=ALU.add), [sq], [ss])
    kb.op("dve", lambda e: e.tensor_scalar(out=ss[:, 0:n], in0=ss[:, 0:n], scalar1=1.0 / 64, scalar2=1e-6, op0=ALU.mult,
                                           op1=ALU.add), [ss], [ss])
    kb.op("act", lambda e: e.activation(out=ss[:, 0:n], in_=ss[:, 0:n], func=AF.Sqrt), [ss], [ss])
    kb.op("dve", lambda e: e.reciprocal(out=ss[:, 0:n], in_=ss[:, 0:n]), [ss], [ss])
    if qscale is not None:
        kb.op("dve", lambda e: e.tensor_scalar(out=ss[:, 0:n], in0=ss[:, 0:n], scalar1=qscale, scalar2=None, op0=ALU.mult),
              [ss], [ss])
    kb.op("dve", lambda e: e.tensor_tensor(out=xnv, in0=xap, in1=ss[:, 0:n].unsqueeze(2).to_broadcast([128, n, 64]),
                                           op=ALU.mult), [X, ss], [xn])
    kb.op("pool", lambda e: e.tensor_tensor(out=xnv, in0=xnv, in1=g[:].unsqueeze(1).to_broadcast([128, n, 64]),
                                            op=ALU.mult), [xn, g], [xn])
    x1, x2 = xn[:, 0:n, 0:32], xn[:, 0:n, 32:64]
    kb.op("dve", lambda e: e.tensor_tensor(out=t1v, in0=x1, in1=cosap, op=ALU.mult), [xn, cos], [t1])
    kb.op("pool", lambda e: e.tensor_tensor(out=t2v, in0=x2, in1=sinap, op=ALU.mult), [xn, sin], [t2])
    kb.op("dve", lambda e: e.tensor_tensor(out=yap[:, :, 0:32], in0=t1v, in1=t2v, op=ALU.subtract), [t1, t2], [Y])
    kb.op("pool", lambda e: e.tensor_tensor(out=t1v, in0=x2, in1=cosap, op=ALU.mult), [xn, cos], [t1])
    kb.op("dve", lambda e: e.tensor_tensor(out=t2v, in0=x1, in1=sinap, op=ALU.mult), [xn, sin], [t2])
    kb.op("pool", lambda e: e.tensor_tensor(out=yap[:, :, 32:64], in0=t1v, in1=t2v, op=ALU.add), [t1, t2], [Y])


def emit_nsa(kb, ps, ident, I, ynsa, stage=9, dbg=None, ytok=None):
    NT = T // 128
    with kb.phase() as ph:
        invf = ph.sb([128, 32], F32, "invf")
        qg = ph.sb([128, 64], F32, "qg")
        kg = ph.sb([128, 64], F32, "kg")
        posi = ph.sb([128, NT], I32, "posi")
        posf = ph.sb([128, NT], F32, "posf")
        pci = ph.sb([128, 4], I32, "pci")
        pcf = ph.sb([128, 4], F32, "pcf")
        for dst, src in ((invf, "invf"), (qg, "qg"), (kg, "kg"), (posi, "pos"), (pci, "pos_cmp")):
            kb.dma("sp", dst, dst[:], I[src], I[src][:])
        kb.op("dve", lambda e: e.tensor_copy(out=posf[:], in_=posi[:]), [posi], [posf])
        kb.op("dve", lambda e: e.tensor_copy(out=pcf[:], in_=pci[:]), [pci], [pcf])
        cosA = ph.sb([128, NT, 32], F32, "cosA")
        sinA = ph.sb([128, NT, 32], F32, "sinA")
        cosC = ph.sb([128, 4, 32], F32, "cosC")
        sinC = ph.sb([128, 4, 32], F32, "sinC")
        emit_rope_tables(kb, ph, posf[:], posf, NT, invf, cosA, sinA)
        emit_rope_tables(kb, ph, pcf[:], pcf, 4, invf, cosC, sinC)

        ksT = ph.sb([128, T], BF16, "ksT")
        kwT = ph.sb([128, T], BF16, "kwT")
        kb.op("pool", lambda e: e.memset(ksT[64:128, :], 0.0), [], [ksT])
        kb.op("pool", lambda e: e.memset(kwT[64:128, :], 0.0), [], [kwT])
        vsa = ph.sb([128, NT, 65], BF16, "vsa")
        vwa = ph.sb([128, NT, 65], BF16, "vwa")
        kcT = ph.sb([64, 512], F32, "kcT")
        rhsc = ph.sb([128, 4, 194], F32, "rhsc")
        e128 = ph.sb([128, NT, 128], BF16, "e128")
        for g4 in range(4):
            kb.dma("pool", e128, e128[:, g4 * 16:(g4 + 1) * 16, :], I["e128"], I["e128"][:, g4 * 16:(g4 + 1) * 16, :])
        kb.dma("sp", rhsc, rhsc[:, :, 64:192], I["ov"], I["ov"][:])
        kb.op("dve", lambda e: e.memset(rhsc[:, :, 192:194], 1.0), [], [rhsc])
        for va, src in ((vsa, "vs"), (vwa, "vw")):
            kb.op("pool", lambda e: e.memset(va[:, :, 64:65], 1.0), [], [va])
            kb.dma("pool", va, va[:, :, 0:64], I[src], I[src].t.rearrange("(kt p) d -> p kt d", p=128))
        nr_tmp = {"sq": ph.sb([128, 16, 64], F32, "nr_sq"), "ss": ph.sb([128, 16], F32, "nr_ss"),
                  "xn": ph.sb([128, 16, 64], F32, "nr_xn"), "t1": ph.sb([128, 16, 32], F32, "nr_t1"),
                  "t2": ph.sb([128, 16, 32], F32, "nr_t2")}
        Xk = [ph.sb([128, 16, 64], F32, "Xk") for _ in range(2)]
        Yk = [ph.sb([128, 16, 64], F32, "Yk") for _ in range(2)]
        it = 0
        for dstT, src in ((ksT, "ks"), (kwT, "kw")):
            sv = I[src].t.rearrange("(tt p) d -> p tt d", p=128)
            for st in range(NT // 16):
                X, Y = Xk[it % 2], Yk[it % 2]
                it += 1
                kb.dma("sp", X, X[:], I[src], sv[:, st * 16:(st + 1) * 16, :])
                emit_normrope(kb, X, X[:], 16, kg, cosA, cosA[:, st * 16:(st + 1) * 16, :], sinA,
                              sinA[:, st * 16:(st + 1) * 16, :], Y, Y[:], nr_tmp)
                for g4 in range(4):
                    pb = ps[g4 % 2]
                    for k in range(4):
                        tt = g4 * 4 + k
                        kb.op("pe", lambda e: e.transpose(out=pb[0:64, k * 128:(k + 1) * 128], in_=Y[:, tt, :],
                                                          identity=ident[:]), [Y, ident], [pb])
                    c0 = (st * 16 + g4 * 4) * 128
                    if g4 % 2 == 0:
                        kb.op("act", lambda e: e.copy(dstT[0:64, c0:c0 + 512], pb[0:64, :]), [pb], [dstT])
                    else:
                        kb.op("dve", lambda e: e.tensor_copy(out=dstT[0:64, c0:c0 + 512], in_=pb[0:64, :]), [pb], [dstT])
        qv = I["q"].t.rearrange("(tt p) e -> p tt e", p=128)
        qnv = I["qn"].t.rearrange("(tt p) e -> p tt e", p=128)
        with kb.phase() as phq:
            Xq16 = [phq.sb([128, 16, 128], F32, "Xq16") for _ in range(2)]
            Yq16 = [phq.sb([128, 16, 128], F32, "Yq16") for _ in range(2)]
            for st in range(NT // 16):
                X, Y = Xq16[st % 2], Yq16[st % 2]
                kb.dma("sp", X, X[:], I["q"], qv[:, st * 16:(st + 1) * 16, :])
                for h in range(2):
                    emit_normrope(kb, X, X[:, :, h * 64:(h + 1) * 64], 16, qg, cosA, cosA[:, st * 16:(st + 1) * 16, :],
                                  sinA, sinA[:, st * 16:(st + 1) * 16, :], Y, Y[:, :, h * 64:(h + 1) * 64], nr_tmp,
                                  qscale=0.125)
                kb.dma("sp", I["qn"], qnv[:, st * 16:(st + 1) * 16, :], Y, Y[:], anchor=Y)
        gs_all = ph.sb([128, NT, 6], F32, "gs_all")
        glv = I["gl"].t.rearrange("(tt p) e -> p tt e", p=128)
        for g4 in range(4):
            kb.dma("sp", gs_all, gs_all[:, g4 * 16:(g4 + 1) * 16, :], I["gl"], glv[:, g4 * 16:(g4 + 1) * 16, :])
        kb.op("act", lambda e: e.activation(out=gs_all[:], in_=gs_all[:], func=AF.Sigmoid), [gs_all], [gs_all])
        with kb.phase() as ph2:
            xT = ph2.sb([64, T], F32, "xcT")
            w1 = ph2.sb([64, 32, 256], F32, "cw1")
            w2 = ph2.sb([128, 2, 64], F32, "cw2")
            peT = ph2.sb([64, 32], F32, "peT")
            cb = ph2.sb([128, 2], F32, "cb")
            hidT = ph2.sb([128, 2, 512], F32, "hidT")
            craw = ph2.sb([128, 4, 64], F32, "craw")
            kcn = ph2.sb([128, 4, 64], F32, "kcn")
            kb.op("dve", lambda e: e.memset(hidT[:, :, 511:512], 0.0), [], [hidT])
            for which in ("k", "v"):
                kb.dma("sp", xT, xT[:], I[which + "cT"], I[which + "cT"][:])
                w1v = I[which + "_w1"].t.rearrange("(r d) j -> d r j", d=64)
                for r4 in range(4):
                    kb.dma("sp", w1, w1[:, r4 * 8:(r4 + 1) * 8, :], I[which + "_w1"], w1v[:, r4 * 8:(r4 + 1) * 8, :])
                kb.dma("sp", w2, w2[:], I[which + "_w2"], I[which + "_w2"].t.rearrange("(jt p) d -> p jt d", p=128))
                kb.dma("sp", peT, peT[:], I["pe_" + which], I["pe_" + which][:])
                pcb = ps[2]
                for jt in range(2):
                    for r in range(32):
                        mm(kb, pcb, pcb[:, jt:jt + 1], w1, w1[:, r, jt * 128:(jt + 1) * 128], peT, peT[:, r:r + 1],
                           r == 0, r == 31)
                kb.op("dve", lambda e: e.tensor_copy(out=cb[:], in_=pcb[:, 0:2]), [pcb], [cb])
                xT3 = xT[:].rearrange("d (c s) -> d c s", s=16)
                for jt in range(2):
                    phd = ps[3 + jt]
                    for r in range(32):
                        mm(kb, phd, phd[:, 0:511], w1, w1[:, r, jt * 128:(jt + 1) * 128], xT,
                           xT3[:, (r // 16):(r // 16) + 511, r % 16], r == 0, r == 31)
                    kb.op("act", lambda e: e.activation(out=hidT[:, jt, 0:511], in_=phd[:, 0:511], func=AF.Silu,
                                                        bias=cb[:, jt:jt + 1]), [phd, cb], [hidT])
                pk = ps[5]
                for nt in range(4):
                    for jt in range(2):
                        mm(kb, pk, pk[:, nt * 64:(nt + 1) * 64], hidT, hidT[:, jt, nt * 128:(nt + 1) * 128], w2, w2[:, jt, :],
                           nt == 0 and jt == 0, jt == 1, skip=True)
                if which == "k":
                    kb.op("dve", lambda e: e.tensor_copy(out=craw[:].rearrange("p a d -> p (a d)"), in_=pk[:, 0:256]),
                          [pk], [craw])
                    emit_normrope(kb, craw, craw[:], 4, kg, cosC, cosC[:], sinC, sinC[:], kcn, kcn[:], nr_tmp)
                    pb = ps[0]
                    for nt in range(4):
                        kb.op("pe", lambda e: e.transpose(out=pb[0:64, nt * 128:(nt + 1) * 128], in_=kcn[:, nt, :],
                                                          identity=ident[:]), [kcn, ident], [pb])
                    kb.op("dve", lambda e: e.tensor_copy(out=kcT[:], in_=pb[0:64, :]), [pb], [kcT])
                else:
                    kb.op("dve", lambda e: e.tensor_copy(out=rhsc[:, :, 0:64],
                                                         in_=pk[:, 0:256].rearrange("p (a d) -> p a d", d=64)),
                          [pk], [rhsc])
        NC_ = T // 256
        Xq = [ph.sb([128, 2, 128], F32, "Xq") for _ in range(2)]
        qTf = ph.sb([64, 512], F32, "qTf")
        qTb = [ph.sb([128, 512], BF16, "qTb") for _ in range(2)]
        for q_ in qTb:
            kb.op("pool", lambda e: e.memset(q_[64:128, :], 0.0), [], [q_])
        PcT = [ph.sb([128, 512], F32, "PcT") for _ in range(4)]
        ocs = [ph.sb([128, 4, 64], F32, "ocs") for _ in range(2)]
        imph = ph.sb([128, 4, 128], F32, "imph")
        imp = ph.sb([128, 2, 128], F32, "imp")
        scr = ph.sb([128, 128], F32, "scr")
        m16 = ph.sb([128, 16], F32, "m16")
        self_ = ph.sb([128, 128], F32, "self")
        selT = [ph.sb([128, 2, 256], BF16, "selT") for _ in range(2)]
        rdF = ph.sb([128, 4], F32, "rdF")
        rdB = ph.sb([128, 2, 2, 2], F32, "rdB")
        Pb = [ph.sb([128, 512], BF16, "Pb") for _ in range(4)]
        yt = [ph.sb([128, 2, 128], F32, "yt") for _ in range(2)]
        ytT = [ph.sb([128, 256], F32, "ytT") for _ in range(2)]
        pF0, pF1 = ps[5], ps[6]
        pOs, pOw = ps[7], ps[0]

        def front(c):
            th = []
            X, q_b, sT, oc_ = Xq[c % 2], qTb[c % 2], selT[c % 2], ocs[c % 2]

            th.append(lambda: kb.dma("pool", X, X[:], I["qn"], qnv[:, 2 * c:2 * c + 2, :]))

            def t_qT():
                for h in range(2):
                    for tt in range(2):
                        c0 = h * 256 + tt * 128
                        kb.op("pe", lambda e: e.transpose(out=pF0[0:64, c0:c0 + 128], in_=X[:, tt, h * 64:(h + 1) * 64],
                                                          identity=ident[:]), [X, ident], [pF0])
                kb.op("act", lambda e: e.copy(qTf[:], pF0[0:64, :]), [pF0], [qTf])
                kb.op("dve", lambda e: e.tensor_copy(out=q_b[0:64, :], in_=qTf[:]), [qTf], [q_b])
            th.append(t_qT)
            nts = [nt for nt in range(4) if 2048 * nt + 31 <= 256 * c + 255]

            def t_cmpS(nt):
                mm(kb, pF0, pF0[:, :], kcT, kcT[:, nt * 128:(nt + 1) * 128], qTf, qTf[:], True, True)
                P = PcT[nt]
                kb.op("act", lambda e: e.activation(out=P[:], in_=pF0[:, :], func=AF.Exp), [pF0], [P])
                for hh_ in range(2):
                    Ph = P[:, hh_ * 256:(hh_ + 1) * 256]
                    kb.op("pool", lambda e: e.affine_select(out=Ph, in_=Ph, pattern=[[1, 256]], compare_op=ALU.is_ge,
                                                            fill=0.0, base=256 * c - 31 - 2048 * nt,
                                                            channel_multiplier=-16), [P], [P])
            for nt in nts:
                th.append(lambda nt=nt: t_cmpS(nt))

            def t_cmpO(half):
                for s2 in range(2):
                    sub = 2 * half + s2
                    o0 = s2 * 194
                    for i, nt in enumerate(nts):
                        mm(kb, pF1, pF1[:, o0:o0 + 194], PcT[nt], PcT[nt][:, sub * 128:(sub + 1) * 128], rhsc, rhsc[:, nt, :],
                           s2 == 0 and i == 0, i == len(nts) - 1, skip=True)
                for s2 in range(2):
                    sub = 2 * half + s2
                    o0 = s2 * 194
                    kb.op("dve", lambda e: e.tensor_scalar(out=rdF[:, sub:sub + 1], in0=pF1[:, o0 + 192:o0 + 193],
                                                           scalar1=1e-30, scalar2=None, op0=ALU.max), [pF1], [rdF])
                    kb.op("dve", lambda e: e.reciprocal(out=rdF[:, sub:sub + 1], in_=rdF[:, sub:sub + 1]), [rdF], [rdF])
                    kb.op("dve", lambda e: e.tensor_scalar(out=oc_[:, sub, :], in0=pF1[:, o0:o0 + 64],
                                                           scalar1=rdF[:, sub:sub + 1], scalar2=None, op0=ALU.mult),
                          [pF1, rdF], [oc_])
                    kb.op("dve", lambda e: e.tensor_scalar(out=imph[:, sub, :], in0=pF1[:, o0 + 64:o0 + 192],
                                                           scalar1=rdF[:, sub:sub + 1], scalar2=None, op0=ALU.mult),
                          [pF1, rdF], [imph])
            for half in range(2):
                th.append(lambda half=half: t_cmpO(half))
            th.append(lambda: kb.op("pool", lambda e: e.tensor_tensor(out=imp[:], in0=imph[:, 0:2, :], in1=imph[:, 2:4, :],
                                                                      op=ALU.add), [imph], [imp]))

            def t_edit(tt):
                for hf in range(2):
                    blk = 4 * c + 2 * tt + hf
                    rows = slice(64 * hf, 64 * hf + 64)
                    forced = sorted({0, blk} | ({blk - 1} if blk >= 1 else set()))
                    runs = []
                    for s_ in forced:
                        if runs and runs[-1][1] == s_:
                            runs[-1][1] = s_ + 1
                        else:
                            runs.append([s_, s_ + 1])
                    for a, b in runs:
                        kb.op("pool", lambda e: e.tensor_scalar(out=imp[rows, tt, a:b], in0=imp[rows, tt, a:b], scalar1=1e4,
                                                                scalar2=None, op0=ALU.add), [imp], [imp])
                    if blk < 127:
                        kb.op("pool", lambda e: e.memset(imp[rows, tt, blk + 1:128], -1e30), [], [imp])

            def t_topk(tt):
                kb.op("dve", lambda e: e.max(out=m16[:, 0:8], in_=imp[:, tt, :]), [imp], [m16])
                kb.op("dve", lambda e: e.match_replace(out=scr[:], in_to_replace=m16[:, 0:8], in_values=imp[:, tt, :],
                                                       imm_value=-3e38), [imp, m16], [scr])
                kb.op("dve", lambda e: e.max(out=m16[:, 8:16], in_=scr[:]), [scr], [m16])
                kb.op("dve", lambda e: e.tensor_scalar(out=self_[:], in0=imp[:, tt, :], scalar1=m16[:, 15:16], scalar2=None,
                                                       op0=ALU.is_ge), [imp, m16], [self_])
                kb.op("pe", lambda e: e.transpose(out=pF0[:, 0:128], in_=self_[:], identity=ident[:]), [self_, ident], [pF0])
                kb.op("act", lambda e: e.copy(sT[:, 0, tt * 128:(tt + 1) * 128], pF0[:, 0:128]), [pF0], [sT])
                kb.op("act", lambda e: e.copy(sT[:, 1, tt * 128:(tt + 1) * 128], pF0[:, 0:128]), [pF0], [sT])
            for tt in range(2):
                th.append(lambda tt=tt: t_edit(tt))
                th.append(lambda tt=tt: t_topk(tt))
            return th

        def back(c, pending):
            q_b, sT, oc_ = qTb[c % 2], selT[c % 2], ocs[c % 2]
            g_ = gs_all[:, 2 * c:2 * c + 2, :]
            sT2 = sT[:].rearrange("s h t -> s (h t)")
            wk = [kt for kt in range(2 * c - 4, 2 * c + 2) if kt >= 0]
            nk = 2 * c + 2
            its = [("w", kt, i, len(wk)) for i, kt in enumerate(wk)] + [("s", kt, kt, nk) for kt in range(nk)]

            def stA1(j):
                kind, kt, i, n = its[j]
                pS = ps[1 + j % 2]
                src = kwT if kind == "w" else ksT
                mm(kb, pS, pS[:, :], src, src[:, kt * 128:(kt + 1) * 128], q_b, q_b[:], True, True)
                if kind == "s":
                    pM = ps[3 + j % 2]
                    mm(kb, pM, pM[:, :], e128, e128[:, kt, :], sT, sT2, True, True)

            def stA2(j):
                kind, kt, i, n = its[j]
                pS, P = ps[1 + j % 2], Pb[j % 4]
                kb.op("act", lambda e: e.activation(out=P[:], in_=pS[:, :], func=AF.Exp), [pS], [P])

            def stB(j):
                kind, kt, i, n = its[j]
                P = Pb[j % 4]
                if kind == "s":
                    pM = ps[3 + j % 2]
                    kb.op("dve", lambda e: e.tensor_tensor(out=P[:], in0=P[:], in1=pM[:, :], op=ALU.mult), [P, pM], [P])
                for hh_ in range(2):
                    Ph = P[:, hh_ * 256:(hh_ + 1) * 256]
                    if kt >= 2 * c:
                        kb.op("pool", lambda e: e.affine_select(out=Ph, in_=Ph, pattern=[[1, 256]], compare_op=ALU.is_ge,
                                                                fill=0.0, base=256 * c - 128 * kt, channel_multiplier=-1),
                              [P], [P])
                    if kind == "w" and kt <= 2 * c - 3:
                        kb.op("pool", lambda e: e.affine_select(out=Ph, in_=Ph, pattern=[[-1, 256]], compare_op=ALU.is_gt,
                                                                fill=0.0, base=128 * kt - 256 * c + 512,
                                                                channel_multiplier=1), [P], [P])

            def stC(j):
                kind, kt, i, n = its[j]
                P = Pb[j % 4]
                pO, va = (pOw, vwa) if kind == "w" else (pOs, vsa)
                for sub in range(4):
                    mm(kb, pO, pO[:, sub * 65:(sub + 1) * 65], P, P[:, sub * 128:(sub + 1) * 128], va, va[:, kt, :],
                       i == 0 and sub == 0, i == n - 1, skip=True)

            stages = (stA1, stA2, stB, stC)
            n = len(its)
            for step in range(n + len(stages) - 1):
                for si in reversed(range(len(stages))):
                    j = step - si
                    if 0 <= j < n:
                        stages[si](j)
                for _ in range(1):
                    if pending:
                        pending.pop(0)()
            while pending:
                pending.pop(0)()
            y = yt[c % 2]
            g4 = g_.rearrange("p t (h b) -> p t h b", b=3)
            for bi, pO in ((1, pOs), (2, pOw)):
                den = pO[:, 0:260].rearrange("p (h t e) -> p t h e", h=2, t=2, e=65)[:, :, :, 64]
                rb_ = rdB[:, bi - 1, :, :]
                kb.op("dve", lambda e: e.tensor_scalar(out=rb_, in0=den, scalar1=1e-30, scalar2=None, op0=ALU.max), [pO], [rdB])
                kb.op("dve", lambda e: e.reciprocal(out=rb_, in_=rb_), [rdB], [rdB])
                kb.op("dve", lambda e: e.tensor_tensor(out=rb_, in0=rb_, in1=g4[:, :, :, bi], op=ALU.mult), [rdB, gs_all], [rdB])
            for sub in range(4):
                h, tt = sub // 2, sub % 2
                yo = y[:, tt, h * 64:(h + 1) * 64]
                kb.op("dve", lambda e: e.tensor_scalar(out=yo, in0=oc_[:, sub, :], scalar1=g_[:, tt, h * 3:h * 3 + 1],
                                                       scalar2=None, op0=ALU.mult), [oc_, gs_all], [y])
                for bi, pO in ((1, pOs), (2, pOw)):
                    kb.op("dve", lambda e: e.scalar_tensor_tensor(out=yo, in0=pO[:, sub * 65:sub * 65 + 64],
                                                                  scalar=rdB[:, bi - 1, tt, h:h + 1], in1=yo, op0=ALU.mult,
                                                                  op1=ALU.add), [pO, rdB, y], [y])
            if ytok is not None:
                kb.dma("sp", ytok, ytok[c * 256:(c + 1) * 256, :].rearrange("(t p) e -> p t e", p=128), y, y[:], anchor=y)
                return
            pyT = ps[0]
            for tt in range(2):
                kb.op("pe", lambda e: e.transpose(out=pyT[:, tt * 128:(tt + 1) * 128], in_=y[:, tt, :], identity=ident[:]),
                      [y, ident], [pyT])
            yT_ = ytT[c % 2]
            kb.op("act", lambda e: e.copy(yT_[:], pyT[:, 0:256]), [pyT], [yT_])
            kb.dma("sp", ynsa, ynsa[:, c * 256:(c + 1) * 256], yT_, yT_[:], anchor=yT_)

        for t_ in front(0):
            t_()
        for c in range(NC_):
            back(c, front(c + 1) if c + 1 < NC_ else [])


NF = 1792
NK = 1036


def win_perm():
    kv0 = 2048
    f = list(range(0, 1024)) + list(range(1024, 1536)) + list(range(kv0, kv0 + 128)) + list(range(kv0 + 128, kv0 + 256))
    k = (list(range(1536, 1792)) + list(range(1792, 2048)) + list(range(kv0 + 256, kv0 + 768)) + list(range(2816, 2828)))
    assert len(f) == NF and len(k) == NK
    return np.array(f + k, np.int64)


def emit_P2(kb, ps, ident, x, ntok, ccol, adaw, adab, gcol, winp, zT, ztok):
    with kb.phase():
        modT = kb.sb([128, 48], F32, "modT")
        with kb.phase() as ph:
            emit_mod(kb, ph, modT, ccol, adaw, adab, ps[0], ps[1])
        gm = emit_gmod(kb, modT, gcol, 8, "gm1")
        wb = kb.sb([128, 8, N_IN], BF16, "winb")
        win_v = winp.t.rearrange("(kt p) n -> p kt n", p=128)
        for kt in range(8):
            kb.dma("pool", wb, wb[:, kt, :], winp, win_v[:, kt, :])
        xt = [kb.sb([128, D], F32, "xt") for _ in range(2)]
        aT = [kb.sb([128, 8, 512], BF16, "aT") for _ in range(2)]
        zf = [kb.sb([128, 512], F32, "zf") for _ in range(3)]
        zk = [kb.sb([128, NK], F32, "zk") for _ in range(2)]
        tmps = [{"junk": kb.sb([128, D], F32, "junk"), "xs": kb.sb([128, D], F32, "xs"), "st": kb.sb([128, 4], F32, "st")}
                for _ in range(2)]
        kchunks = [(0, 512), (512, 512), (1024, NK - 1024)]
        NG = ntok // 512

        def norm_thunk(g, tt):
            def f():
                ab = aT[g % 2]
                i = 4 * g + tt
                xb = xt[i % 2]
                kb.dma("pool", xb, xb[:], x, x[i * 128:(i + 1) * 128, :])
                emit_norm_T(kb, xb, xb[:], gm, modT, 0, ident, ps[0:2],
                            [(ab, ab[:, kt, tt * 128:(tt + 1) * 128]) for kt in range(8)], None, tmps[i % 2])
            return f

        for tt in range(4):
            norm_thunk(0, tt)()
        ev = 0
        for g in range(NG):
            ab = aT[g % 2]
            nxt = [norm_thunk(g + 1, tt) for tt in range(4)] if g + 1 < NG else []
            for ct in range(NF // 128):
                pz = ps[2 + ev % 4]
                zb = zf[ev % 3]
                for kt in range(8):
                    mm(kb, pz, pz[:, :], wb, wb[:, kt, ct * 128:(ct + 1) * 128], ab, ab[:, kt, :], kt == 0, kt == 7)
                if ev % 2 == 0:
                    kb.op("act", lambda e: e.copy(zb[:], pz[:, :]), [pz], [zb])
                else:
                    kb.op("dve", lambda e: e.tensor_copy(out=zb[:], in_=pz[:, :]), [pz], [zb])
                ev += 1
                kb.dma("sp", zT, zT[ct * 128:(ct + 1) * 128, g * 512:(g + 1) * 512], zb, zb[:], anchor=zb)
                if ct % 4 == 3 and nxt:
                    nxt.pop(0)()
            for tt in range(4):
                i = 4 * g + tt
                zb = zk[i % 2]
                for (c0, cw) in kchunks:
                    pz = ps[2 + ev % 4]
                    for kt in range(8):
                        mm(kb, pz, pz[:, 0:cw], ab, ab[:, kt, tt * 128:(tt + 1) * 128], wb, wb[:, kt, NF + c0:NF + c0 + cw],
                           kt == 0, kt == 7)
                    if ev % 2 == 0:
                        kb.op("act", lambda e: e.copy(zb[:, c0:c0 + cw], pz[:, 0:cw]), [pz], [zb])
                    else:
                        kb.op("dve", lambda e: e.tensor_copy(out=zb[:, c0:c0 + cw], in_=pz[:, 0:cw]), [pz], [zb])
                    ev += 1
                kb.dma("sp", ztok, ztok[i * 128:(i + 1) * 128, :], zb, zb[:], anchor=zb)
                if tt == 1 and nxt:
                    nxt.pop(0)()
            while nxt:
                nxt.pop(0)()


def build_fused(depth=2):
    kb = KB()
    EI = "ExternalInput"
    x = kb.dram("x", [T, D], F32, EI)
    ccol = kb.dram("ccol", [128, 8], F32, EI)
    L = []
    for l in range(depth):
        d = {}
        for name, shape in (("adaw", [D, 6 * D]), ("adab", [1, 6 * D]), ("gcol1", [128, 8]), ("gcol2", [128, 8]),
                            ("winp", [D, N_IN]), ("wglu", [256, 256]), ("wout", [D, D]),
                            ("qg", [128, 64]), ("kg", [128, 64]), ("pe_k", [64, 32]), ("pe_v", [64, 32]),
                            ("k_w1", [2048, 256]), ("v_w1", [2048, 256]), ("k_w2", [256, 64]), ("v_w2", [256, 64])):
            d[name] = kb.dram(f"{name}_{l}", shape, F32, EI)
        for p in range(2):
            for name, shape in (("cw", [128, 3]), ("sp_lam", [128, 4, 2]), ("sp_ldt", [128, 4]), ("sp_b", [128, 4, 2, 128]),
                                ("sp_c", [128, 4, 2, 128]), ("sp_d", [128, 1])):
                d[f"{name}{p}"] = kb.dram(f"{name}_{l}_{p}", shape, F32, EI)
        moe = (l % 2 == 1)
        NE = NEXP if moe else 1
        d["wg"] = kb.dram(f"wg_{l}", [NE, D, DFF], F32, EI)
        d["wu"] = kb.dram(f"wu_{l}", [NE, D, DFF], F32, EI)
        d["wd"] = kb.dram(f"wd_{l}", [NE, DFF, D], F32, EI)
        d["router"] = kb.dram(f"router_{l}", [D, NEXP], F32, EI) if moe else None
        L.append(d)
    C = {}
    for name, shape, dt in (("pos", [128, 64], I32), ("pos_cmp", [128, 4], I32), ("invf", [128, 32], F32),
                            ("ov", [128, 4, 128], F32), ("e128", [128, 64, 128], F32)):
        C[name] = kb.dram("nsa_" + name, shape, dt, EI)
    HT = T // 2
    out = kb.dram("out", [HT, D], F32, "ExternalOutput")
    tokidx = kb.dram("tokidx", [128, HT // 128], I32, EI)
    ytokd = kb.dram("ytok_last", [T, D], F32)
    zT = kb.dram("zT", [NF, T], F32)
    ztok = kb.dram("ztok", [T, NK], F32)
    yT = kb.dram("yT", [D, T], F32)
    h1s = kb.dram("h1s", [T, D], F32)
    hmid = [kb.dram(f"hmid{l}", [T, D], F32) for l in range(depth - 1)]
    C["qn"] = kb.dram("nsa_qn", [T, 128], F32)

    ps = [kb.ps(f"P{i}") for i in range(8)]
    ident = make_ident(kb)
    hin = x
    for l in range(depth):
        d = L[l]
        moe = (l % 2 == 1)
        hout = out if l == depth - 1 else hmid[l]
        emit_P2(kb, ps, ident, hin, T, ccol, d["adaw"], d["adab"], d["gcol1"], d["winp"], zT, ztok)
        last = (l == depth - 1)

        def yv(c0):
            return View(ytokd, ytokd.t[:, c0:c0 + 128]) if last else None

        for p in range(2):
            conv_in = View(zT, zT.t[0:768, :].rearrange("(w c) t -> w c t", w=3)[:, 128 * p:128 * p + 128, :])
            emit_conv(kb, ps, conv_in, d[f"cw{p}"], View(yT, yT.t[128 * p:128 * p + 128, :]), ident=ident, ytok=yv(128 * p))
            emit_ssm(kb, ps, ident, View(zT, zT.t[768 + 128 * p:768 + 128 * p + 128, :]), d[f"sp_lam{p}"], d[f"sp_ldt{p}"],
                     d[f"sp_b{p}"], d[f"sp_c{p}"], d[f"sp_d{p}"], View(yT, yT.t[256 + 128 * p:256 + 128 * p + 128, :]),
                     ytok=yv(256 + 128 * p))
            qT = View(zT, zT.t[1024 + 128 * p:1024 + 128 * p + 128, :].rearrange("(h d) t -> h d t", h=2))
            kT = View(zT, zT.t[1280 + 128 * p:1280 + 128 * p + 128, :].rearrange("(h d) t -> h d t", h=2))
            emit_sb(kb, ps, ident, qT, kT, View(ztok, ztok.t[:, 128 * p:128 * p + 128]),
                    View(yT, yT.t[512 + 128 * p:512 + 128 * p + 128, :]), ytok=yv(512 + 128 * p))
            I = dict(C)
            I.update({"q": View(ztok, ztok.t[:, 256 + 128 * p:256 + 128 * p + 128]),
                      "kcT": View(zT, zT.t[1536 + 64 * p:1536 + 64 * p + 64, :]),
                      "vcT": View(zT, zT.t[1664 + 64 * p:1664 + 64 * p + 64, :]),
                      "ks": View(ztok, ztok.t[:, 512 + 64 * p:512 + 64 * p + 64]),
                      "vs": View(ztok, ztok.t[:, 640 + 64 * p:640 + 64 * p + 64]),
                      "kw": View(ztok, ztok.t[:, 768 + 64 * p:768 + 64 * p + 64]),
                      "vw": View(ztok, ztok.t[:, 896 + 64 * p:896 + 64 * p + 64]),
                      "gl": View(ztok, ztok.t[:, 1024 + 6 * p:1024 + 6 * p + 6])})
            for nm in ("qg", "kg", "pe_k", "pe_v", "k_w1", "v_w1", "k_w2", "v_w2"):
                I[nm] = d[nm]
            emit_nsa(kb, ps, ident, I, View(yT, yT.t[768 + 128 * p:768 + 128 * p + 128, :]), ytok=yv(768 + 128 * p))
        if last:
            emit_F(kb, ps, ident, moe, HT, hin, None, ccol, d["adaw"], d["adab"], d["gcol2"], d["wglu"], d["wout"],
                   d["wg"], d["wu"], d["wd"], d["router"], hout, h1s, gather={"idx": tokidx, "ytok": ytokd})
        else:
            emit_F(kb, ps, ident, moe, T, hin, yT, ccol, d["adaw"], d["adab"], d["gcol2"], d["wglu"], d["wout"],
                   d["wg"], d["wu"], d["wd"], d["router"], hout, h1s)
        hin = hout
    kb.finish([out])
    return kb


_PROGS = {}


def _col128(v):
    return np.ascontiguousarray(np.asarray(v).reshape(-1, 128).T)


def _C(a):
    return np.ascontiguousarray(a)


def kernel(**inp):
    inp = {k: np.asarray(v) for k, v in inp.items()}
    depth = inp["ada_w"].shape[0]
    if "fused" not in _PROGS:
        _PROGS["fused"] = build_fused(depth)
    kb = _PROGS["fused"]
    ov, e128, invf = nsa_consts()
    perm = win_perm()
    shared = {"nsa_invf": invf, "nsa_ov": ov, "nsa_e128": e128}
    for l in range(depth):
        moe = (l % 2 == 1)
        i = l // 2
        shared[f"adaw_{l}"] = inp["ada_w"][l]
        shared[f"adab_{l}"] = _C(inp["ada_b"][l][None, :])
        shared[f"gcol1_{l}"] = _col128(inp["norm_mix_g"][l])
        shared[f"gcol2_{l}"] = _col128(inp["norm_ffn_g"][l])
        shared[f"winp_{l}"] = _C(inp["w_in"][l][:, perm])
        shared[f"wglu_{l}"] = inp["ssm_w_glu"][l]
        shared[f"wout_{l}"] = inp["w_out"][l]
        shared[f"qg_{l}"] = _C(np.broadcast_to(inp["nsa_q_norm_g"][l][None, :], (128, 64)))
        shared[f"kg_{l}"] = _C(np.broadcast_to(inp["nsa_k_norm_g"][l][None, :], (128, 64)))
        shared[f"pe_k_{l}"] = _C(inp["cmp_pos_k"][l].T)
        shared[f"pe_v_{l}"] = _C(inp["cmp_pos_v"][l].T)
        shared[f"k_w1_{l}"] = inp["cmp_k_w1"][l]
        shared[f"v_w1_{l}"] = inp["cmp_v_w1"][l]
        shared[f"k_w2_{l}"] = inp["cmp_k_w2"][l]
        shared[f"v_w2_{l}"] = inp["cmp_v_w2"][l]
        for p in range(2):
            shared[f"cw_{l}_{p}"] = _C(inp["conv_w"][l][:, 128 * p:128 * p + 128].T)
            for k, v in ssm_layout(inp, l, p).items():
                shared[f"{k}_{l}_{p}"] = v
        if moe:
            shared[f"wg_{l}"] = inp["moe_w_gate"][i]
            shared[f"wu_{l}"] = inp["moe_w_up"][i]
            shared[f"wd_{l}"] = inp["moe_w_down"][i]
            shared[f"router_{l}"] = inp["moe_router"][i]
        else:
            shared[f"wg_{l}"] = inp["ffn_w_gate"][i:i + 1]
            shared[f"wu_{l}"] = inp["ffn_w_up"][i:i + 1]
            shared[f"wd_{l}"] = inp["ffn_w_down"][i:i + 1]
    maps = []
    for c in range(8):
        b = c // 2
        pos = inp["positions"][b].astype(np.int32)
        pc = np.zeros(512, np.int32)
        pc[:511] = pos[16 * np.arange(511) + 31]
        m = dict(shared)
        m["x"] = _C(inp["x"][b].astype(np.float32))
        m["ccol"] = _col128(inp["c"][b])
        m["nsa_pos"] = _C(pos.reshape(64, 128).T)
        m["nsa_pos_cmp"] = _C(pc.reshape(4, 128).T)
        half = c % 2
        m["tokidx"] = _C((half * (T // 2) + np.arange(T // 2, dtype=np.int32)).reshape(T // 256, 128).T)
        maps.append({k: _C(v) for k, v in m.items()})
    res = run_bass_kernel_spmd(kb.nc, maps, core_ids=list(range(8)))
    out = np.stack([np.concatenate([res.results[2 * b]["out"], res.results[2 * b + 1]["out"]], axis=0) for b in range(NB)])
    return out.astype(np.float32)
```
